# Optimizing a Trainium2 kernel written in Bass

```python
import math
import jax
import jax.numpy as jnp
from jax import lax
import numpy as np

D_MODEL = 2048
BATCH = 2
SEQ = 4096
DEPTH = 2

CTX_LEN = 256
GRID_W = 64
EPS = 1e-6
ROPE_THETA = 10000.0
Q_BLOCK = 128

DN_HEADS = 8
DN_HEAD_DIM = 128
DN_CONV = 5
DN_CHUNK = 64
MLA_HEADS = 8
MLA_Q_RANK = 768
MLA_KV_RANK = 512
MLA_NOPE = 128
MLA_ROPE = 64
MLA_V = 128
GQA_HEADS = 32
GQA_KV_HEADS = 4
GQA_HEAD_DIM = 64
WINDOW = 128
D_FF = 7168
N_EXPERTS = 8
TOP_K = 2
MOE_BLOCK = 128

DN_QKV = 3 * DN_HEADS * DN_HEAD_DIM
DN_IN = DN_QKV + DN_HEADS * DN_HEAD_DIM + 4 * DN_HEADS
MLA_IN = MLA_Q_RANK + MLA_KV_RANK + MLA_ROPE
AB_IN = DN_IN + MLA_IN
AB_OUT = DN_HEADS * DN_HEAD_DIM + MLA_HEADS * MLA_V
GQA_Q = GQA_HEADS * GQA_HEAD_DIM
GQA_IN = GQA_Q + 2 * GQA_KV_HEADS * GQA_HEAD_DIM

kernel_name = 'hybrid_deltanet_mla_swa_moe_flow_block'


def rms_norm(x, g):
    x32 = x.astype(jnp.float32)
    y = x32 * lax.rsqrt(jnp.mean(x32 * x32, axis=-1, keepdims=True) + EPS)
    return (y * g.astype(jnp.float32)).astype(x.dtype)


def l2_normalize(x):
    x32 = x.astype(jnp.float32)
    return (x32 * lax.rsqrt(jnp.sum(x32 * x32, axis=-1, keepdims=True) + EPS)).astype(x.dtype)


def modulate(x, g, shift, scale):
    return rms_norm(x, g) * (1 + scale) + shift


def axial_rope(rows, rot_dim):
    row = jnp.repeat(jnp.arange(rows), GRID_W).astype(jnp.float32)
    col = jnp.tile(jnp.arange(GRID_W), rows).astype(jnp.float32)
    n_freq = rot_dim // 4
    inv_freq = ROPE_THETA ** (-jnp.arange(n_freq, dtype=jnp.float32) / n_freq)
    ang = jnp.concatenate([row[:, None] * inv_freq, col[:, None] * inv_freq], axis=-1)
    return jnp.cos(ang), jnp.sin(ang)


def apply_rope(x, cos, sin):
    half = x.shape[-1] // 2
    shape = (1, cos.shape[0]) + (1,) * (x.ndim - 3) + (half,)
    c = cos.reshape(shape).astype(x.dtype)
    s = sin.reshape(shape).astype(x.dtype)
    x1, x2 = x[..., :half], x[..., half:]
    return jnp.concatenate([x1 * c - x2 * s, x2 * c + x1 * s], axis=-1)


def short_conv(x, w):
    pad = DN_CONV // 2
    return lax.conv_general_dilated(x, w[:, None, :].astype(x.dtype), window_strides=(1,),
                                    padding=[(pad, pad)], dimension_numbers=('NWC', 'WIO', 'NWC'),
                                    feature_group_count=x.shape[-1])


def swiglu(h, w_gate, w_up, w_down):
    return jnp.dot(jax.nn.silu(jnp.dot(h, w_gate)) * jnp.dot(h, w_up), w_down)


def block_attention(q, k, v, scale, sink=None):
    B, T, Hk, G, Dq = q.shape
    n_keys = k.shape[1]
    qb = jnp.moveaxis(q.reshape(B, T // Q_BLOCK, Q_BLOCK, Hk, G, Dq), 1, 0)

    def one(qi):
        s = jnp.einsum('bqhgd,bshd->bhgqs', qi, k).astype(jnp.float32) * scale
        if sink is not None:
            s_sink = jnp.broadcast_to(sink.astype(jnp.float32)[None, :, :, None, None], s.shape[:-1] + (1,))
            s = jnp.concatenate([s, s_sink], axis=-1)
        p = jax.nn.softmax(s, axis=-1)[..., :n_keys].astype(v.dtype)
        return jnp.einsum('bhgqs,bshd->bqhgd', p, v)

    o = lax.map(one, qb)
    return jnp.moveaxis(o, 0, 1).reshape(B, T, Hk, G, v.shape[-1])


def window_sink_attention(q, k, v, k_ctx, v_ctx, sink, scale):
    B, T, Hk, G, D = q.shape
    nb = T // Q_BLOCK
    span = Q_BLOCK + 2 * WINDOW
    n_ctx = k_ctx.shape[1]
    kp = jnp.pad(k, ((0, 0), (WINDOW, WINDOW), (0, 0), (0, 0)))
    vp = jnp.pad(v, ((0, 0), (WINDOW, WINDOW), (0, 0), (0, 0)))
    qb = jnp.moveaxis(q.reshape(B, nb, Q_BLOCK, Hk, G, D), 1, 0)
    rel = jnp.arange(span)[None, :] - WINDOW - jnp.arange(Q_BLOCK)[:, None]
    band = jnp.abs(rel) <= WINDOW
    sink_logit = sink.astype(jnp.float32)[None, :, :, None, None]

    def one(args):
        i, qi = args
        start = i * Q_BLOCK
        ki = lax.dynamic_slice_in_dim(kp, start, span, axis=1)
        vi = lax.dynamic_slice_in_dim(vp, start, span, axis=1)
        kpos = start - WINDOW + jnp.arange(span)
        valid = band & ((kpos >= 0) & (kpos < T))[None, :]
        s_ctx = jnp.einsum('bqhgd,bshd->bhgqs', qi, k_ctx).astype(jnp.float32) * scale
        s_lat = jnp.einsum('bqhgd,bshd->bhgqs', qi, ki).astype(jnp.float32) * scale
        s_lat = jnp.where(valid, s_lat, -jnp.inf)
        s_sink = jnp.broadcast_to(sink_logit, s_ctx.shape[:-1] + (1,))
        p = jax.nn.softmax(jnp.concatenate([s_ctx, s_lat, s_sink], axis=-1), axis=-1).astype(v.dtype)
        return (jnp.einsum('bhgqs,bshd->bqhgd', p[..., :n_ctx], v_ctx)
                + jnp.einsum('bhgqs,bshd->bqhgd', p[..., n_ctx:n_ctx + span], vi))

    o = lax.map(one, (jnp.arange(nb), qb))
    return jnp.moveaxis(o, 0, 1).reshape(B, T, Hk, G, D)


def gated_delta_chunked(q, k, v, beta, g, s0):
    B, T, H, Dk = q.shape
    Dv = v.shape[-1]
    C = DN_CHUNK
    n = T // C

    def chunk(t):
        t = t.astype(jnp.float32).reshape((B, n, C) + t.shape[2:])
        return jnp.moveaxis(jnp.swapaxes(t, 2, 3), 1, 0)

    q, k, v, beta, g = chunk(q), chunk(k), chunk(v), chunk(beta), chunk(g)
    gc = jnp.cumsum(g, axis=-1)
    idx = jnp.arange(C)
    incl = idx[:, None] >= idx[None, :]
    strict = idx[:, None] > idx[None, :]
    gamma = jnp.exp(jnp.where(incl, gc[..., :, None] - gc[..., None, :], -jnp.inf))
    a_mat = jnp.where(strict, beta[..., :, None] * jnp.einsum('nbhik,nbhjk->nbhij', k, k) * gamma, 0.0)
    rhs = jnp.concatenate([v * beta[..., None], k * (beta * jnp.exp(gc))[..., None]], axis=-1)
    sol = lax.linalg.triangular_solve(a_mat + jnp.eye(C, dtype=jnp.float32), rhs,
                                      left_side=True, lower=True, unit_diagonal=True)
    u, w = sol[..., :Dv], sol[..., Dv:]
    qk = jnp.einsum('nbhik,nbhjk->nbhij', q, k) * gamma
    q_dec = q * jnp.exp(gc)[..., None]
    k_dec = k * jnp.exp(gc[..., -1:] - gc)[..., None]
    last = jnp.exp(gc[..., -1])[..., None, None]

    def step(s, xs):
        u_i, w_i, qk_i, qd_i, kd_i, l_i = xs
        v_new = u_i - jnp.einsum('bhck,bhkv->bhcv', w_i, s)
        o_i = jnp.einsum('bhck,bhkv->bhcv', qd_i, s) + jnp.einsum('bhij,bhjv->bhiv', qk_i, v_new)
        s = s * l_i + jnp.einsum('bhck,bhcv->bhkv', kd_i, v_new)
        return s, o_i

    s_fin, o = lax.scan(step, s0.astype(jnp.float32), (u, w, qk, q_dec, k_dec, last))
    o = jnp.swapaxes(jnp.moveaxis(o, 0, 1), 2, 3).reshape(B, T, H, Dv)
    return o, s_fin


def deltanet_inputs(p, conv_w, a_log, dt_bias):
    B, T, _ = p.shape
    H, Dh = DN_HEADS, DN_HEAD_DIM
    qkv = jax.nn.silu(short_conv(p[..., :DN_QKV], conv_w)).reshape(B, T, 3, H, Dh)
    q = l2_normalize(qkv[:, :, 0]) * (Dh ** -0.5)
    k = l2_normalize(qkv[:, :, 1])
    v = qkv[:, :, 2]
    z = p[..., DN_QKV:DN_QKV + H * Dh].reshape(B, T, H, Dh)
    ba = p[..., DN_QKV + H * Dh:].astype(jnp.float32).reshape(B, T, 2, 2, H)
    beta = jax.nn.sigmoid(ba[:, :, 0])
    g = -jnp.exp(a_log.astype(jnp.float32)) * jax.nn.softplus(ba[:, :, 1] + dt_bias.astype(jnp.float32))
    return q, k, v, z, beta, g


def bidir_deltanet(q, k, v, beta, g, s_fwd, s_bwd):
    rev = lambda t: jnp.flip(t, axis=1)
    o_f, s_fwd = gated_delta_chunked(q, k, v, beta[:, :, 0], g[:, :, 0], s_fwd)
    o_b, s_bwd = gated_delta_chunked(rev(q), rev(k), rev(v), rev(beta[:, :, 1]), rev(g[:, :, 1]), s_bwd)
    return o_f + rev(o_b), s_fwd, s_bwd


def deltanet_out(o, z, norm_g):
    B, T = z.shape[:2]
    y = rms_norm(o, norm_g) * jax.nn.silu(z.astype(jnp.float32))
    return y.astype(z.dtype).reshape(B, T, DN_HEADS * DN_HEAD_DIM)


def mla_project(p, q_norm_g, w_qb, kv_norm_g, w_kvb, cos, sin):
    B, T, _ = p.shape
    cq = rms_norm(p[..., :MLA_Q_RANK], q_norm_g)
    ckv = rms_norm(p[..., MLA_Q_RANK:MLA_Q_RANK + MLA_KV_RANK], kv_norm_g)
    k_rope = p[..., MLA_Q_RANK + MLA_KV_RANK:]
    q = jnp.dot(cq, w_qb).reshape(B, T, MLA_HEADS, MLA_NOPE + MLA_ROPE)
    kv = jnp.dot(ckv, w_kvb).reshape(B, T, MLA_HEADS, MLA_NOPE + MLA_V)
    q_nope, q_rope = q[..., :MLA_NOPE], q[..., MLA_NOPE:]
    if cos is not None:
        q_rope = apply_rope(q_rope, cos, sin)
        k_rope = apply_rope(k_rope, cos, sin)
    k_rope = jnp.broadcast_to(k_rope[:, :, None, :], (B, T, MLA_HEADS, MLA_ROPE))
    q = jnp.concatenate([q_nope, q_rope], axis=-1)
    k = jnp.concatenate([kv[..., :MLA_NOPE], k_rope], axis=-1)
    return q, k, kv[..., MLA_NOPE:]


def mixer_ab(u, uc, w_in, conv_w, a_log, dt_bias, dn_norm_g, q_norm_g, w_qb, kv_norm_g, w_kvb, w_out,
             cos, sin, need_ctx):
    B, T, _ = u.shape
    p, pc = jnp.dot(u, w_in), jnp.dot(uc, w_in)
    qa, ka, va, za, beta, g = deltanet_inputs(p[..., :DN_IN], conv_w, a_log, dt_bias)
    qac, kac, vac, zac, beta_c, g_c = deltanet_inputs(pc[..., :DN_IN], conv_w, a_log, dt_bias)
    s0 = jnp.zeros((B, DN_HEADS, DN_HEAD_DIM, DN_HEAD_DIM), jnp.float32)
    oac, s_fwd, s_bwd = bidir_deltanet(qac, kac, vac, beta_c, g_c, s0, s0)
    oa, _, _ = bidir_deltanet(qa, ka, va, beta, g, s_fwd, s_bwd)
    qb, kb, vb = mla_project(p[..., DN_IN:], q_norm_g, w_qb, kv_norm_g, w_kvb, cos, sin)
    qbc, kbc, vbc = mla_project(pc[..., DN_IN:], q_norm_g, w_qb, kv_norm_g, w_kvb, None, None)
    scale = (MLA_NOPE + MLA_ROPE) ** -0.5
    ob = block_attention(qb[:, :, :, None], jnp.concatenate([kbc, kb], axis=1),
                         jnp.concatenate([vbc, vb], axis=1), scale)
    y = jnp.dot(jnp.concatenate([deltanet_out(oa, za, dn_norm_g),
                                 ob.reshape(B, T, MLA_HEADS * MLA_V)], axis=-1), w_out)
    yc = None
    if need_ctx:
        obc = block_attention(qbc[:, :, :, None], kbc, vbc, scale)
        yc = jnp.dot(jnp.concatenate([deltanet_out(oac, zac, dn_norm_g),
                                      obc.reshape(B, uc.shape[1], MLA_HEADS * MLA_V)], axis=-1), w_out)
    return y, yc


def gqa_project(h, w_qkv, b_qkv):
    B, T, _ = h.shape
    groups = GQA_HEADS // GQA_KV_HEADS
    p = jnp.dot(h, w_qkv) + b_qkv
    q = p[..., :GQA_Q].reshape(B, T, GQA_KV_HEADS, groups, GQA_HEAD_DIM)
    kv = p[..., GQA_Q:].reshape(B, T, 2, GQA_KV_HEADS, GQA_HEAD_DIM)
    return q, kv[:, :, 0], kv[:, :, 1]


def mixer_c(u, uc, w_qkv, b_qkv, sink, w_out, b_out, cos, sin, need_ctx):
    B, T, _ = u.shape
    sink_r = sink.reshape(GQA_KV_HEADS, GQA_HEADS // GQA_KV_HEADS)
    scale = GQA_HEAD_DIM ** -0.5
    q, k, v = gqa_project(u, w_qkv, b_qkv)
    q, k = apply_rope(q, cos, sin), apply_rope(k, cos, sin)
    qc, kc, vc = gqa_project(uc, w_qkv, b_qkv)
    o = window_sink_attention(q, k, v, kc, vc, sink_r, scale)
    y = jnp.dot(o.reshape(B, T, GQA_Q), w_out) + b_out
    yc = None
    if need_ctx:
        oc = block_attention(qc, kc, vc, scale, sink_r)
        yc = jnp.dot(oc.reshape(B, uc.shape[1], GQA_Q), w_out) + b_out
    return y, yc


def moe_swiglu(h, w_router, w_gate, w_up, w_down):
    n_tok, d = h.shape
    logits = jnp.dot(h, w_router).astype(jnp.float32)
    top_logit, top_idx = lax.top_k(logits, TOP_K)
    top_w = jax.nn.softmax(top_logit, axis=-1)
    n_assign = n_tok * TOP_K
    flat_e = top_idx.reshape(-1).astype(jnp.int32)
    flat_tok = jnp.repeat(jnp.arange(n_tok, dtype=jnp.int32), TOP_K)
    flat_w = top_w.reshape(-1)
    order = jnp.argsort(flat_e)
    sorted_e = flat_e[order]
    counts = jnp.zeros((N_EXPERTS,), jnp.int32).at[flat_e].add(1)
    padded = (counts + MOE_BLOCK - 1) // MOE_BLOCK * MOE_BLOCK
    start = jnp.cumsum(counts) - counts
    pad_end = jnp.cumsum(padded)
    pad_start = pad_end - padded
    dest = pad_start[sorted_e] + jnp.arange(n_assign, dtype=jnp.int32) - start[sorted_e]
    n_blocks = -(-n_assign // MOE_BLOCK) + N_EXPERTS
    slots = n_blocks * MOE_BLOCK
    slot_tok = jnp.full((slots,), n_tok, jnp.int32).at[dest].set(flat_tok[order])
    slot_w = jnp.zeros((slots,), jnp.float32).at[dest].set(flat_w[order])
    block_e = jnp.searchsorted(pad_end, jnp.arange(n_blocks, dtype=jnp.int32) * MOE_BLOCK, side='right')
    block_e = jnp.minimum(block_e, N_EXPERTS - 1)
    h_pad = jnp.concatenate([h, jnp.zeros((1, d), h.dtype)], axis=0)
    xb = h_pad[slot_tok].reshape(n_blocks, MOE_BLOCK, d)

    def expert_block(args):
        xi, e = args
        return swiglu(xi, w_gate[e], w_up[e], w_down[e])

    yb = lax.map(expert_block, (xb, block_e)).reshape(slots, d)
    yb = yb * slot_w[:, None].astype(yb.dtype)
    return jnp.zeros((n_tok + 1, d), yb.dtype).at[slot_tok].add(yb)[:n_tok]


def setup_inputs(seed: int = 0) -> dict:
    key = jax.random.key(seed)
    ks = iter(jax.random.split(key, 40))
    f32 = jnp.float32
    D = D_MODEL
    ne, no = (DEPTH + 1) // 2, DEPTH // 2

    def nrm(shape, scale):
        return jax.random.normal(next(ks), shape, f32) * scale

    x = nrm((BATCH, SEQ, D), 1.0)
    c = nrm((BATCH, D), 1.0)
    ctx = nrm((BATCH, CTX_LEN, D), 1.0)
    c_ctx = nrm((D,), 1.0)
    ada_w = nrm((DEPTH, D, 6 * D), 0.5 * D ** -0.5)
    ada_b = nrm((DEPTH, 6 * D), 0.01)
    norm_g = 1.0 + nrm((DEPTH, 4, D), 0.05)
    ab_w_in = nrm((ne, D, AB_IN), D ** -0.5)
    dn_conv_w = nrm((ne, DN_CONV, DN_QKV), DN_CONV ** -0.5)
    dn_a_log = jnp.log(jax.random.uniform(next(ks), (ne, 2, DN_HEADS), f32, 1.0, 16.0))
    dt = jnp.exp(jax.random.uniform(next(ks), (ne, 2, DN_HEADS), f32, math.log(1e-3), math.log(1e-1)))
    dn_dt_bias = dt + jnp.log(-jnp.expm1(-dt))
    dn_norm_g = 1.0 + nrm((ne, DN_HEAD_DIM), 0.05)
    mla_q_norm_g = 1.0 + nrm((ne, MLA_Q_RANK), 0.05)
    mla_w_qb = nrm((ne, MLA_Q_RANK, MLA_HEADS * (MLA_NOPE + MLA_ROPE)), MLA_Q_RANK ** -0.5)
    mla_kv_norm_g = 1.0 + nrm((ne, MLA_KV_RANK), 0.05)
    mla_w_kvb = nrm((ne, MLA_KV_RANK, MLA_HEADS * (MLA_NOPE + MLA_V)), MLA_KV_RANK ** -0.5)
    ab_w_out = nrm((ne, AB_OUT, D), AB_OUT ** -0.5)
    ffn_w_gate = nrm((ne, D, D_FF), D ** -0.5)
    ffn_w_up = nrm((ne, D, D_FF), D ** -0.5)
    ffn_w_down = nrm((ne, D_FF, D), D_FF ** -0.5)
    gqa_w_qkv = nrm((no, D, GQA_IN), D ** -0.5)
    gqa_b_qkv = nrm((no, GQA_IN), 0.01)
    gqa_sink = nrm((no, GQA_HEADS), 1.0)
    gqa_w_out = nrm((no, GQA_Q, D), GQA_Q ** -0.5)
    gqa_b_out = nrm((no, D), 0.01)
    moe_w_router = nrm((no, D, N_EXPERTS), D ** -0.5)
    moe_w_gate = nrm((no, N_EXPERTS, D, D_FF), D ** -0.5)
    moe_w_up = nrm((no, N_EXPERTS, D, D_FF), D ** -0.5)
    moe_w_down = nrm((no, N_EXPERTS, D_FF, D), D_FF ** -0.5)
    return {'x': x, 'c': c, 'ctx': ctx, 'c_ctx': c_ctx, 'ada_w': ada_w, 'ada_b': ada_b, 'norm_g': norm_g,
            'ab_w_in': ab_w_in, 'dn_conv_w': dn_conv_w, 'dn_a_log': dn_a_log, 'dn_dt_bias': dn_dt_bias,
            'dn_norm_g': dn_norm_g, 'mla_q_norm_g': mla_q_norm_g, 'mla_w_qb': mla_w_qb,
            'mla_kv_norm_g': mla_kv_norm_g, 'mla_w_kvb': mla_w_kvb, 'ab_w_out': ab_w_out,
            'ffn_w_gate': ffn_w_gate, 'ffn_w_up': ffn_w_up, 'ffn_w_down': ffn_w_down,
            'gqa_w_qkv': gqa_w_qkv, 'gqa_b_qkv': gqa_b_qkv, 'gqa_sink': gqa_sink, 'gqa_w_out': gqa_w_out,
            'gqa_b_out': gqa_b_out, 'moe_w_router': moe_w_router, 'moe_w_gate': moe_w_gate,
            'moe_w_up': moe_w_up, 'moe_w_down': moe_w_down}


def reference(x, c, ctx, c_ctx, ada_w, ada_b, norm_g, ab_w_in, dn_conv_w, dn_a_log, dn_dt_bias, dn_norm_g,
              mla_q_norm_g, mla_w_qb, mla_kv_norm_g, mla_w_kvb, ab_w_out, ffn_w_gate, ffn_w_up, ffn_w_down,
              gqa_w_qkv, gqa_b_qkv, gqa_sink, gqa_w_out, gqa_b_out, moe_w_router, moe_w_gate, moe_w_up,
              moe_w_down):
    B, T, D = x.shape
    rows = T // GRID_W
    cos_b, sin_b = axial_rope(rows, MLA_ROPE)
    cos_c, sin_c = axial_rope(rows, GQA_HEAD_DIM)
    silu_c = jax.nn.silu(c)[:, None, :]
    silu_cc = jax.nn.silu(c_ctx)
    h, hc = x, ctx
    for layer in range(DEPTH):
        need_ctx = layer < DEPTH - 1
        i = layer // 2
        mod = jnp.split(jnp.dot(silu_c, ada_w[layer]) + ada_b[layer], 6, axis=-1)
        mod_c = jnp.split(jnp.dot(silu_cc, ada_w[layer]) + ada_b[layer], 6, axis=-1)
        g_pre_m, g_post_m, g_pre_f, g_post_f = norm_g[layer]
        u = modulate(h, g_pre_m, mod[0], mod[1])
        uc = modulate(hc, g_pre_m, mod_c[0], mod_c[1])
        if layer % 2 == 0:
            y, yc = mixer_ab(u, uc, ab_w_in[i], dn_conv_w[i], dn_a_log[i], dn_dt_bias[i], dn_norm_g[i],
                             mla_q_norm_g[i], mla_w_qb[i], mla_kv_norm_g[i], mla_w_kvb[i], ab_w_out[i],
                             cos_b, sin_b, need_ctx)
        else:
            y, yc = mixer_c(u, uc, gqa_w_qkv[i], gqa_b_qkv[i], gqa_sink[i], gqa_w_out[i], gqa_b_out[i],
                            cos_c, sin_c, need_ctx)
        h = h + mod[2] * rms_norm(y, g_post_m)
        u = modulate(h, g_pre_f, mod[3], mod[4])
        if need_ctx:
            hc = hc + mod_c[2] * rms_norm(yc, g_post_m)
            uc = modulate(hc, g_pre_f, mod_c[3], mod_c[4])
        if layer % 2 == 0:
            y = swiglu(u, ffn_w_gate[i], ffn_w_up[i], ffn_w_down[i])
            if need_ctx:
                yc = swiglu(uc, ffn_w_gate[i], ffn_w_up[i], ffn_w_down[i])
        else:
            tok = u.reshape(-1, D)
            if need_ctx:
                tok = jnp.concatenate([tok, uc.reshape(-1, D)], axis=0)
            y_all = moe_swiglu(tok, moe_w_router[i], moe_w_gate[i], moe_w_up[i], moe_w_down[i])
            y = y_all[:B * T].reshape(B, T, D)
            if need_ctx:
                yc = y_all[B * T:].reshape(hc.shape)
        h = h + mod[5] * rms_norm(y, g_post_f)
        if need_ctx:
            hc = hc + mod_c[5] * rms_norm(yc, g_post_f)
    return h
```

```python
import contextlib
import numpy as np
import concourse.bass as bass
import concourse.mybir as mybir
from concourse.bass_utils import run_bass_kernel_spmd

F32 = mybir.dt.float32
BF16 = mybir.dt.bfloat16
I32 = mybir.dt.int32
AF = mybir.ActivationFunctionType
ALU = mybir.AluOpType
AX = mybir.AxisListType

D = 2048
DFF = 7168
EPS = 1e-6
ENGS = ('pe', 'act', 'dve', 'pool', 'sp')
ENGATTR = {'pe': 'tensor', 'act': 'scalar', 'dve': 'vector', 'pool': 'gpsimd', 'sp': 'sync'}
DMAQ = ('sp', 'act', 'pool')
NDMASEM = 4


class Op:
    __slots__ = ('eng', 'fn', 'deps', 'seq', 'sig', 'dma', 'dsem', 'dval')

    def __init__(self, eng, fn):
        self.eng = eng
        self.fn = fn
        self.deps = {}
        self.sig = False
        self.dma = 0
        self.dsem = None
        self.dval = 0


class Prog:
    def __init__(self, nc):
        self.nc = nc
        self.outer = contextlib.ExitStack()
        self.sems = {}
        for e in ENGS:
            self.sems[('c', e)] = self.outer.enter_context(nc.semaphore(f"c_{e}"))
        for q in DMAQ:
            for k in range(NDMASEM):
                self.sems[('d', q, k)] = self.outer.enter_context(nc.semaphore(f"d_{q}{k}"))
        self.base = {e: 0 for e in ENGS}
        self.dma_issued = {}
        self.dma_batches = {q: 0 for q in DMAQ}
        self.known = {e: {} for e in ENGS}
        self.n_names = 0
        self.es = None
        self.total_ops = {e: 0 for e in ENGS}

    def begin(self):
        self.es = contextlib.ExitStack()
        self.ops = {e: [] for e in ENGS}
        self.state = {}

    def sb(self, shape, dtype=F32, name=None):
        self.n_names += 1
        return self.es.enter_context(self.nc.sbuf_tensor(name or f"sb{self.n_names}", list(shape), dtype))

    def ps(self, shape, dtype=F32, name=None):
        self.n_names += 1
        return self.es.enter_context(self.nc.psum_tensor(name or f"ps{self.n_names}", list(shape), dtype))

    @staticmethod
    def _key(r):
        if isinstance(r, tuple):
            return (Prog._key(r[0]),) + tuple(r[1:])
        if isinstance(r, (str, int)):
            return r
        return r.name

    @staticmethod
    def _is_psum(k):
        if isinstance(k, tuple):
            k = k[0]
        return isinstance(k, str) and k.startswith('ps')

    def _dep_on(self, op, d):
        if d is None:
            return
        if d[0] == 'c' and d[1] == 'pe' and op.eng == 'pe':
            return
        if d[0] == 'c':
            key = ('c', d[1])
            if op.deps.get(key, -1) < d[2]:
                op.deps[key] = d[2]
        else:
            key = ('d', d[1], d[2])
            if op.deps.get(key, -1) < d[3]:
                op.deps[key] = d[3]

    def op(self, eng, fn, reads=(), writes=(), dma=0):
        reads = [self._key(r) for r in reads]
        writes = [self._key(w) for w in writes]
        pr = [r for r in reads if self._is_psum(r)]
        if pr:
            reads = [r for r in reads if not self._is_psum(r)]
            writes = writes + [r for r in pr if r not in writes]
        o = Op(eng, fn)
        lst = self.ops[eng]
        o.seq = len(lst)
        for r in reads:
            st = self.state.get(r)
            if st is not None:
                self._dep_on(o, st[0])
        for w in writes:
            st = self.state.get(w)
            if st is not None:
                self._dep_on(o, st[0])
                for rd in st[1]:
                    self._dep_on(o, rd)
        if dma:
            b = self.dma_batches[eng]
            self.dma_batches[eng] = b + 1
            k = b % NDMASEM
            prev = self.dma_issued.get((eng, k), 0)
            if prev:
                self._dep_on(o, ('d', eng, k, prev))
            val = prev + 16 * dma
            self.dma_issued[(eng, k)] = val
            o.dma = dma
            o.dsem = (eng, k)
            o.dval = val
            me = ('d', eng, k, val)
        else:
            me = ('c', eng, o.seq)
        for r in reads:
            st = self.state.setdefault(r, [None, []])
            st[1].append(me)
        for w in writes:
            self.state[w] = [me, []]
        lst.append(o)
        return o

    def end(self):
        nc = self.nc
        for e in ENGS:
            for o in self.ops[e]:
                for key, v in o.deps.items():
                    if key[0] == 'c':
                        self.ops[key[1]][v].sig = True
            for o in reversed(self.ops[e]):
                if not o.dma:
                    o.sig = True
                    break
        cnt = {}
        for e in ENGS:
            c = self.base[e]
            arr = []
            for o in self.ops[e]:
                if o.sig:
                    c += 1
                arr.append(c)
            cnt[e] = arr
        final = {e: (cnt[e][-1] if cnt[e] else self.base[e]) for e in ENGS}

        def replay(e, eng):
            known = self.known[e]
            for o in self.ops[e]:
                for key, v in o.deps.items():
                    val = cnt[key[1]][v] if key[0] == 'c' else v
                    if known.get(key, 0) >= val:
                        continue
                    known[key] = val
                    eng.wait_ge(self.sems[key], val)
                r = o.fn(eng)
                if o.dma:
                    if not isinstance(r, (list, tuple)):
                        r = [r]
                    assert len(r) == o.dma, (len(r), o.dma)
                    for ins in r:
                        ins.then_inc(self.sems[('d',) + o.dsem], 16)
                elif o.sig:
                    r.then_inc(self.sems[('c', e)], 1)
            for e2 in ENGS:
                if e2 == e:
                    continue
                key = ('c', e2)
                if final[e2] > known.get(key, 0):
                    known[key] = final[e2]
                    eng.wait_ge(self.sems[key], final[e2])
            for (q, k), val in self.dma_issued.items():
                key = ('d', q, k)
                if val > known.get(key, 0):
                    known[key] = val
                    eng.wait_ge(self.sems[key], val)

        with nc.Block() as block:
            for e in ENGS:
                getattr(block, ENGATTR[e])(lambda eng, e=e: replay(e, eng))
        for e in ENGS:
            self.base[e] = final[e]
            self.total_ops[e] += len(self.ops[e])
        self.es.close()
        self.es = None

    def close(self):
        self.outer.close()


def make_ident(P, dtype=F32):
    idf = P.sb([128, 128], F32)
    P.op('pool', lambda e: e.memset(idf[:], 0.0), writes=[idf])
    P.op('pool', lambda e: e.affine_select(out=idf[:], in_=idf[:], pattern=[[-1, 128]], compare_op=ALU.not_equal,
                                           fill=1.0, base=0, channel_multiplier=1), reads=[idf], writes=[idf])
    if dtype == F32:
        return idf
    idb = P.sb([128, 128], dtype)
    P.op('dve', lambda e: e.tensor_copy(out=idb[:], in_=idf[:]), reads=[idf], writes=[idb])
    return idb


def rstd_from_ss(P, ss, rstd, n, rk=None):
    P.op('dve', lambda e: e.tensor_scalar(out=rstd[:], in0=ss[:], scalar1=1.0 / n, scalar2=EPS, op0=ALU.mult, op1=ALU.add),
         reads=[ss], writes=[rstd])
    P.op('act', lambda e: e.activation(out=rstd[:], in_=rstd[:], func=AF.Sqrt), reads=[rstd], writes=[rstd])
    P.op('dve', lambda e: e.reciprocal(out=rstd[:], in_=rstd[:]), reads=[rstd], writes=[rstd])


def vec_pc(v):
    return v.rearrange("(c p) -> p c", p=128)


def stage_ada(P, cv, adaw, adab, modq):
    P.begin()
    NCOL = 6144
    cvt = P.sb([128, 16, 2], F32)
    st = P.sb([128, 16, 2], F32)
    wb = [P.sb([128, 16, 512], F32) for _ in range(2)]
    bt = P.sb([2, NCOL], F32)
    ot = P.sb([2, NCOL], F32)
    pp = [P.ps([2, 512], F32) for _ in range(2)]
    P.op('sp', lambda e: e.dma_start(out=cvt[:], in_=cv), writes=[cvt], dma=1)
    P.op('sp', lambda e: e.dma_start(out=bt[:], in_=adab.partition_broadcast(2)), writes=[bt], dma=1)
    P.op('act', lambda e: e.activation(out=st[:], in_=cvt[:], func=AF.Silu), reads=[cvt], writes=[st])
    wv = adaw.rearrange("(c p) n -> p c n", p=128)
    for pi in range(NCOL // 512):
        w = wb[pi % 2]
        q = 'sp' if pi % 2 == 0 else 'act'
        P.op(q, lambda e, w=w, pi=pi: e.dma_start(out=w[:], in_=wv[:, :, pi * 512:(pi + 1) * 512]), writes=[w], dma=1)
        ps = pp[pi % 2]
        for c in range(16):
            P.op('pe', lambda e, w=w, ps=ps, c=c: e.matmul(ps[:], lhsT=st[:, c, :], rhs=w[:, c, :], start=(c == 0), stop=(c == 15)),
                 reads=[st, w], writes=[ps])
        P.op('dve', lambda e, ps=ps, pi=pi: e.tensor_tensor(out=ot[:, pi * 512:(pi + 1) * 512], in0=ps[:],
                                                             in1=bt[:, pi * 512:(pi + 1) * 512], op=ALU.add),
             reads=[ps, bt], writes=[(ot, pi)])
    P.op('sp', lambda e: e.dma_start(out=modq, in_=ot[:]), reads=[(ot, pi) for pi in range(NCOL // 512)], writes=['modq'], dma=1)
    P.end()


def modvec(G, k, r):
    return G[k // 3, r, (k % 3) * 2048:(k % 3 + 1) * 2048]


class PCLoader:
    def __init__(self, P, ident, nchunk=16):
        self.P, self.ident, self.n = P, ident, nchunk
        self.rows = [P.sb([nchunk, 128], F32) for _ in range(2)]
        self.pt = P.ps([128, 512], F32)
        self.i = 0

    def load(self, q, dst, vec, n=None):
        P, ident = self.P, self.ident
        n = n or self.n
        row = self.rows[self.i % 2]
        self.i += 1
        pt = self.pt
        P.op(q, lambda e: e.dma_start(out=row[0:n, :], in_=vec.rearrange("(c p) -> c p", p=128)), writes=[row], dma=1)
        P.op('pe', lambda e: e.transpose(out=pt[:, 0:n], in_=row[0:n, :], identity=ident[0:n, 0:n]), reads=[row, ident], writes=[pt])
        P.op('dve', lambda e: e.tensor_copy(out=dst[:, 0:n], in_=pt[:, 0:n]), reads=[pt], writes=[dst])


MODSTOP = 9


def stage_modulate(P, h, uT, G, kg, ksh, ksc, normg, groups, ntiles, uT32=None, key_h='h', key_u='uT', t0=0):
    P.begin()
    NT = ntiles * 128
    ident = make_ident(P)
    rows = sorted(set(r for _, _, r in groups))
    gs, sh = {}, {}
    gt = P.sb([128, 16], F32)
    pcl = PCLoader(P, ident)
    pcl.load('sp', gt, normg)
    for r in rows:
        sc = P.sb([128, 16], F32)
        sh[r] = P.sb([128, 16], F32)
        gs[r] = P.sb([128, 16], F32)
        pcl.load('sp', sc, modvec(G, ksc, r))
        pcl.load('sp', sh[r], modvec(G, ksh, r))
        P.op('dve', lambda e, r=r, sc=sc: e.scalar_tensor_tensor(out=gs[r][:], in0=sc[:], scalar=1.0, in1=gt[:], op0=ALU.add, op1=ALU.mult),
             reads=[sc, gt], writes=[gs[r]])
    uTs = P.sb([128, 16, NT], BF16)
    zero1 = P.sb([128, 1], F32)
    P.op('pool', lambda e: e.memset(zero1[:], 0.0), writes=[zero1])
    uT32s = P.sb([128, 16, NT], F32) if uT32 is not None else None
    ht = [P.sb([128, D], F32) for _ in range(2)]
    xh = [P.sb([128, D], F32) for _ in range(2)]
    junk = P.sb([128, D], F32)
    ss = [P.sb([128, 1], F32) for _ in range(2)]
    rs = [P.sb([128, 1], F32) for _ in range(2)]
    pst = [P.ps([128, 512], F32) for _ in range(4)]
    npt = 0
    for lo, hi, r in groups:
        for t in range(lo, hi):
            b = t % 2
            P.op('sp', lambda e, t=t, b=b: e.dma_start(out=ht[b][:], in_=h[(t0 + t) * 128:(t0 + t + 1) * 128, :]), reads=[key_h], writes=[ht[b]], dma=1)
            P.op('act', lambda e, b=b: e.activation(out=junk[:], in_=ht[b][:], func=AF.Square, accum_out=ss[b][:]),
                 reads=[ht[b]], writes=[junk, ss[b]])
            if MODSTOP < 2:
                continue
            rstd_from_ss(P, ss[b], rs[b], D)
            P.op('dve', lambda e, b=b: e.tensor_scalar(out=xh[b][:], in0=ht[b][:], scalar1=rs[b][:, 0:1], scalar2=zero1[:, 0:1], op0=ALU.mult, op1=ALU.add),
                 reads=[ht[b], rs[b], zero1], writes=[xh[b]])
            for g4 in range(4 if MODSTOP >= 3 else 0):
                ps = pst[npt % 4]
                npt += 1
                for j in range(4):
                    c = g4 * 4 + j
                    P.op('pe', lambda e, ps=ps, j=j, c=c, b=b: e.transpose(out=ps[:, j * 128:(j + 1) * 128], in_=xh[b][:, c * 128:(c + 1) * 128], identity=ident[:]),
                         reads=[xh[b], ident], writes=[ps])
                for j in range(4):
                    c = g4 * 4 + j
                    if g4 % 2 == 0:
                        P.op('act', lambda e, ps=ps, j=j, c=c, t=t, r=r: e.activation(
                            out=uTs[:, c, t * 128:(t + 1) * 128], in_=ps[:, j * 128:(j + 1) * 128], func=AF.Identity,
                            scale=gs[r][:, c:c + 1], bias=sh[r][:, c:c + 1]), reads=[ps, gs[r], sh[r]], writes=[(uTs, t, c)])
                    else:
                        P.op('dve', lambda e, ps=ps, j=j, c=c, t=t, r=r: e.tensor_scalar(
                            out=uTs[:, c, t * 128:(t + 1) * 128], in0=ps[:, j * 128:(j + 1) * 128],
                            scalar1=gs[r][:, c:c + 1], scalar2=sh[r][:, c:c + 1], op0=ALU.mult, op1=ALU.add),
                            reads=[ps, gs[r], sh[r]], writes=[(uTs, t, c)])
                    if uT32s is not None:
                        P.op('pool' if False else 'dve', lambda e, ps=ps, j=j, c=c, t=t, r=r: e.tensor_scalar(
                            out=uT32s[:, c, t * 128:(t + 1) * 128], in0=ps[:, j * 128:(j + 1) * 128],
                            scalar1=gs[r][:, c:c + 1], scalar2=sh[r][:, c:c + 1], op0=ALU.mult, op1=ALU.add),
                            reads=[ps, gs[r], sh[r]], writes=[(uT32s, t, c)])
    allk = [(uTs, t, c) for lo, hi, r in groups for t in range(lo, hi) for c in range(16)]
    if MODSTOP < 3:
        P.op('pool', lambda e: e.memset(uTs[:], 0.0), writes=allk)
    for c4 in range(4):
        P.op('sp', lambda e, c4=c4: e.dma_start(out=uT[:, c4 * 4:(c4 + 1) * 4, t0 * 128:t0 * 128 + NT], in_=uTs[:, c4 * 4:(c4 + 1) * 4, :]), reads=allk, writes=[key_u], dma=1)
    if uT32s is not None:
        allk32 = [(uT32s, t, c) for lo, hi, r in groups for t in range(lo, hi) for c in range(16)]
        P.op('sp', lambda e: e.dma_start(out=uT32, in_=uT32s[:]), reads=allk32, writes=[key_u + '32'], dma=1)
    P.end()


def load_bc(P, q, dst, vec, n=128):
    P.op(q, lambda e: e.dma_start(out=dst[:], in_=vec.partition_broadcast(n)), writes=[dst], dma=1)


def stage_resid(P, y, h_in, h_out, G, kgate, normg, groups, key_y='y', key_hin='hin', key_hout='hout'):
    P.begin()
    rows = sorted(set(r for _, _, r in groups))
    gt = P.sb([128, D], F32)
    load_bc(P, 'sp', gt, normg)
    gm = {}
    for r in rows:
        gm[r] = P.sb([128, D], F32)
        load_bc(P, 'act', gm[r], modvec(G, kgate, r))
        P.op('pool', lambda e, r=r: e.tensor_tensor(out=gm[r][:], in0=gm[r][:], in1=gt[:], op=ALU.mult), reads=[gm[r], gt], writes=[gm[r]])
    yt = [P.sb([128, D], F32) for _ in range(2)]
    ht = [P.sb([128, D], F32) for _ in range(2)]
    ot = [P.sb([128, D], F32) for _ in range(2)]
    junk = P.sb([128, D], F32)
    ss = [P.sb([128, 1], F32) for _ in range(2)]
    rs = [P.sb([128, 1], F32) for _ in range(2)]
    for lo, hi, r in groups:
        for t in range(lo, hi):
            b = t % 2
            P.op('sp', lambda e, t=t, b=b: e.dma_start(out=yt[b][:], in_=y[t * 128:(t + 1) * 128, :]), reads=[key_y], writes=[yt[b]], dma=1)
            P.op('act', lambda e, t=t, b=b: e.dma_start(out=ht[b][:], in_=h_in[t * 128:(t + 1) * 128, :]), reads=[key_hin], writes=[ht[b]], dma=1)
            P.op('act', lambda e, b=b: e.activation(out=junk[:], in_=yt[b][:], func=AF.Square, accum_out=ss[b][:]),
                 reads=[yt[b]], writes=[junk, ss[b]])
            rstd_from_ss(P, ss[b], rs[b], D)
            P.op('dve', lambda e, b=b, r=r: e.scalar_tensor_tensor(out=ot[b][:], in0=yt[b][:], scalar=rs[b][:, 0:1], in1=gm[r][:],
                                                                   op0=ALU.mult, op1=ALU.mult), reads=[yt[b], rs[b], gm[r]], writes=[ot[b]])
            P.op('pool', lambda e, b=b: e.tensor_tensor(out=ot[b][:], in0=ot[b][:], in1=ht[b][:], op=ALU.add), reads=[ot[b], ht[b]], writes=[ot[b]])
            P.op('sp', lambda e, t=t, b=b: e.dma_start(out=h_out[t * 128:(t + 1) * 128, :], in_=ot[b][:]), reads=[ot[b]], writes=[key_hout], dma=1)
    P.end()

WMODE = 'cast'


def load_w_bf16(P, ws, wv, KC, ncols, pw, keyf):
    if WMODE == 'cast':
        for q in range(ncols // pw):
            P.op('pool', lambda e, q=q: e.dma_start(out=ws[:, :, q * pw:(q + 1) * pw], in_=wv[:, :, q * pw:(q + 1) * pw]),
                 writes=[keyf(q)], dma=1)
    else:
        stg = [P.sb([128, KC, pw], F32) for _ in range(2)]
        for q in range(ncols // pw):
            s_ = stg[q % 2]
            P.op('sp' if q % 2 == 0 else 'act', lambda e, q=q, s_=s_: e.dma_start(out=s_[:], in_=wv[:, :, q * pw:(q + 1) * pw]), writes=[s_], dma=1)
            P.op('pool', lambda e, q=q, s_=s_: e.tensor_copy(out=ws[:, :, q * pw:(q + 1) * pw], in_=s_[:]), reads=[s_], writes=[keyf(q)])


def stage_outproj(P, inT, W, y, ntiles, KC=16, bias=None, key_in='inT', key_y='y'):
    P.begin()
    NT = ntiles * 128
    xs = P.sb([128, KC, NT], BF16)
    if isinstance(inT, (list, tuple)):
        off = 0
        for i, (ap_, kc_) in enumerate(inT):
            P.op('sp' if i % 2 == 0 else 'act', lambda e, ap_=ap_, off=off, kc_=kc_: e.dma_start(out=xs[:, off:off + kc_, :], in_=ap_), reads=[key_in], writes=[(xs, i)], dma=1)
            off += kc_
        xs_keys = [(xs, i) for i in range(len(inT))]
    else:
        P.op('sp', lambda e: e.dma_start(out=xs[:], in_=inT), reads=[key_in], writes=[xs], dma=1)
        xs_keys = [xs]
    wv = W.rearrange("(c p) n -> p c n", p=128)
    wsl = [P.sb([128, KC, 512], BF16) for _ in range(4)]
    for q4 in range(4):
        load_w_bf16(P, wsl[q4], wv[:, :, q4 * 512:(q4 + 1) * 512], KC, 512, 512, lambda q, q4=q4: wsl[q4])
    bt = None
    if bias is not None:
        bt = P.sb([128, D], F32)
        load_bc(P, 'act', bt, bias)
    pp = [P.ps([128, 512], F32) for _ in range(8)]
    ot = [P.sb([128, D], F32) for _ in range(2)]
    n = 0
    for t in range(ntiles):
        b = t % 2
        for q4 in range(4):
            ps = pp[n % 8]
            n += 1
            for c in range(KC):
                P.op('pe', lambda e, ps=ps, c=c, t=t, q4=q4: e.matmul(ps[:], lhsT=xs[:, c, t * 128:(t + 1) * 128], rhs=wsl[q4][:, c, :],
                                                                  start=(c == 0), stop=(c == KC - 1)), reads=xs_keys + [wsl[q4]], writes=[ps])
            if bt is None:
                eng = 'act' if q4 % 2 == 0 else 'dve'
                if eng == 'act':
                    P.op('act', lambda e, ps=ps, b=b, q4=q4: e.copy(out=ot[b][:, q4 * 512:(q4 + 1) * 512], in_=ps[:]), reads=[ps], writes=[(ot[b], q4)])
                else:
                    P.op('dve', lambda e, ps=ps, b=b, q4=q4: e.tensor_copy(out=ot[b][:, q4 * 512:(q4 + 1) * 512], in_=ps[:]), reads=[ps], writes=[(ot[b], q4)])
            else:
                P.op('dve', lambda e, ps=ps, b=b, q4=q4: e.tensor_tensor(out=ot[b][:, q4 * 512:(q4 + 1) * 512], in0=ps[:], in1=bt[:, q4 * 512:(q4 + 1) * 512], op=ALU.add),
                     reads=[ps, bt], writes=[(ot[b], q4)])
        P.op('sp', lambda e, t=t, b=b: e.dma_start(out=y[t * 128:(t + 1) * 128, :], in_=ot[b][:]), reads=[(ot[b], q4) for q4 in range(4)], writes=[key_y], dma=1)
    P.end()


def stage_ffn(P, uT, Wg, Wu, Wd, y, ntiles, blocks, key_u='uT', key_y='y'):
    NT = ntiles * 128
    FC = DFF // 128
    mid = contextlib.ExitStack()
    P.n_names += 1
    h1T = mid.enter_context(P.nc.sbuf_tensor(f"h1T{P.n_names}", [128, FC, NT], BF16))
    P.begin()
    us = P.sb([128, 16, NT], BF16)
    P.op('sp', lambda e: e.dma_start(out=us[:], in_=uT), reads=[key_u], writes=[us], dma=1)
    PW = 256
    wg = [P.sb([128, 16, PW], BF16) for _ in range(2)]
    wu = [P.sb([128, 16, PW], BF16) for _ in range(2)]
    sg = [P.sb([128, 512], F32) for _ in range(2)]
    pg = [P.ps([128, 512], F32) for _ in range(2)]
    pu = [P.ps([128, 512], F32) for _ in range(2)]
    wgv = Wg.rearrange("(c p) n -> p c n", p=128)
    wuv = Wu.rearrange("(c p) n -> p c n", p=128)
    n = 0
    for pi in range(DFF // PW):
        b = pi % 2
        P.op('pool', lambda e, b=b, pi=pi: e.dma_start(out=wg[b][:], in_=wgv[:, :, pi * PW:(pi + 1) * PW]), writes=[wg[b]], dma=1)
        P.op('pool', lambda e, b=b, pi=pi: e.dma_start(out=wu[b][:], in_=wuv[:, :, pi * PW:(pi + 1) * PW]), writes=[wu[b]], dma=1)
        for m in range(PW // 128):
            fc = pi * (PW // 128) + m
            for (lo, hi) in blocks:
                w = hi - lo
                k = n % 2
                n += 1
                for c in range(16):
                    P.op('pe', lambda e, k=k, c=c, b=b, m=m, lo=lo, hi=hi, w=w: e.matmul(
                        pg[k][:, 0:w], lhsT=wg[b][:, c, m * 128:(m + 1) * 128], rhs=us[:, c, lo:hi], start=(c == 0), stop=(c == 15)),
                        reads=[wg[b], us], writes=[pg[k]])
                for c in range(16):
                    P.op('pe', lambda e, k=k, c=c, b=b, m=m, lo=lo, hi=hi, w=w: e.matmul(
                        pu[k][:, 0:w], lhsT=wu[b][:, c, m * 128:(m + 1) * 128], rhs=us[:, c, lo:hi], start=(c == 0), stop=(c == 15)),
                        reads=[wu[b], us], writes=[pu[k]])
                P.op('act', lambda e, k=k, w=w: e.activation(out=sg[k][:, 0:w], in_=pg[k][:, 0:w], func=AF.Silu), reads=[pg[k]], writes=[sg[k]])
                P.op('dve', lambda e, k=k, w=w, fc=fc, lo=lo, hi=hi: e.tensor_tensor(out=h1T[:, fc, lo:hi], in0=sg[k][:, 0:w], in1=pu[k][:, 0:w], op=ALU.mult),
                     reads=[sg[k], pu[k]], writes=[('h1T', fc)])
    P.end()
    P.begin()
    PD = 256
    wd = [P.sb([128, FC, PD], BF16) for _ in range(2)]
    oy = [P.sb([128, PD], F32) for _ in range(4)]
    py = [P.ps([128, 512], F32) for _ in range(4)]
    wdv = Wd.rearrange("(c p) n -> p c n", p=128)
    n = 0
    for pi in range(D // PD):
        b = pi % 2
        h2 = FC // 2
        P.op('pool', lambda e, b=b, pi=pi: e.dma_start(out=wd[b][:, 0:h2, :], in_=wdv[:, 0:h2, pi * PD:(pi + 1) * PD]), writes=[(wd[b], 0)], dma=1)
        P.op('pool', lambda e, b=b, pi=pi: e.dma_start(out=wd[b][:, h2:FC, :], in_=wdv[:, h2:FC, pi * PD:(pi + 1) * PD]), writes=[(wd[b], 1)], dma=1)
        for t in range(ntiles):
            k = n % 4
            n += 1
            for c in range(FC):
                P.op('pe', lambda e, k=k, c=c, b=b, t=t: e.matmul(py[k][:, 0:PD], lhsT=h1T[:, c, t * 128:(t + 1) * 128], rhs=wd[b][:, c, :],
                                                                  start=(c == 0), stop=(c == FC - 1)), reads=[(wd[b], 0), (wd[b], 1)], writes=[py[k]])
            if k % 2 == 0:
                P.op('act', lambda e, k=k: e.copy(out=oy[k][:], in_=py[k][:, 0:PD]), reads=[py[k]], writes=[oy[k]])
            else:
                P.op('dve', lambda e, k=k: e.tensor_copy(out=oy[k][:], in_=py[k][:, 0:PD]), reads=[py[k]], writes=[oy[k]])
            P.op('sp', lambda e, k=k, t=t, pi=pi: e.dma_start(out=y[t * 128:(t + 1) * 128, pi * PD:(pi + 1) * PD], in_=oy[k][:]),
                 reads=[oy[k]], writes=[key_y], dma=1)
    P.end()
    mid.close()


NTB = 4352
NTB_T = 34


def stage_proj(P, uT, Wfm, fm_out, fm_rows, Wtm, tm_out, ntm, ntiles=NTB_T, key_u='uT', KC=16, bias_fm=None, bias_tm=None):
    P.begin()
    NT = ntiles * 128
    ng = len(fm_rows)
    wfm = []
    off = 0
    for gi, rws in enumerate(fm_rows):
        wt = P.sb([128, KC, rws], BF16)
        P.op('pool', lambda e, wt=wt, off=off, rws=rws: e.dma_start(out=wt[:], in_=Wfm.rearrange("(c p) n -> p c n", p=128)[:, :, off:off + rws]),
             writes=[wt], dma=1)
        wfm.append(wt)
        off += rws
    wtm = None
    if ntm:
        wtm = P.sb([128, KC, ntm], BF16)
        P.op('pool', lambda e: e.dma_start(out=wtm[:], in_=Wtm.rearrange("(c p) n -> p c n", p=128)), writes=[wtm], dma=1)
    bfm = None
    one1 = P.sb([128, 1], F32)
    P.op('pool', lambda e: e.memset(one1[:], 1.0), writes=[one1])
    if bias_fm is not None:
        ident = make_ident(P)
        pcl = PCLoader(P, ident, nchunk=ng)
        bfm = P.sb([128, ng], F32)
        pcl.load('sp', bfm, bias_fm, n=ng)
    btm = None
    if bias_tm is not None:
        btm = P.sb([128, ntm], F32)
        load_bc(P, 'act', btm, bias_tm)
    ub = [P.sb([128, KC, 512], BF16) for _ in range(2)]
    ofm = [P.sb([128, 512], F32) for _ in range(4)]
    otm = [P.sb([128, max(ntm, 1)], F32) for _ in range(2)]
    pf = [P.ps([128, 512], F32) for _ in range(4)]
    pt = [P.ps([128, 512], F32) for _ in range(2)]
    nblk = (ntiles + 3) // 4
    n = 0
    ntmc = 0
    for bi in range(nblk):
        tl = min(4, ntiles - bi * 4)
        w = tl * 128
        lo = bi * 512
        u = ub[bi % 2]
        P.op('sp', lambda e, u=u, lo=lo, w=w: e.dma_start(out=u[:, :, 0:w], in_=uT[:, :, lo:lo + w]), reads=[key_u], writes=[u], dma=1)
        for gi, rws in enumerate(fm_rows):
            k = n % 4
            n += 1
            for c in range(KC):
                P.op('pe', lambda e, k=k, c=c, gi=gi, u=u, w=w, rws=rws: e.matmul(pf[k][0:rws, 0:w], lhsT=wfm[gi][:, c, :], rhs=u[:, c, 0:w],
                                                                                 start=(c == 0), stop=(c == KC - 1)), reads=[wfm[gi], u], writes=[pf[k]])
            if bfm is not None:
                P.op('act', lambda e, k=k, w=w, rws=rws, gi=gi: e.activation(out=ofm[k][0:rws, 0:w], in_=pf[k][0:rws, 0:w], func=AF.Identity,
                                                                            bias=bfm[0:rws, gi:gi + 1], scale=one1[0:rws, 0:1]), reads=[pf[k], bfm, one1], writes=[ofm[k]])
            elif k % 2 == 0:
                P.op('act', lambda e, k=k, w=w, rws=rws: e.copy(out=ofm[k][0:rws, 0:w], in_=pf[k][0:rws, 0:w]), reads=[pf[k]], writes=[ofm[k]])
            else:
                P.op('dve', lambda e, k=k, w=w, rws=rws: e.tensor_copy(out=ofm[k][0:rws, 0:w], in_=pf[k][0:rws, 0:w]), reads=[pf[k]], writes=[ofm[k]])
            P.op('sp' if k % 2 == 0 else 'act', lambda e, k=k, w=w, rws=rws, gi=gi, lo=lo: e.dma_start(out=fm_out[gi, 0:rws, lo:lo + w], in_=ofm[k][0:rws, 0:w]),
                 reads=[ofm[k]], writes=['fm_out'], dma=1)
        if ntm:
            for ti in range(tl):
                k = ntmc % 2
                ntmc += 1
                for c in range(KC):
                    P.op('pe', lambda e, k=k, c=c, u=u, ti=ti: e.matmul(pt[k][:, 0:ntm], lhsT=u[:, c, ti * 128:(ti + 1) * 128], rhs=wtm[:, c, :],
                                                                       start=(c == 0), stop=(c == KC - 1)), reads=[wtm, u], writes=[pt[k]])
                if btm is not None:
                    P.op('dve', lambda e, k=k: e.tensor_tensor(out=otm[k][:], in0=pt[k][:, 0:ntm], in1=btm[:], op=ALU.add), reads=[pt[k], btm], writes=[otm[k]])
                else:
                    P.op('dve', lambda e, k=k: e.tensor_copy(out=otm[k][:], in_=pt[k][:, 0:ntm]), reads=[pt[k]], writes=[otm[k]])
                P.op('sp', lambda e, k=k, ti=ti, lo=lo: e.dma_start(out=tm_out[lo + ti * 128:lo + (ti + 1) * 128, :], in_=otm[k][:]),
                     reads=[otm[k]], writes=['tm_out'], dma=1)
    P.end()


SEGS = [(0, 256), (256, NTB)]


def stage_dn_pre(P, pre, convw, fmT, tokm):
    P.begin()
    identb = make_ident(P, BF16)
    ones_b = P.sb([128, 128], BF16)
    P.op('pool', lambda e: e.memset(ones_b[:], 1.0), writes=[ones_b])
    zero1 = P.sb([128, 1], F32)
    P.op('pool', lambda e: e.memset(zero1[:], 0.0), writes=[zero1])
    xin = [P.sb([128, NTB], F32) for _ in range(2)]
    acc = [P.sb([128, NTB], F32) for _ in range(2)]
    cw = P.sb([128, 6, 5], F32)
    P.op('sp', lambda e: e.dma_start(out=cw[:], in_=convw.rearrange("g p j -> p g j")), writes=[cw], dma=1)
    ob = [P.sb([128, NTB], BF16) for _ in range(2)]
    sq = [P.sb([128, 512], BF16) for _ in range(2)]
    rn = [P.sb([128, 512], F32) for _ in range(2)]
    pss = [P.ps([128, 512], F32) for _ in range(2)]
    ptr = [P.ps([128, 1024], BF16) for _ in range(2)]
    tk = [P.sb([128, 8, 128], BF16) for _ in range(2)]
    ntr = 0
    for g in range(6):
        hh, ty = g // 3, g % 3
        b = g % 2
        ve = 'dve'
        P.op('sp', lambda e, g=g, b=b: e.dma_start(out=xin[b][:], in_=pre[g]), writes=[xin[b]], dma=1)
        P.op(ve, lambda e, g=g, b=b: e.tensor_scalar(out=acc[b][:], in0=xin[b][:], scalar1=cw[:, g, 2:3], scalar2=zero1[:, 0:1], op0=ALU.mult, op1=ALU.add),
             reads=[xin[b], cw, zero1], writes=[acc[b]])
        for j in (0, 1, 3, 4):
            d = j - 2
            for (s0, s1) in SEGS:
                a, bb = max(s0, s0 - d), min(s1, s1 - d)
                P.op(ve, lambda e, g=g, b=b, j=j, a=a, bb=bb, d=d: e.scalar_tensor_tensor(
                    out=acc[b][:, a:bb], in0=xin[b][:, a + d:bb + d], scalar=cw[:, g, j:j + 1], in1=acc[b][:, a:bb], op0=ALU.mult, op1=ALU.add),
                    reads=[xin[b], cw, acc[b]], writes=[acc[b]])
        P.op('act', lambda e, b=b: e.activation(out=acc[b][:], in_=acc[b][:], func=AF.Silu), reads=[acc[b]], writes=[acc[b]])
        if ty < 2:
            qs = (128.0 ** -0.5) if ty == 0 else 1.0
            for bi in range((NTB + 511) // 512):
                lo = bi * 512
                w = min(512, NTB - lo)
                k = bi % 2
                P.op('act', lambda e, b=b, k=k, lo=lo, w=w: e.activation(out=sq[k][:, 0:w], in_=acc[b][:, lo:lo + w], func=AF.Square),
                     reads=[acc[b]], writes=[sq[k]])
                P.op('pe', lambda e, k=k, w=w: e.matmul(pss[k][:, 0:w], lhsT=ones_b[:], rhs=sq[k][:, 0:w], start=True, stop=True),
                     reads=[ones_b, sq[k]], writes=[pss[k]])
                P.op('dve', lambda e, k=k, w=w, qs=qs: e.tensor_scalar(out=rn[k][:, 0:w], in0=pss[k][:, 0:w], scalar1=1.0 / (qs * qs), scalar2=EPS / (qs * qs),
                                                               op0=ALU.mult, op1=ALU.add), reads=[pss[k]], writes=[rn[k]])
                P.op('act', lambda e, k=k, w=w: e.activation(out=rn[k][:, 0:w], in_=rn[k][:, 0:w], func=AF.Sqrt), reads=[rn[k]], writes=[rn[k]])
                P.op('dve', lambda e, k=k, w=w: e.reciprocal(out=rn[k][:, 0:w], in_=rn[k][:, 0:w]), reads=[rn[k]], writes=[rn[k]])
                P.op('dve', lambda e, b=b, k=k, lo=lo, w=w: e.tensor_tensor(out=ob[b][:, lo:lo + w], in0=acc[b][:, lo:lo + w], in1=rn[k][:, 0:w], op=ALU.mult),
                     reads=[acc[b], rn[k]], writes=[ob[b]])
            P.op('sp', lambda e, b=b, hh=hh, ty=ty: e.dma_start(out=fmT[hh * 2 + ty], in_=ob[b][:]), reads=[ob[b]], writes=['fmT'], dma=1)
        else:
            P.op('dve', lambda e, b=b: e.tensor_copy(out=ob[b][:], in_=acc[b][:]), reads=[acc[b]], writes=[ob[b]])
        if ty >= 1:
            for t8 in range((NTB_T + 7) // 8):
                nt = min(8, NTB_T - t8 * 8)
                pp = ptr[ntr % 2]
                tt = tk[ntr % 2]
                ntr += 1
                for i in range(nt):
                    t = t8 * 8 + i
                    P.op('pe', lambda e, pp=pp, i=i, t=t, b=b: e.transpose(out=pp[:, i * 128:(i + 1) * 128], in_=ob[b][:, t * 128:(t + 1) * 128], identity=identb[:]),
                         reads=[ob[b], identb], writes=[pp])
                P.op('act' if t8 % 2 == 0 else 'dve', lambda e, pp=pp, tt=tt, nt=nt, t8=t8: (e.copy if t8 % 2 == 0 else e.tensor_copy)(
                    out=tt[:, 0:nt, :], in_=pp[:, 0:nt * 128].rearrange("p (a b) -> p a b", b=128)), reads=[pp], writes=[tt])
                P.op('sp', lambda e, tt=tt, nt=nt, t8=t8, hh=hh, ty=ty: e.dma_start(
                    out=tokm[hh * 2 + ty - 1, t8 * 1024:t8 * 1024 + nt * 128, :].rearrange("(a p) d -> p a d", p=128), in_=tt[:, 0:nt, :]),
                    reads=[tt], writes=['tokm'], dma=1)
    P.end()


def tri_mask(P, kind):
    m = P.sb([128, 128], F32)
    P.op('pool', lambda e: e.memset(m[:], 1.0), writes=[m])
    if kind in ('ge', 'gt'):
        pat, cm = [[-1, 128]], 1
    else:
        pat, cm = [[1, 128]], -1
    op = ALU.is_ge if kind in ('ge', 'le') else ALU.is_gt
    P.op('pool', lambda e: e.affine_select(out=m[:], in_=m[:], pattern=pat, compare_op=op, fill=0.0, base=0, channel_multiplier=cm),
         reads=[m], writes=[m])
    return m


def stage_dn_scan(P, fmT, tokm, tm, alog8, dtb8, dng, dnout, ZC=256):
    P.begin()
    NTt = NTB_T
    identf = make_ident(P)
    zero1 = P.sb([128, 1], F32)
    negone1 = P.sb([128, 1], F32)
    one1 = P.sb([128, 1], F32)
    ones_f = P.sb([128, 128], F32)
    P.op('pool', lambda e: e.memset(zero1[:], 0.0), writes=[zero1])
    P.op('pool', lambda e: e.memset(negone1[:], -1.0), writes=[negone1])
    P.op('pool', lambda e: e.memset(one1[:], 1.0), writes=[one1])
    P.op('pool', lambda e: e.memset(ones_f[:], 1.0), writes=[ones_f])
    L = {0: tri_mask(P, 'le'), 1: tri_mask(P, 'ge')}
    SM = {0: tri_mask(P, 'gt'), 1: tri_mask(P, 'lt')}
    qT = [P.sb([128, NTB], BF16) for _ in range(2)]
    kT = [P.sb([128, NTB], BF16) for _ in range(2)]
    kt = [P.sb([128, NTt, 128], BF16) for _ in range(2)]
    vt = [P.sb([128, NTt, 128], BF16) for _ in range(2)]
    for hh in range(2):
        P.op('sp', lambda e, hh=hh: e.dma_start(out=qT[hh][:], in_=fmT[hh * 2]), writes=[qT[hh]], dma=1)
        P.op('act', lambda e, hh=hh: e.dma_start(out=kT[hh][:], in_=fmT[hh * 2 + 1]), writes=[kT[hh]], dma=1)
        P.op('sp', lambda e, hh=hh: e.dma_start(out=kt[hh][:], in_=tokm[hh * 2].rearrange("(a p) d -> p a d", p=128)), writes=[kt[hh]], dma=1)
        P.op('act', lambda e, hh=hh: e.dma_start(out=vt[hh][:], in_=tokm[hh * 2 + 1].rearrange("(a p) d -> p a d", p=128)), writes=[vt[hh]], dma=1)
    gt = P.sb([128, NTt, 8], F32)
    for t in range(NTt):
        P.op('sp' if t % 2 == 0 else 'act', lambda e, t=t: e.dma_start(out=gt[:, t, :], in_=tm[t * 128:(t + 1) * 128, ZC:ZC + 8]), writes=[(gt, t)], dma=1)
    allg = [(gt, t) for t in range(NTt)]
    al = P.sb([128, 8], F32)
    db = P.sb([128, 8], F32)
    load_bc(P, 'sp', al, alog8)
    load_bc(P, 'sp', db, dtb8)
    P.op('act', lambda e: e.activation(out=al[:], in_=al[:], func=AF.Exp), reads=[al], writes=[al])
    P.op('dve', lambda e: e.tensor_scalar(out=al[:], in0=al[:], scalar1=negone1[:, 0:1], scalar2=zero1[:, 0:1], op0=ALU.mult, op1=ALU.add),
         reads=[al, negone1, zero1], writes=[al])
    beta, nbeta, gg = {}, {}, {}
    for hh in range(2):
        for d in range(2):
            cb, cg = hh * 4 + d, hh * 4 + 2 + d
            bt = P.sb([128, NTt], F32)
            nb = P.sb([128, NTt], F32)
            gx = P.sb([128, NTt], F32)
            P.op('act', lambda e, bt=bt, cb=cb: e.activation(out=bt[:], in_=gt[:, :, cb], func=AF.Sigmoid), reads=allg, writes=[bt])
            P.op('dve', lambda e, bt=bt, nb=nb: e.tensor_scalar(out=nb[:], in0=bt[:], scalar1=negone1[:, 0:1], scalar2=zero1[:, 0:1], op0=ALU.mult, op1=ALU.add),
                 reads=[bt, negone1, zero1], writes=[nb])
            P.op('dve', lambda e, gx=gx, cg=cg: e.tensor_scalar(out=gx[:], in0=gt[:, :, cg], scalar1=db[:, cg:cg + 1], scalar2=zero1[:, 0:1], op0=ALU.add, op1=ALU.add),
                 reads=allg + [db, zero1], writes=[gx])
            P.op('act', lambda e, gx=gx: e.activation(out=gx[:], in_=gx[:], func=AF.Exp), reads=[gx], writes=[gx])
            P.op('act', lambda e, gx=gx: e.activation(out=gx[:], in_=gx[:], func=AF.Ln, bias=one1[:, 0:1], scale=one1[:, 0:1]), reads=[gx, one1], writes=[gx])
            P.op('dve', lambda e, gx=gx, cg=cg: e.tensor_scalar(out=gx[:], in0=gx[:], scalar1=al[:, cg:cg + 1], scalar2=zero1[:, 0:1], op0=ALU.mult, op1=ALU.add),
                 reads=[gx, al, zero1], writes=[gx])
            beta[(hh, d)], nbeta[(hh, d)], gg[(hh, d)] = bt, nb, gx
    b_cs = P.ps([128, 512], F32)
    b_kk = P.ps([128, 512], F32)
    b_kq = P.ps([128, 512], F32)
    b_tr = P.ps([128, 512], F32)
    b_p = P.ps([128, 512], F32)
    b_pt = P.ps([128, 512], F32)
    b_s1 = P.ps([128, 512], F32)
    b_s2 = P.ps([128, 512], F32)
    o_acc = [P.sb([128, NTt, 128], F32) for _ in range(2)]
    written = set()
    seqs = [(hh, d) for hh in range(2) for d in range(2)]
    order = {0: list(range(NTt)), 1: [1, 0] + list(range(NTt - 1, 1, -1))}
    S32, Sbf = {}, {}
    for sq_ in seqs:
        S32[sq_] = P.sb([128, 128], F32)
        Sbf[sq_] = S32[sq_]
        P.op('pool', lambda e, sq_=sq_: e.memset(S32[sq_][:], 0.0), writes=[S32[sq_]])

    def ring(shape, dt):
        return {sq_: [P.sb(shape, dt) for _ in range(2)] for sq_ in seqs}
    gbc_r = ring([128, 128], F32)
    gcs_r = ring([128, 130], F32)
    F_r = ring([128, 128], F32)
    FM_r = ring([128, 256], F32)
    Nb_r = ring([128, 2, 128], F32)
    Nb2_r = ring([128, 2, 128], F32)
    QK_r = ring([128, 128], F32)
    R_r = ring([128, 2, 256], F32)
    sc_r = ring([128, 4], F32)
    eg_r = ring([128, 128], F32)
    qd_r = ring([128, 128], F32)
    kd_r = ring([128, 128], F32)
    wT_r = ring([128, 128], F32)
    vn_r = ring([128, 128], F32)

    def precompute(sq_, s):
        hh, d = sq_
        t = order[d][s]
        r = s % 2
        cs = slice(t * 128, (t + 1) * 128)
        gcol = gg[sq_][:, t:t + 1]
        gbc, gcs, Ft, FM, Nb, Nb2, QK, R, sc, eg, qd, kd, wT = (gbc_r[sq_][r], gcs_r[sq_][r], F_r[sq_][r], FM_r[sq_][r], Nb_r[sq_][r], Nb2_r[sq_][r],
                                                                QK_r[sq_][r], R_r[sq_][r], sc_r[sq_][r], eg_r[sq_][r], qd_r[sq_][r], kd_r[sq_][r], wT_r[sq_][r])
        lastc = 127 if d == 0 else 0
        P.op('dve', lambda e: e.tensor_scalar(out=gbc[:], in0=ones_f[:], scalar1=gcol, scalar2=zero1[:, 0:1], op0=ALU.mult, op1=ALU.add),
             reads=[ones_f, gg[sq_], zero1], writes=[gbc])
        P.op('pe', lambda e: e.matmul(b_cs[:, 0:128], lhsT=gbc[:], rhs=L[d][:], start=True, stop=True), reads=[gbc, L[d]], writes=[b_cs])
        P.op('pe', lambda e: e.matmul(b_cs[:, 128:129], lhsT=L[d][:], rhs=gcol, start=True, stop=True), reads=[gg[sq_], L[d]], writes=[b_cs])
        P.op('dve', lambda e: e.tensor_copy(out=gcs[:, 0:129], in_=b_cs[:, 0:129]), reads=[b_cs], writes=[gcs])
        P.op('dve', lambda e: e.tensor_scalar(out=Ft[:], in0=gcs[:, 0:128], scalar1=gcs[:, 128:129], scalar2=zero1[:, 0:1], op0=ALU.subtract, op1=ALU.add),
             reads=[gcs, zero1], writes=[Ft])
        P.op('act', lambda e: e.activation(out=Ft[:], in_=Ft[:], func=AF.Abs), reads=[Ft], writes=[Ft])
        P.op('act', lambda e: e.activation(out=Ft[:], in_=Ft[:], func=AF.Exp, scale=negone1[:, 0:1], bias=zero1[:, 0:1]), reads=[Ft, negone1, zero1], writes=[Ft])
        P.op('pool', lambda e: e.tensor_tensor(out=FM[:, 0:128], in0=Ft[:], in1=SM[d][:], op=ALU.mult), reads=[Ft, SM[d]], writes=[(FM, 0)])
        P.op('pool', lambda e: e.tensor_tensor(out=FM[:, 128:256], in0=Ft[:], in1=L[d][:], op=ALU.mult), reads=[Ft, L[d]], writes=[(FM, 1)])
        P.op('act', lambda e: e.activation(out=eg[:], in_=gcs[:, 0:128], func=AF.Exp), reads=[gcs], writes=[eg])
        P.op('act', lambda e: e.activation(out=sc[:, 0:1], in_=gcs[:, 128:129], func=AF.Exp), reads=[gcs], writes=[(sc, 0)])
        P.op('dve', lambda e: e.tensor_tensor(out=sc[:, 0:1], in0=sc[:, 0:1], in1=beta[sq_][:, t:t + 1], op=ALU.mult), reads=[(sc, 0), beta[sq_]], writes=[(sc, 0)])
        P.op('act', lambda e: e.activation(out=sc[:, 1:2], in_=gcs[:, 128:129], func=AF.Exp, scale=negone1[:, 0:1], bias=gcs[:, lastc:lastc + 1]),
             reads=[gcs, negone1], writes=[(sc, 1)])
        P.op('act', lambda e: e.activation(out=sc[:, 2:3], in_=gcs[:, lastc:lastc + 1], func=AF.Exp), reads=[gcs], writes=[(sc, 2)])
        P.op('pe', lambda e: e.matmul(b_kk[:, 0:128], lhsT=kT[hh][:, cs], rhs=kT[hh][:, cs], start=True, stop=True), reads=[kT[hh]], writes=[b_kk])
        P.op('pe', lambda e: e.matmul(b_kq[:, 0:128], lhsT=kT[hh][:, cs], rhs=qT[hh][:, cs], start=True, stop=True), reads=[kT[hh], qT[hh]], writes=[b_kq])
        P.op('dve', lambda e: e.scalar_tensor_tensor(out=Nb[:, 0, :], in0=b_kk[:, 0:128], scalar=nbeta[sq_][:, t:t + 1], in1=FM[:, 0:128], op0=ALU.mult, op1=ALU.mult),
             reads=[b_kk, nbeta[sq_], (FM, 0)], writes=[(Nb, 0)])
        P.op('dve', lambda e: e.tensor_tensor(out=QK[:], in0=b_kq[:, 0:128], in1=FM[:, 128:256], op=ALU.mult), reads=[b_kq, (FM, 1)], writes=[QK])
        P.op('pe', lambda e: e.transpose(out=b_tr[:, 0:128], in_=Nb[:, 0, :], identity=identf[:]), reads=[(Nb, 0), identf], writes=[b_tr])
        P.op('act', lambda e: e.copy(out=Nb[:, 1, :], in_=b_tr[:, 0:128]), reads=[b_tr], writes=[(Nb, 1)])
        P.op('pool', lambda e: e.tensor_scalar(out=R[:, 0, 0:128], in0=vt[hh][:, t, :], scalar1=beta[sq_][:, t:t + 1], scalar2=zero1[:, 0:1], op0=ALU.mult, op1=ALU.add),
             reads=[vt[hh], beta[sq_], zero1], writes=[(R, 0)])
        P.op('pool', lambda e: e.tensor_scalar(out=R[:, 0, 128:256], in0=kt[hh][:, t, :], scalar1=sc[:, 0:1], scalar2=zero1[:, 0:1], op0=ALU.mult, op1=ALU.add),
             reads=[kt[hh], (sc, 0), zero1], writes=[(R, 0)])
        cur, nxt = Nb, Nb2
        ri = 0
        for k in range(7):
            P.op('pe', lambda e, cur=cur, ri=ri: e.matmul(b_pt[:, 0:256], lhsT=cur[:, 1, :], rhs=R[:, ri, :], start=True, stop=True),
                 reads=[(cur, 1), (R, ri)], writes=[b_pt])
            P.op('dve', lambda e, ri=ri: e.tensor_tensor(out=R[:, 1 - ri, :], in0=b_pt[:, 0:256], in1=R[:, ri, :], op=ALU.add),
                 reads=[b_pt, (R, ri)], writes=[(R, 1 - ri)])
            ri = 1 - ri
            if k < 6:
                P.op('pe', lambda e, cur=cur: e.matmul(b_p[:, 0:128], lhsT=cur[:, 1, :], rhs=cur[:, 0, :], start=True, stop=True),
                     reads=[(cur, 0), (cur, 1)], writes=[b_p])
                P.op('pe', lambda e, cur=cur: e.matmul(b_p[:, 128:256], lhsT=cur[:, 0, :], rhs=cur[:, 1, :], start=True, stop=True),
                     reads=[(cur, 0), (cur, 1)], writes=[b_p])
                P.op('act', lambda e, nxt=nxt: e.copy(out=nxt[:, :, :], in_=b_p[:, 0:256].rearrange("p (a b) -> p a b", a=2)), reads=[b_p], writes=[(nxt, 0), (nxt, 1)])
                cur, nxt = nxt, cur
        P.op('pe', lambda e, ri=ri: e.transpose(out=b_tr[:, 128:256], in_=R[:, ri, 128:256], identity=identf[:]), reads=[(R, ri), identf], writes=[b_tr])
        P.op('act', lambda e: e.copy(out=wT[:], in_=b_tr[:, 128:256]), reads=[b_tr], writes=[wT])
        P.op('pool', lambda e: e.tensor_tensor(out=qd[:], in0=qT[hh][:, cs], in1=eg[:], op=ALU.mult), reads=[qT[hh], eg], writes=[qd])
        P.op('pool', lambda e: e.tensor_scalar(out=kd[:], in0=kt[hh][:, t, :], scalar1=sc[:, 1:2], scalar2=zero1[:, 0:1], op0=ALU.mult, op1=ALU.add),
             reads=[kt[hh], (sc, 1), zero1], writes=[kd])
        return ri

    def scan(sq_, s, ri):
        hh, d = sq_
        t = order[d][s]
        r = s % 2
        R, sc, qd, kd, wT, QK, vn = R_r[sq_][r], sc_r[sq_][r], qd_r[sq_][r], kd_r[sq_][r], wT_r[sq_][r], QK_r[sq_][r], vn_r[sq_][r]
        P.op('pe', lambda e: e.matmul(b_s1[:, 0:128], lhsT=wT[:], rhs=Sbf[sq_][:], start=True, stop=True), reads=[wT, Sbf[sq_]], writes=[b_s1])
        P.op('dve', lambda e: e.tensor_tensor(out=vn[:], in0=R[:, ri, 0:128], in1=b_s1[:, 0:128], op=ALU.subtract), reads=[(R, ri), b_s1], writes=[vn])
        P.op('pe', lambda e: e.matmul(b_s2[:, 0:128], lhsT=qd[:], rhs=Sbf[sq_][:], start=True, stop=False), reads=[qd, Sbf[sq_]], writes=[b_s2])
        P.op('pe', lambda e: e.matmul(b_s2[:, 0:128], lhsT=QK[:], rhs=vn[:], start=False, stop=True), reads=[QK, vn], writes=[b_s2])
        key = (hh, t)
        if key not in written:
            written.add(key)
            P.op('act', lambda e: e.copy(out=o_acc[hh][:, t, :], in_=b_s2[:, 0:128]), reads=[b_s2], writes=[(o_acc[hh], t)])
        else:
            P.op('dve', lambda e: e.tensor_tensor(out=o_acc[hh][:, t, :], in0=b_s2[:, 0:128], in1=o_acc[hh][:, t, :], op=ALU.add),
                 reads=[b_s2, (o_acc[hh], t)], writes=[(o_acc[hh], t)])
        P.op('pe', lambda e: e.matmul(b_s1[:, 128:256], lhsT=kd[:], rhs=vn[:], start=True, stop=True), reads=[kd, vn], writes=[b_s1])
        P.op('dve', lambda e: e.scalar_tensor_tensor(out=S32[sq_][:], in0=S32[sq_][:], scalar=sc[:, 2:3], in1=b_s1[:, 128:256], op0=ALU.mult, op1=ALU.add),
             reads=[S32[sq_], (sc, 2), b_s1], writes=[S32[sq_]])

    ris = {}
    for sq_ in seqs:
        ris[(sq_, 0)] = precompute(sq_, 0)
    for s in range(NTt):
        for sq_ in seqs:
            if s + 1 < NTt:
                ris[(sq_, s + 1)] = precompute(sq_, s + 1)
            scan(sq_, s, ris[(sq_, s)])
    gb = P.sb([128, 128], F32)
    load_bc(P, 'sp', gb, dng)
    zt = [P.sb([128, 256], F32) for _ in range(2)]
    ot = [P.sb([128, 256], F32) for _ in range(2)]
    junk = P.sb([128, 128], F32)
    ss = [P.sb([128, 1], F32) for _ in range(4)]
    rs = [P.sb([128, 1], F32) for _ in range(4)]
    for t in range(NTt):
        b = t % 2
        P.op('sp', lambda e, t=t, b=b: e.dma_start(out=zt[b][:], in_=tm[t * 128:(t + 1) * 128, 0:256]), writes=[zt[b]], dma=1)
        P.op('act', lambda e, b=b: e.activation(out=zt[b][:], in_=zt[b][:], func=AF.Silu), reads=[zt[b]], writes=[zt[b]])
        for hh in range(2):
            k = b * 2 + hh
            P.op('act', lambda e, hh=hh, t=t, k=k: e.activation(out=junk[:], in_=o_acc[hh][:, t, :], func=AF.Square, accum_out=ss[k][:]),
                 reads=[(o_acc[hh], t)], writes=[junk, ss[k]])
            rstd_from_ss(P, ss[k], rs[k], 128)
            P.op('dve', lambda e, hh=hh, t=t, k=k, b=b: e.scalar_tensor_tensor(out=ot[b][:, hh * 128:(hh + 1) * 128], in0=o_acc[hh][:, t, :], scalar=rs[k][:, 0:1],
                                                                             in1=gb[:], op0=ALU.mult, op1=ALU.mult), reads=[(o_acc[hh], t), rs[k], gb], writes=[(ot[b], hh)])
            P.op('pool', lambda e, hh=hh, b=b: e.tensor_tensor(out=ot[b][:, hh * 128:(hh + 1) * 128], in0=ot[b][:, hh * 128:(hh + 1) * 128],
                                                              in1=zt[b][:, hh * 128:(hh + 1) * 128], op=ALU.mult), reads=[(ot[b], hh), zt[b]], writes=[(ot[b], hh)])
        P.op('sp', lambda e, t=t, b=b: e.dma_start(out=dnout[t * 128:(t + 1) * 128, :], in_=ot[b][:]), reads=[(ot[b], 0), (ot[b], 1)], writes=['dnout'], dma=1)
    P.end()


def stage_mla_proj(P, fm, qg, kvg, wq, wkv, cosT, ssT, qkT, krT, vtok):
    P.begin()
    ident = make_ident(P)
    pcl = PCLoader(P, ident, nchunk=6)
    g6 = P.sb([128, 6], F32)
    g4 = P.sb([128, 6], F32)
    pcl.load('sp', g6, qg, n=6)
    pcl.load('sp', g4, kvg, n=4)
    ones_b = P.sb([128, 128], BF16)
    P.op('pool', lambda e: e.memset(ones_b[:], 1.0), writes=[ones_b])
    wqs = P.sb([128, 6, 512], BF16)
    wkvs = P.sb([128, 4, 512], BF16)
    P.op('pool', lambda e: e.dma_start(out=wqs[:], in_=wq.rearrange("(c p) n -> p c n", p=128)), writes=[wqs], dma=1)
    P.op('pool', lambda e: e.dma_start(out=wkvs[:], in_=wkv.rearrange("(c p) n -> p c n", p=128)), writes=[wkvs], dma=1)
    cqn = P.sb([128, 6, NTB], BF16)
    ckn = P.sb([128, 4, NTB], BF16)
    xin = [P.sb([128, 6, 512], F32) for _ in range(2)]
    sq = [P.sb([128, 6, 512], BF16) for _ in range(2)]
    rn = [P.sb([128, 512], F32) for _ in range(2)]
    pss = [P.ps([128, 512], F32) for _ in range(2)]
    nb = 0
    NBLK = (NTB + 511) // 512
    for (base, nch, dst, gcol, dim) in ((0, 6, cqn, g6, 768.0), (6, 4, ckn, g4, 512.0)):
        for bi in range(NBLK):
            lo = bi * 512
            w = min(512, NTB - lo)
            k = nb % 2
            nb += 1
            P.op('sp', lambda e, k=k, base=base, nch=nch, lo=lo, w=w: e.dma_start(out=xin[k][:, 0:nch, 0:w], in_=fm[base:base + nch, :, lo:lo + w].rearrange("c p t -> p c t")),
                 writes=[xin[k]], dma=1)
            P.op('act', lambda e, k=k, nch=nch, w=w: e.activation(out=sq[k][:, 0:nch, 0:w], in_=xin[k][:, 0:nch, 0:w], func=AF.Square), reads=[xin[k]], writes=[sq[k]])
            for c in range(nch):
                P.op('pe', lambda e, k=k, c=c, w=w, nch=nch: e.matmul(pss[k][:, 0:w], lhsT=ones_b[:], rhs=sq[k][:, c, 0:w], start=(c == 0), stop=(c == nch - 1)),
                     reads=[ones_b, sq[k]], writes=[pss[k]])
            P.op('dve', lambda e, k=k, w=w, dim=dim: e.tensor_scalar(out=rn[k][:, 0:w], in0=pss[k][:, 0:w], scalar1=1.0 / dim, scalar2=EPS, op0=ALU.mult, op1=ALU.add),
                 reads=[pss[k]], writes=[rn[k]])
            P.op('act', lambda e, k=k, w=w: e.activation(out=rn[k][:, 0:w], in_=rn[k][:, 0:w], func=AF.Sqrt), reads=[rn[k]], writes=[rn[k]])
            P.op('dve', lambda e, k=k, w=w: e.reciprocal(out=rn[k][:, 0:w], in_=rn[k][:, 0:w]), reads=[rn[k]], writes=[rn[k]])
            for c in range(nch):
                P.op('dve', lambda e, k=k, c=c, w=w, lo=lo, dst=dst, gcol=gcol: e.scalar_tensor_tensor(
                    out=dst[:, c, lo:lo + w], in0=xin[k][:, c, 0:w], scalar=gcol[:, c:c + 1], in1=rn[k][:, 0:w], op0=ALU.mult, op1=ALU.mult),
                    reads=[xin[k], gcol, rn[k]], writes=[(dst, bi)])
    cs = P.sb([64, NTB], F32)
    sn = P.sb([64, NTB], F32)
    P.op('sp', lambda e: e.dma_start(out=cs[:], in_=cosT), writes=[cs], dma=1)
    P.op('act', lambda e: e.dma_start(out=sn[:], in_=ssT), writes=[sn], dma=1)
    pj = [P.ps([128, 512], F32) for _ in range(4)]
    ob = [P.sb([128, 512], BF16) for _ in range(4)]
    t1 = [P.sb([64, 512], F32) for _ in range(2)]
    t2 = [P.sb([64, 512], F32) for _ in range(2)]
    krs = [P.sb([64, 2, 512], F32) for _ in range(2)]
    n = 0
    nr = 0
    for bi in range(NBLK):
        lo = bi * 512
        w = min(512, NTB - lo)
        kk = bi % 2
        P.op('sp', lambda e, kk=kk, lo=lo, w=w: e.dma_start(out=krs[kk][:, :, 0:w], in_=fm[10:12, 0:64, lo:lo + w].rearrange("c p t -> p c t")), writes=[krs[kk]], dma=1)
        r = nr % 2
        nr += 1
        P.op('dve', lambda e, kk=kk, r=r, lo=lo, w=w: e.tensor_tensor(out=t1[r][:, 0:w], in0=krs[kk][:, 0, 0:w], in1=cs[:, lo:lo + w], op=ALU.mult), reads=[krs[kk], cs], writes=[t1[r]])
        P.op('pool', lambda e, kk=kk, r=r, lo=lo, w=w: e.tensor_tensor(out=t2[r][:, 0:w], in0=krs[kk][:, 1, 0:w], in1=sn[:, lo:lo + w], op=ALU.mult), reads=[krs[kk], sn], writes=[t2[r]])
        k = n % 4
        n += 1
        P.op('dve', lambda e, k=k, r=r, w=w: e.tensor_tensor(out=ob[k][0:64, 0:w], in0=t1[r][:, 0:w], in1=t2[r][:, 0:w], op=ALU.add), reads=[t1[r], t2[r]], writes=[ob[k]])
        P.op('sp', lambda e, k=k, lo=lo, w=w: e.dma_start(out=krT[:, lo:lo + w], in_=ob[k][0:64, 0:w]), reads=[ob[k]], writes=['krT'], dma=1)
        for hh in range(2):
            k = n % 4
            n += 1
            for c in range(6):
                P.op('pe', lambda e, k=k, c=c, hh=hh, lo=lo, w=w: e.matmul(pj[k][:, 0:w], lhsT=wqs[:, c, hh * 128:(hh + 1) * 128], rhs=cqn[:, c, lo:lo + w], start=(c == 0), stop=(c == 5)),
                     reads=[wqs, (cqn, bi)], writes=[pj[k]])
            P.op('act', lambda e, k=k, w=w: e.copy(out=ob[k][:, 0:w], in_=pj[k][:, 0:w]), reads=[pj[k]], writes=[ob[k]])
            P.op('sp', lambda e, k=k, hh=hh, lo=lo, w=w: e.dma_start(out=qkT[hh, 0, :, lo:lo + w], in_=ob[k][:, 0:w]), reads=[ob[k]], writes=['qkT'], dma=1)
            k1 = n % 4
            n += 1
            k2 = n % 4
            n += 1
            for c in range(6):
                P.op('pe', lambda e, k1=k1, c=c, hh=hh, lo=lo, w=w: e.matmul(pj[k1][0:64, 0:w], lhsT=wqs[:, c, 256 + hh * 64:256 + (hh + 1) * 64], rhs=cqn[:, c, lo:lo + w], start=(c == 0), stop=(c == 5)),
                     reads=[wqs, (cqn, bi)], writes=[pj[k1]])
            for c in range(6):
                P.op('pe', lambda e, k2=k2, c=c, hh=hh, lo=lo, w=w: e.matmul(pj[k2][0:64, 0:w], lhsT=wqs[:, c, 384 + hh * 64:384 + (hh + 1) * 64], rhs=cqn[:, c, lo:lo + w], start=(c == 0), stop=(c == 5)),
                     reads=[wqs, (cqn, bi)], writes=[pj[k2]])
            r = nr % 2
            nr += 1
            P.op('dve', lambda e, k1=k1, r=r, lo=lo, w=w: e.tensor_tensor(out=t1[r][:, 0:w], in0=pj[k1][0:64, 0:w], in1=cs[:, lo:lo + w], op=ALU.mult), reads=[pj[k1], cs], writes=[t1[r]])
            P.op('dve', lambda e, k2=k2, r=r, lo=lo, w=w: e.tensor_tensor(out=t2[r][:, 0:w], in0=pj[k2][0:64, 0:w], in1=sn[:, lo:lo + w], op=ALU.mult), reads=[pj[k2], sn], writes=[t2[r]])
            P.op('pool', lambda e, k1=k1, r=r, w=w: e.tensor_tensor(out=ob[k1][0:64, 0:w], in0=t1[r][:, 0:w], in1=t2[r][:, 0:w], op=ALU.add), reads=[t1[r], t2[r]], writes=[ob[k1]])
            P.op('sp', lambda e, k1=k1, hh=hh, lo=lo, w=w: e.dma_start(out=qkT[hh, 1, 0:64, lo:lo + w], in_=ob[k1][0:64, 0:w]), reads=[ob[k1]], writes=['qkT'], dma=1)
            k = n % 4
            n += 1
            for c in range(4):
                P.op('pe', lambda e, k=k, c=c, hh=hh, lo=lo, w=w: e.matmul(pj[k][:, 0:w], lhsT=wkvs[:, c, hh * 128:(hh + 1) * 128], rhs=ckn[:, c, lo:lo + w], start=(c == 0), stop=(c == 3)),
                     reads=[wkvs, (ckn, bi)], writes=[pj[k]])
            P.op('act', lambda e, k=k, w=w: e.copy(out=ob[k][:, 0:w], in_=pj[k][:, 0:w]), reads=[pj[k]], writes=[ob[k]])
            P.op('sp', lambda e, k=k, hh=hh, lo=lo, w=w: e.dma_start(out=qkT[hh, 2, :, lo:lo + w], in_=ob[k][:, 0:w]), reads=[ob[k]], writes=['qkT'], dma=1)
            for ti in range(w // 128):
                k = n % 4
                n += 1
                for c in range(4):
                    P.op('pe', lambda e, k=k, c=c, hh=hh, lo=lo, ti=ti: e.matmul(pj[k][:, 0:128], lhsT=ckn[:, c, lo + ti * 128:lo + (ti + 1) * 128], rhs=wkvs[:, c, 256 + hh * 128:256 + (hh + 1) * 128],
                                                                                start=(c == 0), stop=(c == 3)), reads=[wkvs, (ckn, bi)], writes=[pj[k]])
                P.op('dve', lambda e, k=k: e.tensor_copy(out=ob[k][:, 0:128], in_=pj[k][:, 0:128]), reads=[pj[k]], writes=[ob[k]])
                P.op('act', lambda e, k=k, hh=hh, lo=lo, ti=ti: e.dma_start(out=vtok[hh, lo + ti * 128:lo + (ti + 1) * 128, :], in_=ob[k][:, 0:128]), reads=[ob[k]], writes=['vtok'], dma=1)
    P.end()


def stage_mla_attn(P, qkT, krT, vtok, oT, scale):
    P.begin()
    ones_b = P.sb([128, 128], BF16)
    P.op('pool', lambda e: e.memset(ones_b[:], 1.0), writes=[ones_b])
    kr = P.sb([64, NTB], BF16)
    P.op('sp', lambda e: e.dma_start(out=kr[:], in_=krT), writes=[kr], dma=1)
    ps_s = [P.ps([128, 512], F32) for _ in range(3)]
    ps_o = [P.ps([128, 512], F32) for _ in range(2)]
    ps_d = [P.ps([128, 512], F32) for _ in range(2)]
    pt = [P.sb([128, 512], BF16) for _ in range(3)]
    rinv = [P.sb([128, 512], F32) for _ in range(2)]
    osb = [P.sb([128, 512], BF16) for _ in range(2)]
    n = 0
    nq = 0
    for hh in range(2):
        qn = P.sb([128, NTB], BF16)
        qr = P.sb([64, NTB], BF16)
        kn = P.sb([128, NTB], BF16)
        vt = P.sb([128, NTB_T, 128], BF16)
        P.op('sp', lambda e, hh=hh, qn=qn: e.dma_start(out=qn[:], in_=qkT[hh, 0]), writes=[qn], dma=1)
        P.op('act', lambda e, hh=hh, qr=qr: e.dma_start(out=qr[:], in_=qkT[hh, 1, 0:64, :]), writes=[qr], dma=1)
        P.op('sp', lambda e, hh=hh, kn=kn: e.dma_start(out=kn[:], in_=qkT[hh, 2]), writes=[kn], dma=1)
        P.op('act', lambda e, hh=hh, vt=vt: e.dma_start(out=vt[:], in_=vtok[hh].rearrange("(a p) d -> p a d", p=128)), writes=[vt], dma=1)
        qblocks = [(0, 256, 2)] + [(256 + i * 512, 512, NTB_T) for i in range(8)]
        for (qlo, qw, nkt) in qblocks:
            a = nq % 2
            nq += 1
            for kt in range(nkt):
                k = n % 3
                n += 1
                ks = slice(kt * 128, (kt + 1) * 128)
                P.op('pe', lambda e, k=k, ks=ks, qlo=qlo, qw=qw, kn=kn, qn=qn: e.matmul(ps_s[k][:, 0:qw], lhsT=kn[:, ks], rhs=qn[:, qlo:qlo + qw], start=True, stop=False),
                     reads=[kn, qn], writes=[ps_s[k]])
                P.op('pe', lambda e, k=k, ks=ks, qlo=qlo, qw=qw, qr=qr: e.matmul(ps_s[k][:, 0:qw], lhsT=kr[:, ks], rhs=qr[:, qlo:qlo + qw], start=False, stop=True),
                     reads=[kr, qr], writes=[ps_s[k]])
                P.op('act', lambda e, k=k, qw=qw: e.activation(out=pt[k][:, 0:qw], in_=ps_s[k][:, 0:qw], func=AF.Exp, scale=float(scale)), reads=[ps_s[k]], writes=[pt[k]])
                P.op('pe', lambda e, k=k, a=a, kt=kt, qw=qw, nkt=nkt, vt=vt: e.matmul(ps_o[a][:, 0:qw], lhsT=vt[:, kt, :], rhs=pt[k][:, 0:qw], start=(kt == 0), stop=(kt == nkt - 1)),
                     reads=[vt, pt[k]], writes=[ps_o[a]])
                P.op('pe', lambda e, k=k, a=a, kt=kt, qw=qw, nkt=nkt: e.matmul(ps_d[a][:, 0:qw], lhsT=ones_b[:], rhs=pt[k][:, 0:qw], start=(kt == 0), stop=(kt == nkt - 1)),
                     reads=[ones_b, pt[k]], writes=[ps_d[a]])
            P.op('dve', lambda e, a=a, qw=qw: e.reciprocal(out=rinv[a][:, 0:qw], in_=ps_d[a][:, 0:qw]), reads=[ps_d[a]], writes=[rinv[a]])
            P.op('dve', lambda e, a=a, qw=qw: e.tensor_tensor(out=osb[a][:, 0:qw], in0=ps_o[a][:, 0:qw], in1=rinv[a][:, 0:qw], op=ALU.mult), reads=[ps_o[a], rinv[a]], writes=[osb[a]])
            P.op('sp', lambda e, a=a, hh=hh, qlo=qlo, qw=qw: e.dma_start(out=oT[hh, :, qlo:qlo + qw], in_=osb[a][:, 0:qw]), reads=[osb[a]], writes=['oT'], dma=1)
    P.end()


NT1 = 1152
NKEXT = 256 + 1280


def stage_rope(P, fm, cosT, ssT, out, npair, ncols):
    P.begin()
    cs = P.sb([128, ncols], F32)
    sn = P.sb([128, ncols], F32)
    P.op('sp', lambda e: e.dma_start(out=cs[:], in_=cosT), writes=[cs], dma=1)
    P.op('act', lambda e: e.dma_start(out=sn[:], in_=ssT), writes=[sn], dma=1)
    a = [P.sb([128, ncols], F32) for _ in range(2)]
    b = [P.sb([128, ncols], F32) for _ in range(2)]
    o = [P.sb([128, ncols], BF16) for _ in range(2)]
    for i in range(npair):
        k = i % 2
        P.op('sp', lambda e, i=i, k=k: e.dma_start(out=a[k][:], in_=fm[i]), writes=[a[k]], dma=1)
        P.op('act', lambda e, i=i, k=k: e.dma_start(out=b[k][:], in_=fm[npair + i]), writes=[b[k]], dma=1)
        P.op('dve', lambda e, k=k: e.tensor_tensor(out=a[k][:], in0=a[k][:], in1=cs[:], op=ALU.mult), reads=[a[k], cs], writes=[a[k]])
        P.op('pool', lambda e, k=k: e.tensor_tensor(out=b[k][:], in0=b[k][:], in1=sn[:], op=ALU.mult), reads=[b[k], sn], writes=[b[k]])
        P.op('dve', lambda e, k=k: e.tensor_tensor(out=o[k][:], in0=a[k][:], in1=b[k][:], op=ALU.add), reads=[a[k], b[k]], writes=[o[k]])
        P.op('sp', lambda e, i=i, k=k: e.dma_start(out=out[i], in_=o[k][:]), reads=[o[k]], writes=['ropeout'], dma=1)
    P.end()


def stage_gqa_attn(P, qT, kTd, vd, sink, lrv, attnT, scale):
    P.begin()
    ones_b = P.sb([128, 128], BF16)
    P.op('pool', lambda e: e.memset(ones_b[:], 1.0), writes=[ones_b])
    zero1 = P.sb([128, 1], F32)
    P.op('pool', lambda e: e.memset(zero1[:], 0.0), writes=[zero1])
    mge = tri_mask(P, 'ge')
    mle = tri_mask(P, 'le')
    lr = P.sb([128, 2], F32)
    load_bc(P, 'sp', lr, lrv)
    es = P.sb([128, 32], F32)
    load_bc(P, 'sp', es, sink)
    P.op('act', lambda e: e.activation(out=es[:], in_=es[:], func=AF.Exp), reads=[es], writes=[es])
    m8 = [P.sb([128, 8, 128], BF16) for _ in range(4)]
    for hl in range(8):
        P.op('dve', lambda e, hl=hl: e.tensor_copy(out=m8[0][:, hl, :], in_=mge[:]), reads=[mge], writes=[(m8[0], hl)])
        P.op('dve', lambda e, hl=hl: e.tensor_copy(out=m8[1][:, hl, :], in_=mle[:]), reads=[mle], writes=[(m8[1], hl)])
        P.op('dve', lambda e, hl=hl: e.tensor_scalar(out=m8[2][:, hl, :], in0=mge[:], scalar1=lr[:, 0:1], scalar2=zero1[:, 0:1], op0=ALU.mult, op1=ALU.add),
             reads=[mge, lr, zero1], writes=[(m8[2], hl)])
        P.op('dve', lambda e, hl=hl: e.tensor_scalar(out=m8[3][:, hl, :], in0=mle[:], scalar1=lr[:, 1:2], scalar2=zero1[:, 0:1], op0=ALU.mult, op1=ALU.add),
             reads=[mle, lr, zero1], writes=[(m8[3], hl)])
    m8k = [[(m8[i], hl) for hl in range(8)] for i in range(4)]
    aT = P.sb([128, 16, 1024], BF16)
    ps_s = [[P.ps([128, 512], F32) for _ in range(2)] for _ in range(2)]
    ps_o = [P.ps([128, 512], F32) for _ in range(2)]
    ps_d = [P.ps([128, 512], F32) for _ in range(2)]
    pt = [P.sb([128, 8, 128], BF16) for _ in range(2)]
    den = P.sb([128, 8, 128], F32)
    n = 0
    for g in range(4):
        kT = P.sb([128, 2, NKEXT], BF16)
        vv = P.sb([128, NKEXT // 128, 128], BF16)
        q4 = P.sb([128, 4, 1024], BF16)
        P.op('sp', lambda e, g=g, kT=kT: e.dma_start(out=kT[:], in_=kTd[g].rearrange("v p t -> p v t")), writes=[kT], dma=1)
        P.op('act', lambda e, g=g, vv=vv: e.dma_start(out=vv[:], in_=vd[g].rearrange("(a p) d -> p a d", p=128)), writes=[vv], dma=1)
        P.op('sp', lambda e, g=g, q4=q4: e.dma_start(out=q4[:], in_=qT[4 * g:4 * g + 4, :, 0:1024].rearrange("c p t -> p c t")), writes=[q4], dma=1)
        for qb in range(8):
            qs = slice(qb * 128, (qb + 1) * 128)
            ktiles = [(0, None), (1, None), (2 + qb, 2 if qb == 0 else 0), (3 + qb, None), (4 + qb, 3 if qb == 7 else 1)]
            for ki, (kt, mi) in enumerate(ktiles):
                k = n % 2
                n += 1
                ks = slice(kt * 128, (kt + 1) * 128)
                for hl in range(8):
                    P.op('pe', lambda e, k=k, ks=ks, hl=hl, qs=qs, kT=kT, q4=q4: e.matmul(ps_s[k][hl // 4][:, (hl % 4) * 128:(hl % 4 + 1) * 128], lhsT=kT[:, hl % 2, ks],
                                                                                rhs=q4[:, hl // 2, qs], start=True, stop=True),
                         reads=[kT, q4], writes=[ps_s[k][hl // 4]])
                for half in range(2):
                    P.op('act', lambda e, k=k, half=half: e.activation(out=pt[k][:, half * 4:(half + 1) * 4, :], in_=ps_s[k][half][:].rearrange("p (a b) -> p a b", a=4),
                                                                     func=AF.Exp, scale=float(scale)), reads=[ps_s[k][half]], writes=[(pt[k], half)])
                if mi is not None:
                    P.op('pool', lambda e, k=k, mi=mi: e.tensor_tensor(out=pt[k][:], in0=pt[k][:], in1=m8[mi][:], op=ALU.mult), reads=[(pt[k], 0), (pt[k], 1)] + m8k[mi], writes=[(pt[k], 0), (pt[k], 1)])
                for half in range(2):
                    P.op('pe', lambda e, k=k, kt=kt, ki=ki, half=half, vv=vv: e.matmul(ps_o[half][:], lhsT=vv[:, kt, :],
                                                                                 rhs=pt[k][:, half * 4:(half + 1) * 4, :], start=(ki == 0), stop=(ki == 4)),
                         reads=[vv, (pt[k], half)], writes=[ps_o[half]])
                    P.op('pe', lambda e, k=k, ki=ki, half=half: e.matmul(ps_d[half][:], lhsT=ones_b[:],
                                                                       rhs=pt[k][:, half * 4:(half + 1) * 4, :], start=(ki == 0), stop=(ki == 4)),
                         reads=[ones_b, (pt[k], half)], writes=[ps_d[half]])
            for hl in range(8):
                h = 8 * g + hl
                P.op('dve', lambda e, hl=hl, h=h: e.tensor_scalar(out=den[:, hl, :], in0=ps_d[hl // 4][:, (hl % 4) * 128:(hl % 4 + 1) * 128], scalar1=es[:, h:h + 1], scalar2=zero1[:, 0:1],
                                                                  op0=ALU.add, op1=ALU.add), reads=[ps_d[hl // 4], es, zero1], writes=[(den, hl)])
            P.op('dve', lambda e: e.reciprocal(out=den[:], in_=den[:]), reads=[(den, hl) for hl in range(8)], writes=[(den, hl) for hl in range(8)])
            for hl in range(8):
                r0 = (hl % 2) * 64
                m = 4 * g + hl // 2
                P.op('dve', lambda e, hl=hl, r0=r0, m=m, qs=qs: e.tensor_tensor(out=aT[r0:r0 + 64, m, qs], in0=ps_o[hl // 4][r0:r0 + 64, (hl % 4) * 128:(hl % 4 + 1) * 128],
                                                                              in1=den[r0:r0 + 64, hl, :], op=ALU.mult), reads=[ps_o[hl // 4], (den, hl)], writes=[(aT, m, qb)])
    allk = [(aT, m, qb) for m in range(16) for qb in range(8)]
    for c4 in range(4):
        P.op('sp', lambda e, c4=c4: e.dma_start(out=attnT[:, c4 * 4:(c4 + 1) * 4, 0:1024], in_=aT[:, c4 * 4:(c4 + 1) * 4, :]), reads=allk, writes=['attnT'], dma=1)
    P.end()


def stage_router(P, uT32, wr, sel, wgt, ntiles):
    P.begin()
    NT = ntiles * 128
    ws = P.sb([128, 16, 8], F32)
    P.op('sp', lambda e: e.dma_start(out=ws[:], in_=wr.rearrange("(c p) n -> p c n", p=128)), writes=[ws], dma=1)
    zero1 = P.sb([128, 1], F32)
    one1 = P.sb([128, 1], F32)
    negone1 = P.sb([128, 1], F32)
    negbig1 = P.sb([128, 1], F32)
    P.op('pool', lambda e: e.memset(zero1[:], 0.0), writes=[zero1])
    P.op('pool', lambda e: e.memset(one1[:], 1.0), writes=[one1])
    P.op('pool', lambda e: e.memset(negone1[:], -1.0), writes=[negone1])
    P.op('pool', lambda e: e.memset(negbig1[:], -1e30), writes=[negbig1])
    ut = [P.sb([128, 16, 128], F32) for _ in range(2)]
    pl = [P.ps([128, 512], F32) for _ in range(2)]
    for t in range(ntiles):
        b = t % 2
        lg = P.sb([128, 8], F32, name=f"lg{b}") if t < 2 else lg_[b]
        if t < 2:
            if t == 0:
                lg_ = {}
                tmp_ = {}
            lg_[b] = lg
            tmp_[b] = [P.sb([128, 8], F32) for _ in range(3)] + [P.sb([128, 1], F32) for _ in range(4)]
        e8, l2, so, m1, m2, nm1, dn = tmp_[b]
        P.op('sp', lambda e, t=t, b=b: e.dma_start(out=ut[b][:], in_=uT32[:, :, t * 128:(t + 1) * 128]), writes=[ut[b]], dma=1)
        for c in range(16):
            P.op('pe', lambda e, b=b, c=c: e.matmul(pl[b][:, 0:8], lhsT=ut[b][:, c, :], rhs=ws[:, c, :], start=(c == 0), stop=(c == 15)), reads=[ut[b], ws], writes=[pl[b]])
        P.op('dve', lambda e, b=b, lg=lg: e.tensor_copy(out=lg[:], in_=pl[b][:, 0:8]), reads=[pl[b]], writes=[lg])
        P.op('dve', lambda e, lg=lg, m1=m1: e.reduce_max(out=m1[:], in_=lg[:], axis=AX.X), reads=[lg], writes=[m1])
        P.op('dve', lambda e, lg=lg, m1=m1, l2=l2: e.tensor_scalar(out=l2[:], in0=lg[:], scalar1=m1[:, 0:1], scalar2=negbig1[:, 0:1], op0=ALU.is_ge, op1=ALU.mult),
             reads=[lg, m1, negbig1], writes=[l2])
        P.op('dve', lambda e, lg=lg, l2=l2: e.tensor_tensor(out=l2[:], in0=l2[:], in1=lg[:], op=ALU.add), reads=[l2, lg], writes=[l2])
        P.op('dve', lambda e, l2=l2, m2=m2: e.reduce_max(out=m2[:], in_=l2[:], axis=AX.X), reads=[l2], writes=[m2])
        P.op('dve', lambda e, lg=lg, m2=m2, so=so: e.tensor_scalar(out=so[:], in0=lg[:], scalar1=m2[:, 0:1], scalar2=zero1[:, 0:1], op0=ALU.is_ge, op1=ALU.add),
             reads=[lg, m2, zero1], writes=[so])
        P.op('dve', lambda e, m1=m1, nm1=nm1: e.tensor_scalar(out=nm1[:], in0=m1[:], scalar1=negone1[:, 0:1], scalar2=zero1[:, 0:1], op0=ALU.mult, op1=ALU.add),
             reads=[m1, negone1, zero1], writes=[nm1])
        P.op('act', lambda e, lg=lg, nm1=nm1, e8=e8: e.activation(out=e8[:], in_=lg[:], func=AF.Exp, bias=nm1[:, 0:1], scale=one1[:, 0:1]), reads=[lg, nm1, one1], writes=[e8])
        P.op('act', lambda e, m2=m2, nm1=nm1, dn=dn: e.activation(out=dn[:], in_=m2[:], func=AF.Exp, bias=nm1[:, 0:1], scale=one1[:, 0:1]), reads=[m2, nm1, one1], writes=[dn])
        P.op('dve', lambda e, dn=dn: e.tensor_scalar(out=dn[:], in0=dn[:], scalar1=one1[:, 0:1], scalar2=zero1[:, 0:1], op0=ALU.add, op1=ALU.add), reads=[dn, one1, zero1], writes=[dn])
        P.op('dve', lambda e, dn=dn: e.reciprocal(out=dn[:], in_=dn[:]), reads=[dn], writes=[dn])
        P.op('dve', lambda e, e8=e8, so=so: e.tensor_tensor(out=e8[:], in0=e8[:], in1=so[:], op=ALU.mult), reads=[e8, so], writes=[e8])
        P.op('dve', lambda e, e8=e8, dn=dn: e.tensor_scalar(out=e8[:], in0=e8[:], scalar1=dn[:, 0:1], scalar2=zero1[:, 0:1], op0=ALU.mult, op1=ALU.add), reads=[e8, dn, zero1], writes=[e8])
        P.op('sp', lambda e, t=t, so=so: e.dma_start(out=sel[t * 128:(t + 1) * 128, :], in_=so[:]), reads=[so], writes=['sel'], dma=1)
        P.op('sp', lambda e, t=t, e8=e8: e.dma_start(out=wgt[t * 128:(t + 1) * 128, :], in_=e8[:]), reads=[e8], writes=['wgt'], dma=1)
    P.end()


def stage_combine(P, y1, y2, w12, ysum, ntiles):
    P.begin()
    zero1 = P.sb([128, 1], F32)
    P.op('pool', lambda e: e.memset(zero1[:], 0.0), writes=[zero1])
    a = [P.sb([128, D], F32) for _ in range(2)]
    b = [P.sb([128, D], F32) for _ in range(2)]
    w = [P.sb([128, 2], F32) for _ in range(2)]
    for t in range(ntiles):
        k = t % 2
        rs = slice(t * 128, (t + 1) * 128)
        P.op('sp', lambda e, k=k, rs=rs: e.dma_start(out=a[k][:], in_=y1[rs, :]), writes=[a[k]], dma=1)
        P.op('act', lambda e, k=k, rs=rs: e.dma_start(out=b[k][:], in_=y2[rs, :]), writes=[b[k]], dma=1)
        P.op('sp', lambda e, k=k, rs=rs: e.dma_start(out=w[k][:], in_=w12[rs, :]), writes=[w[k]], dma=1)
        P.op('pool', lambda e, k=k: e.tensor_scalar(out=a[k][:], in0=a[k][:], scalar1=w[k][:, 0:1], scalar2=zero1[:, 0:1], op0=ALU.mult, op1=ALU.add),
             reads=[a[k], w[k], zero1], writes=[a[k]])
        P.op('dve', lambda e, k=k: e.scalar_tensor_tensor(out=b[k][:], in0=b[k][:], scalar=w[k][:, 1:2], in1=a[k][:], op0=ALU.mult, op1=ALU.add),
             reads=[a[k], b[k], w[k]], writes=[b[k]])
        P.op('sp', lambda e, k=k, rs=rs: e.dma_start(out=ysum[rs, :], in_=b[k][:]), reads=[b[k]], writes=['ysum'], dma=1)
    P.end()


def stage_transpose(P, src, dst, ntiles, nch):
    P.begin()
    NT = ntiles * 128
    ident = make_ident(P)
    xt = [P.sb([128, nch * 128], F32) for _ in range(2)]
    ot = P.sb([128, nch, NT], BF16)
    pst = [P.ps([128, 512], F32) for _ in range(4)]
    n = 0
    for t in range(ntiles):
        b = t % 2
        P.op('sp', lambda e, t=t, b=b: e.dma_start(out=xt[b][:], in_=src[t * 128:(t + 1) * 128, :]), writes=[xt[b]], dma=1)
        for g4 in range(nch // 4):
            ps = pst[n % 4]
            for j in range(4):
                c = g4 * 4 + j
                P.op('pe', lambda e, ps=ps, j=j, c=c, b=b: e.transpose(out=ps[:, j * 128:(j + 1) * 128], in_=xt[b][:, c * 128:(c + 1) * 128], identity=ident[:]),
                     reads=[xt[b], ident], writes=[ps])
            if n % 2 == 0:
                P.op('act', lambda e, ps=ps, g4=g4, t=t: e.copy(out=ot[:, g4 * 4:(g4 + 1) * 4, t * 128:(t + 1) * 128], in_=ps[:].rearrange("p (a b) -> p a b", a=4)),
                     reads=[ps], writes=[(ot, t, g4)])
            else:
                P.op('dve', lambda e, ps=ps, g4=g4, t=t: e.tensor_copy(out=ot[:, g4 * 4:(g4 + 1) * 4, t * 128:(t + 1) * 128], in_=ps[:].rearrange("p (a b) -> p a b", a=4)),
                     reads=[ps], writes=[(ot, t, g4)])
            n += 1
    allk = [(ot, t, g4) for t in range(ntiles) for g4 in range(nch // 4)]
    P.op('sp', lambda e: e.dma_start(out=dst, in_=ot[:]), reads=allk, writes=['tdst'], dma=1)
    P.end()


import ml_dtypes as _mld
_BF = _mld.bfloat16
_DBG = {}
GROUPS9 = [(0, 8, 0), (8, 9, 1)]
BLOCKS9 = [(0, 384), (384, 768), (768, 1152)]
CAP = 2304
PIECES_B = [(0, 9), (9, 9), (18, 8), (26, 8)]
FM_ROWS_B = [128] * 16 + [64, 64]


def _dt(nc, name, shape, dtype, kind):
    return nc.dram_tensor(name, list(shape), dtype, kind=kind).ap()


def _run(nc, in_maps):
    res = run_bass_kernel_spmd(nc, in_maps, core_ids=list(range(8)))
    return res.results


def _rope_angles():
    row = np.repeat(np.arange(64), 64).astype(np.float32)
    col = np.tile(np.arange(64), 64).astype(np.float32)
    inv = (np.float32(10000.0) ** (-np.arange(16, dtype=np.float32) / np.float32(16))).astype(np.float32)
    ang = np.concatenate([row[:, None] * inv, col[:, None] * inv], axis=-1).astype(np.float32)
    return np.cos(ang).astype(np.float32), np.sin(ang).astype(np.float32)


def _G_of(modq, b):
    return np.ascontiguousarray(np.stack([modq[b * 4 + q] for q in range(4)]))


def build_L1():
    nc = bass.Bass("TRN2", target_bir_lowering=False)
    cv = _dt(nc, "cv", [128, 16, 2], F32, "ExternalInput")
    adaw = _dt(nc, "adaw", [2048, 6144], F32, "ExternalInput")
    adab = _dt(nc, "adab", [6144], F32, "ExternalInput")
    modq = _dt(nc, "modq", [2, 6144], F32, "ExternalOutput")
    P = Prog(nc)
    stage_ada(P, cv, adaw, adab, modq)
    P.close()
    return nc


def build_L2():
    nc = bass.Bass("TRN2", target_bir_lowering=False)
    I, O, N = "ExternalInput", "ExternalOutput", "Internal"
    h = _dt(nc, "h", [NTB, D], F32, I)
    G = _dt(nc, "G", [4, 2, 6144], F32, I)
    ng = _dt(nc, "ng", [4, D], F32, I)
    wfm = _dt(nc, "wfm", [D, sum(FM_ROWS_B)], F32, I)
    wtm = _dt(nc, "wtm", [D, 264], F32, I)
    alog8 = _dt(nc, "alog8", [8], F32, I)
    dtb8 = _dt(nc, "dtb8", [8], F32, I)
    convw = _dt(nc, "convw", [6, 128, 5], F32, I)
    dng = _dt(nc, "dng", [128], F32, I)
    qg = _dt(nc, "qg", [768], F32, I)
    kvg = _dt(nc, "kvg", [512], F32, I)
    wq = _dt(nc, "wq", [768, 512], F32, I)
    wkv = _dt(nc, "wkv", [512, 512], F32, I)
    cosT = _dt(nc, "cosT", [64, NTB], F32, I)
    ssT = _dt(nc, "ssT", [64, NTB], F32, I)
    uT = _dt(nc, "uT", [128, 16, NTB], BF16, N)
    fm = _dt(nc, "fm", [18, 128, NTB], F32, N)
    tm = _dt(nc, "tm", [NTB, 264], F32, N)
    fmT = _dt(nc, "fmT", [4, 128, NTB], BF16, N)
    tokm = _dt(nc, "tokm", [4, NTB, 128], BF16, N)
    qkT = _dt(nc, "qkT", [2, 3, 128, NTB], BF16, N)
    krT = _dt(nc, "krT", [64, NTB], BF16, N)
    vtok = _dt(nc, "vtok", [2, NTB, 128], BF16, N)
    dnout = _dt(nc, "dnout", [NTB, 256], F32, O)
    oT = _dt(nc, "oT", [2, 128, NTB], BF16, O)
    P = Prog(nc)
    for (t0, n) in PIECES_B:
        groups = [(t, t + 1, 1 if (t0 + t) < 2 else 0) for t in range(n)]
        stage_modulate(P, h, uT, G, None, 0, 1, ng[0], groups, n, t0=t0)
    stage_proj(P, uT, wfm, fm, FM_ROWS_B, wtm, tm, 264)
    stage_dn_pre(P, fm[0:6], convw, fmT, tokm)
    stage_dn_scan(P, fmT, tokm, tm, alog8, dtb8, dng, dnout)
    stage_mla_proj(P, fm[6:18], qg, kvg, wq, wkv, cosT, ssT, qkT, krT, vtok)
    stage_mla_attn(P, qkT, krT, vtok, oT, 192 ** -0.5)
    P.close()
    return nc


def build_L3():
    nc = bass.Bass("TRN2", target_bir_lowering=False)
    I, O, N = "ExternalInput", "ExternalOutput", "Internal"
    dn_tok = _dt(nc, "dn_tok", [NT1, 1024], F32, I)
    mlaT = _dt(nc, "mlaT", [128, 8, NT1], BF16, I)
    hin = _dt(nc, "hin", [NT1, D], F32, I)
    G = _dt(nc, "G", [4, 2, 6144], F32, I)
    ng = _dt(nc, "ng", [2, 4, D], F32, I)
    wout = _dt(nc, "wout", [D, D], F32, I)
    wg = _dt(nc, "wg", [D, DFF], F32, I)
    wu = _dt(nc, "wu", [D, DFF], F32, I)
    wd = _dt(nc, "wd", [DFF, D], F32, I)
    wfm = _dt(nc, "wfm", [D, 36 * 128], F32, I)
    bfm = _dt(nc, "bfm", [36 * 128], F32, I)
    wtm = _dt(nc, "wtm", [D, 256], F32, I)
    btm = _dt(nc, "btm", [256], F32, I)
    cosT = _dt(nc, "cosT", [128, NT1], F32, I)
    ssT = _dt(nc, "ssT", [128, NT1], F32, I)
    dnT = _dt(nc, "dnT", [128, 8, NT1], BF16, N)
    y1 = _dt(nc, "y1", [NT1, D], F32, N)
    hm = _dt(nc, "hm", [NT1, D], F32, N)
    uT = _dt(nc, "uT", [128, 16, NT1], BF16, N)
    y2 = _dt(nc, "y2", [NT1, D], F32, N)
    h0 = _dt(nc, "h0", [NT1, D], F32, O)
    uT1 = _dt(nc, "uT1", [128, 16, NT1], BF16, N)
    fm = _dt(nc, "fm", [36, 128, NT1], F32, N)
    qk = _dt(nc, "qk", [18, 128, NT1], BF16, O)
    vt = _dt(nc, "vt", [NT1, 256], F32, O)
    P = Prog(nc)
    stage_transpose(P, dn_tok, dnT, 9, 8)
    stage_outproj(P, [(dnT, 8), (mlaT, 8)], wout, y1, 9)
    stage_resid(P, y1, hin, hm, G, 2, ng[0, 1], GROUPS9)
    stage_modulate(P, hm, uT, G, None, 3, 4, ng[0, 2], GROUPS9, 9)
    stage_ffn(P, uT, wg, wu, wd, y2, 9, BLOCKS9)
    stage_resid(P, y2, hm, h0, G, 5, ng[0, 3], GROUPS9)
    stage_modulate(P, h0, uT1, G, None, 6, 7, ng[1, 0], GROUPS9, 9)
    stage_proj(P, uT1, wfm, fm, [128] * 36, wtm, vt, 256, ntiles=9, bias_fm=bfm, bias_tm=btm)
    stage_rope(P, fm, cosT, ssT, qk, 18, NT1)
    P.close()
    return nc


def build_L4():
    nc = bass.Bass("TRN2", target_bir_lowering=False)
    I, O, N = "ExternalInput", "ExternalOutput", "Internal"
    qT = _dt(nc, "qT", [16, 128, NT1], BF16, I)
    kTd = _dt(nc, "kTd", [4, 2, 128, NKEXT], BF16, I)
    vd = _dt(nc, "vd", [4, NKEXT, 128], BF16, I)
    sink = _dt(nc, "sink", [32], F32, I)
    lrv = _dt(nc, "lrv", [2], F32, I)
    h0 = _dt(nc, "h0", [1024, D], F32, I)
    G = _dt(nc, "G", [4, 2, 6144], F32, I)
    ng = _dt(nc, "ng", [2, 4, D], F32, I)
    wout = _dt(nc, "wout", [D, D], F32, I)
    bout = _dt(nc, "bout", [D], F32, I)
    wr = _dt(nc, "wr", [D, 8], F32, I)
    attnT = _dt(nc, "attnT", [128, 16, NT1], BF16, N)
    y1 = _dt(nc, "y1", [1024, D], F32, N)
    h1m = _dt(nc, "h1m", [1024, D], F32, O)
    uT = _dt(nc, "uT", [128, 16, 1024], BF16, O)
    uT32 = _dt(nc, "uT32", [128, 16, 1024], F32, N)
    sel = _dt(nc, "sel", [1024, 8], F32, O)
    wgt = _dt(nc, "wgt", [1024, 8], F32, O)
    P = Prog(nc)
    L8 = [(0, 8, 0)]
    stage_gqa_attn(P, qT, kTd, vd, sink, lrv, attnT, 64 ** -0.5)
    stage_outproj(P, attnT[:, :, 0:1024], wout, y1, 8, bias=bout)
    stage_resid(P, y1, h0, h1m, G, 8, ng[1, 1], L8)
    stage_modulate(P, h1m, uT, G, None, 9, 10, ng[1, 2], L8, 8, uT32=uT32)
    stage_router(P, uT32, wr, sel, wgt, 8)
    P.close()
    return nc


NGRP = 3


def build_L5():
    nc = bass.Bass("TRN2", target_bir_lowering=False)
    I, O, N = "ExternalInput", "ExternalOutput", "Internal"
    xT = _dt(nc, "xT", [NGRP, 128, 16, NT1], BF16, I)
    wg = _dt(nc, "wg", [NGRP, D, DFF], F32, I)
    wu = _dt(nc, "wu", [NGRP, D, DFF], F32, I)
    wd = _dt(nc, "wd", [NGRP, DFF, D], F32, I)
    ye = _dt(nc, "ye", [NGRP, NT1, D], F32, O)
    P = Prog(nc)
    for gi in range(NGRP):
        stage_ffn(P, xT[gi], wg[gi], wu[gi], wd[gi], ye[gi], 9, BLOCKS9)
    P.close()
    return nc


def build_L6():
    nc = bass.Bass("TRN2", target_bir_lowering=False)
    I, O, N = "ExternalInput", "ExternalOutput", "Internal"
    ya = _dt(nc, "ya", [1024, D], F32, I)
    yb = _dt(nc, "yb", [1024, D], F32, I)
    w12 = _dt(nc, "w12", [1024, 2], F32, I)
    h1m = _dt(nc, "h1m", [1024, D], F32, I)
    G = _dt(nc, "G", [4, 2, 6144], F32, I)
    ng = _dt(nc, "ng", [2, 4, D], F32, I)
    ysum = _dt(nc, "ysum", [1024, D], F32, N)
    out = _dt(nc, "out", [1024, D], F32, O)
    P = Prog(nc)
    stage_combine(P, ya, yb, w12, ysum, 8)
    stage_resid(P, ysum, h1m, out, G, 11, ng[1, 3], [(0, 8, 0)])
    P.close()
    return nc


def kernel(x, c, ctx, c_ctx, ada_w, ada_b, norm_g, ab_w_in, dn_conv_w, dn_a_log, dn_dt_bias, dn_norm_g,
           mla_q_norm_g, mla_w_qb, mla_kv_norm_g, mla_w_kvb, ab_w_out, ffn_w_gate, ffn_w_up, ffn_w_down,
           gqa_w_qkv, gqa_b_qkv, gqa_sink, gqa_w_out, gqa_b_out, moe_w_router, moe_w_gate, moe_w_up, moe_w_down):
    f32 = np.float32
    A = lambda v: np.ascontiguousarray(np.asarray(v))
    x, c, ctx, c_ctx = A(x), A(c), A(ctx), A(c_ctx)
    norm_g = A(norm_g)
    ims = []
    for core in range(8):
        b, j = core // 4, core % 4
        cvv = np.stack([c[b], c_ctx], axis=-1).reshape(16, 128, 2).transpose(1, 0, 2)
        ims.append({"cv": A(cvv), "adaw": A(ada_w[j // 2][:, (j % 2) * 6144:(j % 2 + 1) * 6144]),
                    "adab": A(ada_b[j // 2][(j % 2) * 6144:(j % 2 + 1) * 6144])})
    r1 = _run(build_L1(), ims)
    modq = [np.asarray(r["modq"]) for r in r1]
    Gb = [_G_of(modq, 0), _G_of(modq, 1)]
    W = np.asarray(ab_w_in[0])
    cos, sin = _rope_angles()
    cosB = np.ones((64, NTB), f32); ssB = np.zeros((64, NTB), f32)
    cosB[:32, 256:] = cos.T; cosB[32:, 256:] = cos.T
    ssB[:32, 256:] = -sin.T; ssB[32:, 256:] = sin.T
    wqb, wkvb = np.asarray(mla_w_qb[0]), np.asarray(mla_w_kvb[0])
    ims = []
    for core in range(8):
        b, j = core // 4, core % 4
        hs = [2 * j, 2 * j + 1]
        cols = []
        for h in hs:
            for ty in range(3):
                cols += list(range(ty * 1024 + h * 128, ty * 1024 + (h + 1) * 128))
        cols += list(range(4128, 4128 + 768 + 512 + 64))
        cols += list(range(5408 + 32, 5408 + 64)) + list(range(5408, 5408 + 32))
        tcols = []
        for h in hs:
            tcols += list(range(3072 + h * 128, 3072 + (h + 1) * 128))
        for h in hs:
            tcols += [4096 + h, 4096 + 8 + h, 4096 + 16 + h, 4096 + 24 + h]
        al = np.zeros(8, f32); db = np.zeros(8, f32)
        cw = np.zeros((6, 128, 5), f32)
        for hh, h in enumerate(hs):
            for d in range(2):
                al[hh * 4 + 2 + d] = dn_a_log[0][d, h]
                db[hh * 4 + 2 + d] = dn_dt_bias[0][d, h]
            for ty in range(3):
                ch = ty * 1024 + h * 128
                cw[hh * 3 + ty] = np.asarray(dn_conv_w[0])[:, ch:ch + 128].T
        qc = []
        for h in hs: qc += list(range(h * 192, h * 192 + 128))
        for h in hs: qc += list(range(h * 192 + 128, h * 192 + 192))
        for h in hs: qc += list(range(h * 192 + 160, h * 192 + 192)) + list(range(h * 192 + 128, h * 192 + 160))
        kc = []
        for h in hs: kc += list(range(h * 256, h * 256 + 128))
        for h in hs: kc += list(range(h * 256 + 128, h * 256 + 256))
        ims.append({"h": A(np.concatenate([ctx[b], x[b]], axis=0)), "G": Gb[b], "ng": A(norm_g[0]), "wfm": A(W[:, cols]), "wtm": A(W[:, tcols]),
                    "alog8": al, "dtb8": db, "convw": cw, "dng": A(dn_norm_g[0]), "qg": A(mla_q_norm_g[0]), "kvg": A(mla_kv_norm_g[0]),
                    "wq": A(wqb[:, qc]), "wkv": A(wkvb[:, kc]), "cosT": cosB, "ssT": ssB})
    r2 = _run(build_L2(), ims)
    Wq = np.asarray(gqa_w_qkv[0]); Bq = np.asarray(gqa_b_qkv[0])
    qp = []
    for h in range(32):
        qp += list(range(h * 64 + 32, h * 64 + 64)) + list(range(h * 64, h * 64 + 32))
    kp = []
    for g in range(4):
        kp += list(range(2048 + g * 64 + 32, 2048 + g * 64 + 64)) + list(range(2048 + g * 64, 2048 + g * 64 + 32))
    pc = list(range(2048)) + list(range(2048, 2304)) + qp + kp

    def tok_rows(j):
        return np.concatenate([256 + j * 1024 + np.arange(1024), j * 64 + np.arange(64)])
    ims = []
    for core in range(8):
        b, j = core // 4, core % 4
        rows = tok_rows(j)
        dn_tok = np.zeros((NT1, 1024), f32)
        mlaT = np.zeros((128, 8, NT1), _BF)
        for q in range(4):
            rq = r2[b * 4 + q]
            dn_tok[:1088, q * 256:(q + 1) * 256] = np.asarray(rq["dnout"])[rows]
            oT = np.asarray(rq["oT"])
            for hh in range(2):
                mlaT[:, q * 2 + hh, :1088] = oT[hh][:, rows]
        hin = np.zeros((NT1, D), f32)
        hin[:1024] = x[b, j * 1024:(j + 1) * 1024]; hin[1024:1088] = ctx[b, j * 64:(j + 1) * 64]
        cosT = np.ones((128, NT1), f32); ssT = np.zeros((128, NT1), f32)
        cj, sj = cos[j * 1024:(j + 1) * 1024].T, sin[j * 1024:(j + 1) * 1024].T
        for r in range(2):
            cosT[r * 64:r * 64 + 32, :1024] = cj; cosT[r * 64 + 32:r * 64 + 64, :1024] = cj
            ssT[r * 64:r * 64 + 32, :1024] = -sj; ssT[r * 64 + 32:r * 64 + 64, :1024] = sj
        ims.append({"dn_tok": dn_tok, "mlaT": mlaT, "hin": hin, "G": Gb[b], "ng": norm_g, "wout": A(ab_w_out[0]), "wg": A(ffn_w_gate[0]),
                    "wu": A(ffn_w_up[0]), "wd": A(ffn_w_down[0]), "wfm": A(Wq[:, pc]), "bfm": A(Bq[pc]), "wtm": A(Wq[:, 2304:2560]),
                    "btm": A(Bq[2304:2560]), "cosT": cosT, "ssT": ssT})
    r3 = _run(build_L3(), ims)
    _DBG['r2'] = r2; _DBG['r3'] = r3
    ims = []
    for core in range(8):
        b, j = core // 4, core % 4
        kT = np.zeros((4, 2, 128, NKEXT), _BF); vd = np.zeros((4, NKEXT, 128), _BF)

        def kv_of(cc, lo, hi):
            return np.asarray(r3[cc]["qk"])[16:18][:, :, lo:hi], np.asarray(r3[cc]["vt"])[lo:hi]
        segs = [(jj * 64, kv_of(b * 4 + jj, 1024, 1088)) for jj in range(4)]
        if j > 0:
            segs.append((256, kv_of(core - 1, 896, 1024)))
        segs.append((384, kv_of(core, 0, 1024)))
        if j < 3:
            segs.append((1408, kv_of(core + 1, 0, 128)))
        for off, (kk, vv) in segs:
            n = kk.shape[2]
            for g in range(4):
                rws = kk[g // 2, (g % 2) * 64:(g % 2) * 64 + 64]
                kT[g, 0, 0:64, off:off + n] = rws; kT[g, 1, 64:128, off:off + n] = rws
                v_ = vv[:, g * 64:(g + 1) * 64]
                vd[g, off:off + n, 0:64] = v_; vd[g, off:off + n, 64:128] = v_
        ims.append({"qT": A(np.asarray(r3[core]["qk"])[0:16]), "kTd": kT, "vd": vd, "sink": A(gqa_sink[0]),
                    "lrv": np.array([1.0 if j > 0 else 0.0, 1.0 if j < 3 else 0.0], f32), "h0": A(np.asarray(r3[core]["h0"])[:1024]),
                    "G": Gb[b], "ng": norm_g, "wout": A(gqa_w_out[0]), "bout": A(gqa_b_out[0]), "wr": A(moe_w_router[0])})
    r4 = _run(build_L4(), ims)
    _DBG['r4'] = r4
    sel = np.concatenate([np.asarray(r["sel"]) for r in r4], axis=0)
    wgt = np.concatenate([np.asarray(r["wgt"]) for r in r4], axis=0)
    uT_all = np.concatenate([np.asarray(r["uT"]) for r in r4], axis=2)
    items = []
    for e in range(8):
        tok = np.nonzero(sel[:, e] > 0.5)[0]
        for k0 in range(0, len(tok), NT1):
            items.append((e, tok[k0:k0 + NT1]))
    assert len(items) <= 8 * NGRP
    wge, wue, wde = np.asarray(moe_w_gate[0]), np.asarray(moe_w_up[0]), np.asarray(moe_w_down[0])
    ims = []
    for core in range(8):
        xT = np.zeros((NGRP, 128, 16, NT1), _BF)
        es = []
        for gi in range(NGRP):
            idx = core * NGRP + gi
            e = 0
            if idx < len(items):
                e, tok = items[idx]
                xT[gi, :, :, :len(tok)] = uT_all[:, :, tok]
            es.append(e)
        ims.append({"xT": xT, "wg": A(wge[es]), "wu": A(wue[es]), "wd": A(wde[es])})
    r5 = _run(build_L5(), ims)
    _DBG['r5'] = r5
    ye_all = np.concatenate([np.asarray(r["ye"]).reshape(NGRP * NT1, D) for r in r5], axis=0)
    pos = np.full((8192, 8), -1, np.int64)
    for idx, (e, tok) in enumerate(items):
        pos[tok, e] = idx * NT1 + np.arange(len(tok))
    order = np.argsort(-sel, axis=1, kind="stable")[:, :2]
    ims = []
    for core in range(8):
        b, j = core // 4, core % 4
        t0 = core * 1024
        ya = np.zeros((1024, D), f32); yb = np.zeros((1024, D), f32); w12 = np.zeros((1024, 2), f32)
        tt = np.arange(t0, t0 + 1024)
        for i, dst in enumerate((ya, yb)):
            ee = order[tt, i]
            pp = pos[tt, ee]
            m = pp >= 0
            dst[m] = ye_all[pp[m]]
            w12[m, i] = wgt[tt[m], ee[m]]
        ims.append({"ya": ya, "yb": yb, "w12": w12, "h1m": A(np.asarray(r4[core]["h1m"])), "G": Gb[b], "ng": norm_g})
    r6 = _run(build_L6(), ims)
    out = np.stack([np.concatenate([np.asarray(r6[b * 4 + j]["out"]) for j in range(4)], axis=0) for b in range(2)])
    return out.astype(f32)
```

```python
import contextlib
import numpy as np
import concourse.bass as bass
import concourse.mybir as mybir
from concourse.bass_utils import run_bass_kernel_spmd

F32 = mybir.dt.float32
BF16 = mybir.dt.bfloat16
I32 = mybir.dt.int32
AF = mybir.ActivationFunctionType
ALU = mybir.AluOpType
AX = mybir.AxisListType

D = 2048
DFF = 7168
EPS = 1e-6
ENGS = ('pe', 'act', 'dve', 'pool', 'sp')
ENGATTR = {'pe': 'tensor', 'act': 'scalar', 'dve': 'vector', 'pool': 'gpsimd', 'sp': 'sync'}
DMAQ = ('sp', 'act', 'pool')
NDMASEM = 4


class Op:
    __slots__ = ('eng', 'fn', 'deps', 'seq', 'sig', 'dma', 'dsem', 'dval')

    def __init__(self, eng, fn):
        self.eng = eng
        self.fn = fn
        self.deps = {}
        self.sig = False
        self.dma = 0
        self.dsem = None
        self.dval = 0


class Prog:
    def __init__(self, nc):
        self.nc = nc
        self.outer = contextlib.ExitStack()
        self.sems = {}
        for e in ENGS:
            self.sems[('c', e)] = self.outer.enter_context(nc.semaphore(f"c_{e}"))
        for q in DMAQ:
            for k in range(NDMASEM):
                self.sems[('d', q, k)] = self.outer.enter_context(nc.semaphore(f"d_{q}{k}"))
        self.base = {e: 0 for e in ENGS}
        self.dma_issued = {}
        self.dma_batches = {q: 0 for q in DMAQ}
        self.known = {e: {} for e in ENGS}
        self.n_names = 0
        self.es = None
        self.total_ops = {e: 0 for e in ENGS}

    def begin(self):
        self.es = contextlib.ExitStack()
        self.ops = {e: [] for e in ENGS}
        self.state = {}

    def sb(self, shape, dtype=F32, name=None):
        self.n_names += 1
        return self.es.enter_context(self.nc.sbuf_tensor(name or f"sb{self.n_names}", list(shape), dtype))

    def ps(self, shape, dtype=F32, name=None):
        self.n_names += 1
        return self.es.enter_context(self.nc.psum_tensor(name or f"ps{self.n_names}", list(shape), dtype))

    @staticmethod
    def _key(r):
        if isinstance(r, tuple):
            return (Prog._key(r[0]),) + tuple(r[1:])
        if isinstance(r, (str, int)):
            return r
        return r.name

    @staticmethod
    def _is_psum(k):
        if isinstance(k, tuple):
            k = k[0]
        return isinstance(k, str) and k.startswith('ps')

    def _dep_on(self, op, d):
        if d is None:
            return
        if d[0] == 'c' and d[1] == 'pe' and op.eng == 'pe':
            return
        if d[0] == 'c':
            key = ('c', d[1])
            if op.deps.get(key, -1) < d[2]:
                op.deps[key] = d[2]
        else:
            key = ('d', d[1], d[2])
            if op.deps.get(key, -1) < d[3]:
                op.deps[key] = d[3]

    def op(self, eng, fn, reads=(), writes=(), dma=0):
        reads = [self._key(r) for r in reads]
        writes = [self._key(w) for w in writes]
        pr = [r for r in reads if self._is_psum(r)]
        if pr:
            reads = [r for r in reads if not self._is_psum(r)]
            writes = writes + [r for r in pr if r not in writes]
        o = Op(eng, fn)
        lst = self.ops[eng]
        o.seq = len(lst)
        for r in reads:
            st = self.state.get(r)
            if st is not None:
                self._dep_on(o, st[0])
        for w in writes:
            st = self.state.get(w)
            if st is not None:
                self._dep_on(o, st[0])
                for rd in st[1]:
                    self._dep_on(o, rd)
        if dma:
            b = self.dma_batches[eng]
            self.dma_batches[eng] = b + 1
            k = b % NDMASEM
            prev = self.dma_issued.get((eng, k), 0)
            if prev:
                self._dep_on(o, ('d', eng, k, prev))
            val = prev + 16 * dma
            self.dma_issued[(eng, k)] = val
            o.dma = dma
            o.dsem = (eng, k)
            o.dval = val
            me = ('d', eng, k, val)
        else:
            me = ('c', eng, o.seq)
        for r in reads:
            st = self.state.setdefault(r, [None, []])
            st[1].append(me)
        for w in writes:
            self.state[w] = [me, []]
        lst.append(o)
        return o

    def end(self):
        nc = self.nc
        for e in ENGS:
            for o in self.ops[e]:
                for key, v in o.deps.items():
                    if key[0] == 'c':
                        self.ops[key[1]][v].sig = True
            for o in reversed(self.ops[e]):
                if not o.dma:
                    o.sig = True
                    break
        cnt = {}
        for e in ENGS:
            c = self.base[e]
            arr = []
            for o in self.ops[e]:
                if o.sig:
                    c += 1
                arr.append(c)
            cnt[e] = arr
        final = {e: (cnt[e][-1] if cnt[e] else self.base[e]) for e in ENGS}

        def replay(e, eng):
            known = self.known[e]
            for o in self.ops[e]:
                for key, v in o.deps.items():
                    val = cnt[key[1]][v] if key[0] == 'c' else v
                    if known.get(key, 0) >= val:
                        continue
                    known[key] = val
                    eng.wait_ge(self.sems[key], val)
                r = o.fn(eng)
                if o.dma:
                    if not isinstance(r, (list, tuple)):
                        r = [r]
                    assert len(r) == o.dma, (len(r), o.dma)
                    for ins in r:
                        ins.then_inc(self.sems[('d',) + o.dsem], 16)
                elif o.sig:
                    r.then_inc(self.sems[('c', e)], 1)
            for e2 in ENGS:
                if e2 == e:
                    continue
                key = ('c', e2)
                if final[e2] > known.get(key, 0):
                    known[key] = final[e2]
                    eng.wait_ge(self.sems[key], final[e2])
            for (q, k), val in self.dma_issued.items():
                key = ('d', q, k)
                if val > known.get(key, 0):
                    known[key] = val
                    eng.wait_ge(self.sems[key], val)

        with nc.Block() as block:
            for e in ENGS:
                getattr(block, ENGATTR[e])(lambda eng, e=e: replay(e, eng))
        for e in ENGS:
            self.base[e] = final[e]
            self.total_ops[e] += len(self.ops[e])
        self.es.close()
        self.es = None

    def close(self):
        self.outer.close()


def make_ident(P, dtype=F32):
    idf = P.sb([128, 128], F32)
    P.op('pool', lambda e: e.memset(idf[:], 0.0), writes=[idf])
    P.op('pool', lambda e: e.affine_select(out=idf[:], in_=idf[:], pattern=[[-1, 128]], compare_op=ALU.not_equal,
                                           fill=1.0, base=0, channel_multiplier=1), reads=[idf], writes=[idf])
    if dtype == F32:
        return idf
    idb = P.sb([128, 128], dtype)
    P.op('dve', lambda e: e.tensor_copy(out=idb[:], in_=idf[:]), reads=[idf], writes=[idb])
    return idb


def rstd_from_ss(P, ss, rstd, n, rk=None):
    P.op('dve', lambda e: e.tensor_scalar(out=rstd[:], in0=ss[:], scalar1=1.0 / n, scalar2=EPS, op0=ALU.mult, op1=ALU.add),
         reads=[ss], writes=[rstd])
    P.op('act', lambda e: e.activation(out=rstd[:], in_=rstd[:], func=AF.Sqrt), reads=[rstd], writes=[rstd])
    P.op('dve', lambda e: e.reciprocal(out=rstd[:], in_=rstd[:]), reads=[rstd], writes=[rstd])


def vec_pc(v):
    return v.rearrange("(c p) -> p c", p=128)


def stage_ada(P, cv, adaw, adab, modq):
    P.begin()
    NCOL = 6144
    cvt = P.sb([128, 16, 2], F32)
    st = P.sb([128, 16, 2], F32)
    wb = [P.sb([128, 16, 512], F32) for _ in range(2)]
    bt = P.sb([2, NCOL], F32)
    ot = P.sb([2, NCOL], F32)
    pp = [P.ps([2, 512], F32) for _ in range(2)]
    P.op('sp', lambda e: e.dma_start(out=cvt[:], in_=cv), writes=[cvt], dma=1)
    P.op('sp', lambda e: e.dma_start(out=bt[:], in_=adab.partition_broadcast(2)), writes=[bt], dma=1)
    P.op('act', lambda e: e.activation(out=st[:], in_=cvt[:], func=AF.Silu), reads=[cvt], writes=[st])
    wv = adaw.rearrange("(c p) n -> p c n", p=128)
    for pi in range(NCOL // 512):
        w = wb[pi % 2]
        q = 'sp' if pi % 2 == 0 else 'act'
        P.op(q, lambda e, w=w, pi=pi: e.dma_start(out=w[:], in_=wv[:, :, pi * 512:(pi + 1) * 512]), writes=[w], dma=1)
        ps = pp[pi % 2]
        for c in range(16):
            P.op('pe', lambda e, w=w, ps=ps, c=c: e.matmul(ps[:], lhsT=st[:, c, :], rhs=w[:, c, :], start=(c == 0), stop=(c == 15)),
                 reads=[st, w], writes=[ps])
        P.op('dve', lambda e, ps=ps, pi=pi: e.tensor_tensor(out=ot[:, pi * 512:(pi + 1) * 512], in0=ps[:],
                                                             in1=bt[:, pi * 512:(pi + 1) * 512], op=ALU.add),
             reads=[ps, bt], writes=[(ot, pi)])
    P.op('sp', lambda e: e.dma_start(out=modq, in_=ot[:]), reads=[(ot, pi) for pi in range(NCOL // 512)], writes=['modq'], dma=1)
    P.end()


def modvec(G, k, r):
    return G[k // 3, r, (k % 3) * 2048:(k % 3 + 1) * 2048]


class PCLoader:
    def __init__(self, P, ident, nchunk=16):
        self.P, self.ident, self.n = P, ident, nchunk
        self.rows = [P.sb([nchunk, 128], F32) for _ in range(2)]
        self.pt = P.ps([128, 512], F32)
        self.i = 0

    def load(self, q, dst, vec, n=None):
        P, ident = self.P, self.ident
        n = n or self.n
        row = self.rows[self.i % 2]
        self.i += 1
        pt = self.pt
        P.op(q, lambda e: e.dma_start(out=row[0:n, :], in_=vec.rearrange("(c p) -> c p", p=128)), writes=[row], dma=1)
        P.op('pe', lambda e: e.transpose(out=pt[:, 0:n], in_=row[0:n, :], identity=ident[0:n, 0:n]), reads=[row, ident], writes=[pt])
        P.op('dve', lambda e: e.tensor_copy(out=dst[:, 0:n], in_=pt[:, 0:n]), reads=[pt], writes=[dst])


MODSTOP = 9


def stage_modulate(P, h, uT, G, kg, ksh, ksc, normg, groups, ntiles, uT32=None, key_h='h', key_u='uT', t0=0):
    P.begin()
    NT = ntiles * 128
    ident = make_ident(P)
    rows = sorted(set(r for _, _, r in groups))
    gs, sh = {}, {}
    gt = P.sb([128, 16], F32)
    pcl = PCLoader(P, ident)
    pcl.load('sp', gt, normg)
    for r in rows:
        sc = P.sb([128, 16], F32)
        sh[r] = P.sb([128, 16], F32)
        gs[r] = P.sb([128, 16], F32)
        pcl.load('sp', sc, modvec(G, ksc, r))
        pcl.load('sp', sh[r], modvec(G, ksh, r))
        P.op('dve', lambda e, r=r, sc=sc: e.scalar_tensor_tensor(out=gs[r][:], in0=sc[:], scalar=1.0, in1=gt[:], op0=ALU.add, op1=ALU.mult),
             reads=[sc, gt], writes=[gs[r]])
    uTs = P.sb([128, 16, NT], BF16)
    zero1 = P.sb([128, 1], F32)
    P.op('pool', lambda e: e.memset(zero1[:], 0.0), writes=[zero1])
    uT32s = P.sb([128, 16, NT], F32) if uT32 is not None else None
    ht = [P.sb([128, D], F32) for _ in range(2)]
    xh = [P.sb([128, D], F32) for _ in range(2)]
    junk = P.sb([128, D], F32)
    ss = [P.sb([128, 1], F32) for _ in range(2)]
    rs = [P.sb([128, 1], F32) for _ in range(2)]
    pst = [P.ps([128, 512], F32) for _ in range(4)]
    npt = 0
    for lo, hi, r in groups:
        for t in range(lo, hi):
            b = t % 2
            P.op('sp', lambda e, t=t, b=b: e.dma_start(out=ht[b][:], in_=h[(t0 + t) * 128:(t0 + t + 1) * 128, :]), reads=[key_h], writes=[ht[b]], dma=1)
            P.op('act', lambda e, b=b: e.activation(out=junk[:], in_=ht[b][:], func=AF.Square, accum_out=ss[b][:]),
                 reads=[ht[b]], writes=[junk, ss[b]])
            if MODSTOP < 2:
                continue
            rstd_from_ss(P, ss[b], rs[b], D)
            P.op('dve', lambda e, b=b: e.tensor_scalar(out=xh[b][:], in0=ht[b][:], scalar1=rs[b][:, 0:1], scalar2=zero1[:, 0:1], op0=ALU.mult, op1=ALU.add),
                 reads=[ht[b], rs[b], zero1], writes=[xh[b]])
            for g4 in range(4 if MODSTOP >= 3 else 0):
                ps = pst[npt % 4]
                npt += 1
                for j in range(4):
                    c = g4 * 4 + j
                    P.op('pe', lambda e, ps=ps, j=j, c=c, b=b: e.transpose(out=ps[:, j * 128:(j + 1) * 128], in_=xh[b][:, c * 128:(c + 1) * 128], identity=ident[:]),
                         reads=[xh[b], ident], writes=[ps])
                for j in range(4):
                    c = g4 * 4 + j
                    if g4 % 2 == 0:
                        P.op('act', lambda e, ps=ps, j=j, c=c, t=t, r=r: e.activation(
                            out=uTs[:, c, t * 128:(t + 1) * 128], in_=ps[:, j * 128:(j + 1) * 128], func=AF.Identity,
                            scale=gs[r][:, c:c + 1], bias=sh[r][:, c:c + 1]), reads=[ps, gs[r], sh[r]], writes=[(uTs, t, c)])
                    else:
                        P.op('dve', lambda e, ps=ps, j=j, c=c, t=t, r=r: e.tensor_scalar(
                            out=uTs[:, c, t * 128:(t + 1) * 128], in0=ps[:, j * 128:(j + 1) * 128],
                            scalar1=gs[r][:, c:c + 1], scalar2=sh[r][:, c:c + 1], op0=ALU.mult, op1=ALU.add),
                            reads=[ps, gs[r], sh[r]], writes=[(uTs, t, c)])
                    if uT32s is not None:
                        P.op('pool' if False else 'dve', lambda e, ps=ps, j=j, c=c, t=t, r=r: e.tensor_scalar(
                            out=uT32s[:, c, t * 128:(t + 1) * 128], in0=ps[:, j * 128:(j + 1) * 128],
                            scalar1=gs[r][:, c:c + 1], scalar2=sh[r][:, c:c + 1], op0=ALU.mult, op1=ALU.add),
                            reads=[ps, gs[r], sh[r]], writes=[(uT32s, t, c)])
    allk = [(uTs, t, c) for lo, hi, r in groups for t in range(lo, hi) for c in range(16)]
    if MODSTOP < 3:
        P.op('pool', lambda e: e.memset(uTs[:], 0.0), writes=allk)
    for c4 in range(4):
        P.op('sp', lambda e, c4=c4: e.dma_start(out=uT[:, c4 * 4:(c4 + 1) * 4, t0 * 128:t0 * 128 + NT], in_=uTs[:, c4 * 4:(c4 + 1) * 4, :]), reads=allk, writes=[key_u], dma=1)
    if uT32s is not None:
        allk32 = [(uT32s, t, c) for lo, hi, r in groups for t in range(lo, hi) for c in range(16)]
        P.op('sp', lambda e: e.dma_start(out=uT32, in_=uT32s[:]), reads=allk32, writes=[key_u + '32'], dma=1)
    P.end()


def load_bc(P, q, dst, vec, n=128):
    P.op(q, lambda e: e.dma_start(out=dst[:], in_=vec.partition_broadcast(n)), writes=[dst], dma=1)


def stage_resid(P, y, h_in, h_out, G, kgate, normg, groups, key_y='y', key_hin='hin', key_hout='hout'):
    P.begin()
    rows = sorted(set(r for _, _, r in groups))
    gt = P.sb([128, D], F32)
    load_bc(P, 'sp', gt, normg)
    gm = {}
    for r in rows:
        gm[r] = P.sb([128, D], F32)
        load_bc(P, 'act', gm[r], modvec(G, kgate, r))
        P.op('pool', lambda e, r=r: e.tensor_tensor(out=gm[r][:], in0=gm[r][:], in1=gt[:], op=ALU.mult), reads=[gm[r], gt], writes=[gm[r]])
    yt = [P.sb([128, D], F32) for _ in range(2)]
    ht = [P.sb([128, D], F32) for _ in range(2)]
    ot = [P.sb([128, D], F32) for _ in range(2)]
    junk = P.sb([128, D], F32)
    ss = [P.sb([128, 1], F32) for _ in range(2)]
    rs = [P.sb([128, 1], F32) for _ in range(2)]
    for lo, hi, r in groups:
        for t in range(lo, hi):
            b = t % 2
            P.op('sp', lambda e, t=t, b=b: e.dma_start(out=yt[b][:], in_=y[t * 128:(t + 1) * 128, :]), reads=[key_y], writes=[yt[b]], dma=1)
            P.op('act', lambda e, t=t, b=b: e.dma_start(out=ht[b][:], in_=h_in[t * 128:(t + 1) * 128, :]), reads=[key_hin], writes=[ht[b]], dma=1)
            P.op('act', lambda e, b=b: e.activation(out=junk[:], in_=yt[b][:], func=AF.Square, accum_out=ss[b][:]),
                 reads=[yt[b]], writes=[junk, ss[b]])
            rstd_from_ss(P, ss[b], rs[b], D)
            P.op('dve', lambda e, b=b, r=r: e.scalar_tensor_tensor(out=ot[b][:], in0=yt[b][:], scalar=rs[b][:, 0:1], in1=gm[r][:],
                                                                   op0=ALU.mult, op1=ALU.mult), reads=[yt[b], rs[b], gm[r]], writes=[ot[b]])
            P.op('pool', lambda e, b=b: e.tensor_tensor(out=ot[b][:], in0=ot[b][:], in1=ht[b][:], op=ALU.add), reads=[ot[b], ht[b]], writes=[ot[b]])
            P.op('sp', lambda e, t=t, b=b: e.dma_start(out=h_out[t * 128:(t + 1) * 128, :], in_=ot[b][:]), reads=[ot[b]], writes=[key_hout], dma=1)
    P.end()

WMODE = 'cast'


def load_w_bf16(P, ws, wv, KC, ncols, pw, keyf):
    if WMODE == 'cast':
        for q in range(ncols // pw):
            P.op('pool', lambda e, q=q: e.dma_start(out=ws[:, :, q * pw:(q + 1) * pw], in_=wv[:, :, q * pw:(q + 1) * pw]),
                 writes=[keyf(q)], dma=1)
    else:
        stg = [P.sb([128, KC, pw], F32) for _ in range(2)]
        for q in range(ncols // pw):
            s_ = stg[q % 2]
            P.op('sp' if q % 2 == 0 else 'act', lambda e, q=q, s_=s_: e.dma_start(out=s_[:], in_=wv[:, :, q * pw:(q + 1) * pw]), writes=[s_], dma=1)
            P.op('pool', lambda e, q=q, s_=s_: e.tensor_copy(out=ws[:, :, q * pw:(q + 1) * pw], in_=s_[:]), reads=[s_], writes=[keyf(q)])


def stage_outproj(P, inT, W, y, ntiles, KC=16, bias=None, key_in='inT', key_y='y'):
    P.begin()
    NT = ntiles * 128
    xs = P.sb([128, KC, NT], BF16)
    if isinstance(inT, (list, tuple)):
        off = 0
        for i, (ap_, kc_) in enumerate(inT):
            P.op('sp' if i % 2 == 0 else 'act', lambda e, ap_=ap_, off=off, kc_=kc_: e.dma_start(out=xs[:, off:off + kc_, :], in_=ap_), reads=[key_in], writes=[(xs, i)], dma=1)
            off += kc_
        xs_keys = [(xs, i) for i in range(len(inT))]
    else:
        P.op('sp', lambda e: e.dma_start(out=xs[:], in_=inT), reads=[key_in], writes=[xs], dma=1)
        xs_keys = [xs]
    wv = W.rearrange("(c p) n -> p c n", p=128)
    wsl = [P.sb([128, KC, 512], BF16) for _ in range(4)]
    for q4 in range(4):
        load_w_bf16(P, wsl[q4], wv[:, :, q4 * 512:(q4 + 1) * 512], KC, 512, 512, lambda q, q4=q4: wsl[q4])
    bt = None
    if bias is not None:
        bt = P.sb([128, D], F32)
        load_bc(P, 'act', bt, bias)
    pp = [P.ps([128, 512], F32) for _ in range(8)]
    ot = [P.sb([128, D], F32) for _ in range(2)]
    n = 0
    for t in range(ntiles):
        b = t % 2
        for q4 in range(4):
            ps = pp[n % 8]
            n += 1
            for c in range(KC):
                P.op('pe', lambda e, ps=ps, c=c, t=t, q4=q4: e.matmul(ps[:], lhsT=xs[:, c, t * 128:(t + 1) * 128], rhs=wsl[q4][:, c, :],
                                                                  start=(c == 0), stop=(c == KC - 1)), reads=xs_keys + [wsl[q4]], writes=[ps])
            if bt is None:
                eng = 'act' if q4 % 2 == 0 else 'dve'
                if eng == 'act':
                    P.op('act', lambda e, ps=ps, b=b, q4=q4: e.copy(out=ot[b][:, q4 * 512:(q4 + 1) * 512], in_=ps[:]), reads=[ps], writes=[(ot[b], q4)])
                else:
                    P.op('dve', lambda e, ps=ps, b=b, q4=q4: e.tensor_copy(out=ot[b][:, q4 * 512:(q4 + 1) * 512], in_=ps[:]), reads=[ps], writes=[(ot[b], q4)])
            else:
                P.op('dve', lambda e, ps=ps, b=b, q4=q4: e.tensor_tensor(out=ot[b][:, q4 * 512:(q4 + 1) * 512], in0=ps[:], in1=bt[:, q4 * 512:(q4 + 1) * 512], op=ALU.add),
                     reads=[ps, bt], writes=[(ot[b], q4)])
        P.op('sp', lambda e, t=t, b=b: e.dma_start(out=y[t * 128:(t + 1) * 128, :], in_=ot[b][:]), reads=[(ot[b], q4) for q4 in range(4)], writes=[key_y], dma=1)
    P.end()


def stage_ffn(P, uT, Wg, Wu, Wd, y, ntiles, blocks, key_u='uT', key_y='y'):
    NT = ntiles * 128
    FC = DFF // 128
    mid = contextlib.ExitStack()
    P.n_names += 1
    h1T = mid.enter_context(P.nc.sbuf_tensor(f"h1T{P.n_names}", [128, FC, NT], BF16))
    P.begin()
    us = P.sb([128, 16, NT], BF16)
    P.op('sp', lambda e: e.dma_start(out=us[:], in_=uT), reads=[key_u], writes=[us], dma=1)
    PW = 256
    wg = [P.sb([128, 16, PW], BF16) for _ in range(2)]
    wu = [P.sb([128, 16, PW], BF16) for _ in range(2)]
    sg = [P.sb([128, 512], F32) for _ in range(2)]
    pg = [P.ps([128, 512], F32) for _ in range(2)]
    pu = [P.ps([128, 512], F32) for _ in range(2)]
    wgv = Wg.rearrange("(c p) n -> p c n", p=128)
    wuv = Wu.rearrange("(c p) n -> p c n", p=128)
    n = 0
    for pi in range(DFF // PW):
        b = pi % 2
        P.op('pool', lambda e, b=b, pi=pi: e.dma_start(out=wg[b][:], in_=wgv[:, :, pi * PW:(pi + 1) * PW]), writes=[wg[b]], dma=1)
        P.op('pool', lambda e, b=b, pi=pi: e.dma_start(out=wu[b][:], in_=wuv[:, :, pi * PW:(pi + 1) * PW]), writes=[wu[b]], dma=1)
        for m in range(PW // 128):
            fc = pi * (PW // 128) + m
            for (lo, hi) in blocks:
                w = hi - lo
                k = n % 2
                n += 1
                for c in range(16):
                    P.op('pe', lambda e, k=k, c=c, b=b, m=m, lo=lo, hi=hi, w=w: e.matmul(
                        pg[k][:, 0:w], lhsT=wg[b][:, c, m * 128:(m + 1) * 128], rhs=us[:, c, lo:hi], start=(c == 0), stop=(c == 15)),
                        reads=[wg[b], us], writes=[pg[k]])
                for c in range(16):
                    P.op('pe', lambda e, k=k, c=c, b=b, m=m, lo=lo, hi=hi, w=w: e.matmul(
                        pu[k][:, 0:w], lhsT=wu[b][:, c, m * 128:(m + 1) * 128], rhs=us[:, c, lo:hi], start=(c == 0), stop=(c == 15)),
                        reads=[wu[b], us], writes=[pu[k]])
                P.op('act', lambda e, k=k, w=w: e.activation(out=sg[k][:, 0:w], in_=pg[k][:, 0:w], func=AF.Silu), reads=[pg[k]], writes=[sg[k]])
                P.op('dve', lambda e, k=k, w=w, fc=fc, lo=lo, hi=hi: e.tensor_tensor(out=h1T[:, fc, lo:hi], in0=sg[k][:, 0:w], in1=pu[k][:, 0:w], op=ALU.mult),
                     reads=[sg[k], pu[k]], writes=[('h1T', fc)])
    P.end()
    P.begin()
    PD = 256
    wd = [P.sb([128, FC, PD], BF16) for _ in range(2)]
    oy = [P.sb([128, PD], F32) for _ in range(4)]
    py = [P.ps([128, 512], F32) for _ in range(4)]
    wdv = Wd.rearrange("(c p) n -> p c n", p=128)
    n = 0
    for pi in range(D // PD):
        b = pi % 2
        h2 = FC // 2
        P.op('pool', lambda e, b=b, pi=pi: e.dma_start(out=wd[b][:, 0:h2, :], in_=wdv[:, 0:h2, pi * PD:(pi + 1) * PD]), writes=[(wd[b], 0)], dma=1)
        P.op('pool', lambda e, b=b, pi=pi: e.dma_start(out=wd[b][:, h2:FC, :], in_=wdv[:, h2:FC, pi * PD:(pi + 1) * PD]), writes=[(wd[b], 1)], dma=1)
        for t in range(ntiles):
            k = n % 4
            n += 1
            for c in range(FC):
                P.op('pe', lambda e, k=k, c=c, b=b, t=t: e.matmul(py[k][:, 0:PD], lhsT=h1T[:, c, t * 128:(t + 1) * 128], rhs=wd[b][:, c, :],
                                                                  start=(c == 0), stop=(c == FC - 1)), reads=[(wd[b], 0), (wd[b], 1)], writes=[py[k]])
            if k % 2 == 0:
                P.op('act', lambda e, k=k: e.copy(out=oy[k][:], in_=py[k][:, 0:PD]), reads=[py[k]], writes=[oy[k]])
            else:
                P.op('dve', lambda e, k=k: e.tensor_copy(out=oy[k][:], in_=py[k][:, 0:PD]), reads=[py[k]], writes=[oy[k]])
            P.op('sp', lambda e, k=k, t=t, pi=pi: e.dma_start(out=y[t * 128:(t + 1) * 128, pi * PD:(pi + 1) * PD], in_=oy[k][:]),
                 reads=[oy[k]], writes=[key_y], dma=1)
    P.end()
    mid.close()


NTB = 4352
NTB_T = 34


def stage_proj(P, uT, Wfm, fm_out, fm_rows, Wtm, tm_out, ntm, ntiles=NTB_T, key_u='uT', KC=16, bias_fm=None, bias_tm=None):
    P.begin()
    NT = ntiles * 128
    ng = len(fm_rows)
    wfm = []
    off = 0
    for gi, rws in enumerate(fm_rows):
        wt = P.sb([128, KC, rws], BF16)
        P.op('pool', lambda e, wt=wt, off=off, rws=rws: e.dma_start(out=wt[:], in_=Wfm.rearrange("(c p) n -> p c n", p=128)[:, :, off:off + rws]),
             writes=[wt], dma=1)
        wfm.append(wt)
        off += rws
    wtm = None
    if ntm:
        wtm = P.sb([128, KC, ntm], BF16)
        P.op('pool', lambda e: e.dma_start(out=wtm[:], in_=Wtm.rearrange("(c p) n -> p c n", p=128)), writes=[wtm], dma=1)
    bfm = None
    one1 = P.sb([128, 1], F32)
    P.op('pool', lambda e: e.memset(one1[:], 1.0), writes=[one1])
    if bias_fm is not None:
        ident = make_ident(P)
        pcl = PCLoader(P, ident, nchunk=ng)
        bfm = P.sb([128, ng], F32)
        pcl.load('sp', bfm, bias_fm, n=ng)
    btm = None
    if bias_tm is not None:
        btm = P.sb([128, ntm], F32)
        load_bc(P, 'act', btm, bias_tm)
    ub = [P.sb([128, KC, 512], BF16) for _ in range(2)]
    ofm = [P.sb([128, 512], F32) for _ in range(4)]
    otm = [P.sb([128, max(ntm, 1)], F32) for _ in range(2)]
    pf = [P.ps([128, 512], F32) for _ in range(4)]
    pt = [P.ps([128, 512], F32) for _ in range(2)]
    nblk = (ntiles + 3) // 4
    n = 0
    ntmc = 0
    for bi in range(nblk):
        tl = min(4, ntiles - bi * 4)
        w = tl * 128
        lo = bi * 512
        u = ub[bi % 2]
        P.op('sp', lambda e, u=u, lo=lo, w=w: e.dma_start(out=u[:, :, 0:w], in_=uT[:, :, lo:lo + w]), reads=[key_u], writes=[u], dma=1)
        for gi, rws in enumerate(fm_rows):
            k = n % 4
            n += 1
            for c in range(KC):
                P.op('pe', lambda e, k=k, c=c, gi=gi, u=u, w=w, rws=rws: e.matmul(pf[k][0:rws, 0:w], lhsT=wfm[gi][:, c, :], rhs=u[:, c, 0:w],
                                                                                 start=(c == 0), stop=(c == KC - 1)), reads=[wfm[gi], u], writes=[pf[k]])
            if bfm is not None:
                P.op('act', lambda e, k=k, w=w, rws=rws, gi=gi: e.activation(out=ofm[k][0:rws, 0:w], in_=pf[k][0:rws, 0:w], func=AF.Identity,
                                                                            bias=bfm[0:rws, gi:gi + 1], scale=one1[0:rws, 0:1]), reads=[pf[k], bfm, one1], writes=[ofm[k]])
            elif k % 2 == 0:
                P.op('act', lambda e, k=k, w=w, rws=rws: e.copy(out=ofm[k][0:rws, 0:w], in_=pf[k][0:rws, 0:w]), reads=[pf[k]], writes=[ofm[k]])
            else:
                P.op('dve', lambda e, k=k, w=w, rws=rws: e.tensor_copy(out=ofm[k][0:rws, 0:w], in_=pf[k][0:rws, 0:w]), reads=[pf[k]], writes=[ofm[k]])
            P.op('sp' if k % 2 == 0 else 'act', lambda e, k=k, w=w, rws=rws, gi=gi, lo=lo: e.dma_start(out=fm_out[gi, 0:rws, lo:lo + w], in_=ofm[k][0:rws, 0:w]),
                 reads=[ofm[k]], writes=['fm_out'], dma=1)
        if ntm:
            for ti in range(tl):
                k = ntmc % 2
                ntmc += 1
                for c in range(KC):
                    P.op('pe', lambda e, k=k, c=c, u=u, ti=ti: e.matmul(pt[k][:, 0:ntm], lhsT=u[:, c, ti * 128:(ti + 1) * 128], rhs=wtm[:, c, :],
                                                                       start=(c == 0), stop=(c == KC - 1)), reads=[wtm, u], writes=[pt[k]])
                if btm is not None:
                    P.op('dve', lambda e, k=k: e.tensor_tensor(out=otm[k][:], in0=pt[k][:, 0:ntm], in1=btm[:], op=ALU.add), reads=[pt[k], btm], writes=[otm[k]])
                else:
                    P.op('dve', lambda e, k=k: e.tensor_copy(out=otm[k][:], in_=pt[k][:, 0:ntm]), reads=[pt[k]], writes=[otm[k]])
                P.op('sp', lambda e, k=k, ti=ti, lo=lo: e.dma_start(out=tm_out[lo + ti * 128:lo + (ti + 1) * 128, :], in_=otm[k][:]),
                     reads=[otm[k]], writes=['tm_out'], dma=1)
    P.end()


SEGS = [(0, 256), (256, NTB)]


def stage_dn_pre(P, pre, convw, fmT, tokm):
    P.begin()
    identb = make_ident(P, BF16)
    ones_b = P.sb([128, 128], BF16)
    P.op('pool', lambda e: e.memset(ones_b[:], 1.0), writes=[ones_b])
    zero1 = P.sb([128, 1], F32)
    P.op('pool', lambda e: e.memset(zero1[:], 0.0), writes=[zero1])
    xin = [P.sb([128, NTB], F32) for _ in range(2)]
    acc = [P.sb([128, NTB], F32) for _ in range(2)]
    cw = P.sb([128, 6, 5], F32)
    P.op('sp', lambda e: e.dma_start(out=cw[:], in_=convw.rearrange("g p j -> p g j")), writes=[cw], dma=1)
    ob = [P.sb([128, NTB], BF16) for _ in range(2)]
    sq = [P.sb([128, 512], BF16) for _ in range(2)]
    rn = [P.sb([128, 512], F32) for _ in range(2)]
    pss = [P.ps([128, 512], F32) for _ in range(2)]
    ptr = [P.ps([128, 1024], BF16) for _ in range(2)]
    tk = [P.sb([128, 8, 128], BF16) for _ in range(2)]
    ntr = 0
    for g in range(6):
        hh, ty = g // 3, g % 3
        b = g % 2
        ve = 'dve'
        P.op('sp', lambda e, g=g, b=b: e.dma_start(out=xin[b][:], in_=pre[g]), writes=[xin[b]], dma=1)
        P.op(ve, lambda e, g=g, b=b: e.tensor_scalar(out=acc[b][:], in0=xin[b][:], scalar1=cw[:, g, 2:3], scalar2=zero1[:, 0:1], op0=ALU.mult, op1=ALU.add),
             reads=[xin[b], cw, zero1], writes=[acc[b]])
        for j in (0, 1, 3, 4):
            d = j - 2
            for (s0, s1) in SEGS:
                a, bb = max(s0, s0 - d), min(s1, s1 - d)
                P.op(ve, lambda e, g=g, b=b, j=j, a=a, bb=bb, d=d: e.scalar_tensor_tensor(
                    out=acc[b][:, a:bb], in0=xin[b][:, a + d:bb + d], scalar=cw[:, g, j:j + 1], in1=acc[b][:, a:bb], op0=ALU.mult, op1=ALU.add),
                    reads=[xin[b], cw, acc[b]], writes=[acc[b]])
        P.op('act', lambda e, b=b: e.activation(out=acc[b][:], in_=acc[b][:], func=AF.Silu), reads=[acc[b]], writes=[acc[b]])
        if ty < 2:
            qs = (128.0 ** -0.5) if ty == 0 else 1.0
            for bi in range((NTB + 511) // 512):
                lo = bi * 512
                w = min(512, NTB - lo)
                k = bi % 2
                P.op('act', lambda e, b=b, k=k, lo=lo, w=w: e.activation(out=sq[k][:, 0:w], in_=acc[b][:, lo:lo + w], func=AF.Square),
                     reads=[acc[b]], writes=[sq[k]])
                P.op('pe', lambda e, k=k, w=w: e.matmul(pss[k][:, 0:w], lhsT=ones_b[:], rhs=sq[k][:, 0:w], start=True, stop=True),
                     reads=[ones_b, sq[k]], writes=[pss[k]])
                P.op('dve', lambda e, k=k, w=w, qs=qs: e.tensor_scalar(out=rn[k][:, 0:w], in0=pss[k][:, 0:w], scalar1=1.0 / (qs * qs), scalar2=EPS / (qs * qs),
                                                               op0=ALU.mult, op1=ALU.add), reads=[pss[k]], writes=[rn[k]])
                P.op('act', lambda e, k=k, w=w: e.activation(out=rn[k][:, 0:w], in_=rn[k][:, 0:w], func=AF.Sqrt), reads=[rn[k]], writes=[rn[k]])
                P.op('dve', lambda e, k=k, w=w: e.reciprocal(out=rn[k][:, 0:w], in_=rn[k][:, 0:w]), reads=[rn[k]], writes=[rn[k]])
                P.op('dve', lambda e, b=b, k=k, lo=lo, w=w: e.tensor_tensor(out=ob[b][:, lo:lo + w], in0=acc[b][:, lo:lo + w], in1=rn[k][:, 0:w], op=ALU.mult),
                     reads=[acc[b], rn[k]], writes=[ob[b]])
            P.op('sp', lambda e, b=b, hh=hh, ty=ty: e.dma_start(out=fmT[hh * 2 + ty], in_=ob[b][:]), reads=[ob[b]], writes=['fmT'], dma=1)
        else:
            P.op('dve', lambda e, b=b: e.tensor_copy(out=ob[b][:], in_=acc[b][:]), reads=[acc[b]], writes=[ob[b]])
        if ty >= 1:
            for t8 in range((NTB_T + 7) // 8):
                nt = min(8, NTB_T - t8 * 8)
                pp = ptr[ntr % 2]
                tt = tk[ntr % 2]
                ntr += 1
                for i in range(nt):
                    t = t8 * 8 + i
                    P.op('pe', lambda e, pp=pp, i=i, t=t, b=b: e.transpose(out=pp[:, i * 128:(i + 1) * 128], in_=ob[b][:, t * 128:(t + 1) * 128], identity=identb[:]),
                         reads=[ob[b], identb], writes=[pp])
                P.op('act' if t8 % 2 == 0 else 'dve', lambda e, pp=pp, tt=tt, nt=nt, t8=t8: (e.copy if t8 % 2 == 0 else e.tensor_copy)(
                    out=tt[:, 0:nt, :], in_=pp[:, 0:nt * 128].rearrange("p (a b) -> p a b", b=128)), reads=[pp], writes=[tt])
                P.op('sp', lambda e, tt=tt, nt=nt, t8=t8, hh=hh, ty=ty: e.dma_start(
                    out=tokm[hh * 2 + ty - 1, t8 * 1024:t8 * 1024 + nt * 128, :].rearrange("(a p) d -> p a d", p=128), in_=tt[:, 0:nt, :]),
                    reads=[tt], writes=['tokm'], dma=1)
    P.end()


def tri_mask(P, kind):
    m = P.sb([128, 128], F32)
    P.op('pool', lambda e: e.memset(m[:], 1.0), writes=[m])
    if kind in ('ge', 'gt'):
        pat, cm = [[-1, 128]], 1
    else:
        pat, cm = [[1, 128]], -1
    op = ALU.is_ge if kind in ('ge', 'le') else ALU.is_gt
    P.op('pool', lambda e: e.affine_select(out=m[:], in_=m[:], pattern=pat, compare_op=op, fill=0.0, base=0, channel_multiplier=cm),
         reads=[m], writes=[m])
    return m


def stage_dn_scan(P, fmT, tokm, tm, alog8, dtb8, dng, dnout, ZC=256):
    P.begin()
    NTt = NTB_T
    identf = make_ident(P)
    zero1 = P.sb([128, 1], F32)
    negone1 = P.sb([128, 1], F32)
    one1 = P.sb([128, 1], F32)
    ones_f = P.sb([128, 128], F32)
    P.op('pool', lambda e: e.memset(zero1[:], 0.0), writes=[zero1])
    P.op('pool', lambda e: e.memset(negone1[:], -1.0), writes=[negone1])
    P.op('pool', lambda e: e.memset(one1[:], 1.0), writes=[one1])
    P.op('pool', lambda e: e.memset(ones_f[:], 1.0), writes=[ones_f])
    L = {0: tri_mask(P, 'le'), 1: tri_mask(P, 'ge')}
    SM = {0: tri_mask(P, 'gt'), 1: tri_mask(P, 'lt')}
    qT = [P.sb([128, NTB], BF16) for _ in range(2)]
    kT = [P.sb([128, NTB], BF16) for _ in range(2)]
    kt = [P.sb([128, NTt, 128], BF16) for _ in range(2)]
    vt = [P.sb([128, NTt, 128], BF16) for _ in range(2)]
    for hh in range(2):
        P.op('sp', lambda e, hh=hh: e.dma_start(out=qT[hh][:], in_=fmT[hh * 2]), writes=[qT[hh]], dma=1)
        P.op('act', lambda e, hh=hh: e.dma_start(out=kT[hh][:], in_=fmT[hh * 2 + 1]), writes=[kT[hh]], dma=1)
        P.op('sp', lambda e, hh=hh: e.dma_start(out=kt[hh][:], in_=tokm[hh * 2].rearrange("(a p) d -> p a d", p=128)), writes=[kt[hh]], dma=1)
        P.op('act', lambda e, hh=hh: e.dma_start(out=vt[hh][:], in_=tokm[hh * 2 + 1].rearrange("(a p) d -> p a d", p=128)), writes=[vt[hh]], dma=1)
    gt = P.sb([128, NTt, 8], F32)
    for t in range(NTt):
        P.op('sp' if t % 2 == 0 else 'act', lambda e, t=t: e.dma_start(out=gt[:, t, :], in_=tm[t * 128:(t + 1) * 128, ZC:ZC + 8]), writes=[(gt, t)], dma=1)
    allg = [(gt, t) for t in range(NTt)]
    al = P.sb([128, 8], F32)
    db = P.sb([128, 8], F32)
    load_bc(P, 'sp', al, alog8)
    load_bc(P, 'sp', db, dtb8)
    P.op('act', lambda e: e.activation(out=al[:], in_=al[:], func=AF.Exp), reads=[al], writes=[al])
    P.op('dve', lambda e: e.tensor_scalar(out=al[:], in0=al[:], scalar1=negone1[:, 0:1], scalar2=zero1[:, 0:1], op0=ALU.mult, op1=ALU.add),
         reads=[al, negone1, zero1], writes=[al])
    beta, nbeta, gg = {}, {}, {}
    for hh in range(2):
        for d in range(2):
            cb, cg = hh * 4 + d, hh * 4 + 2 + d
            bt = P.sb([128, NTt], F32)
            nb = P.sb([128, NTt], F32)
            gx = P.sb([128, NTt], F32)
            P.op('act', lambda e, bt=bt, cb=cb: e.activation(out=bt[:], in_=gt[:, :, cb], func=AF.Sigmoid), reads=allg, writes=[bt])
            P.op('dve', lambda e, bt=bt, nb=nb: e.tensor_scalar(out=nb[:], in0=bt[:], scalar1=negone1[:, 0:1], scalar2=zero1[:, 0:1], op0=ALU.mult, op1=ALU.add),
                 reads=[bt, negone1, zero1], writes=[nb])
            P.op('dve', lambda e, gx=gx, cg=cg: e.tensor_scalar(out=gx[:], in0=gt[:, :, cg], scalar1=db[:, cg:cg + 1], scalar2=zero1[:, 0:1], op0=ALU.add, op1=ALU.add),
                 reads=allg + [db, zero1], writes=[gx])
            P.op('act', lambda e, gx=gx: e.activation(out=gx[:], in_=gx[:], func=AF.Exp), reads=[gx], writes=[gx])
            P.op('act', lambda e, gx=gx: e.activation(out=gx[:], in_=gx[:], func=AF.Ln, bias=one1[:, 0:1], scale=one1[:, 0:1]), reads=[gx, one1], writes=[gx])
            P.op('dve', lambda e, gx=gx, cg=cg: e.tensor_scalar(out=gx[:], in0=gx[:], scalar1=al[:, cg:cg + 1], scalar2=zero1[:, 0:1], op0=ALU.mult, op1=ALU.add),
                 reads=[gx, al, zero1], writes=[gx])
            beta[(hh, d)], nbeta[(hh, d)], gg[(hh, d)] = bt, nb, gx
    bankP_ = {}
    bankS_ = {}
    for hh_ in range(2):
        for d_ in range(2):
            bankP_[(hh_, d_)] = P.ps([128, 512], F32)
            bankS_[(hh_, d_)] = P.ps([128, 512], F32)
    o_acc = [P.sb([128, NTt, 128], F32) for _ in range(2)]
    written = set()
    seqs = [(hh, d) for hh in range(2) for d in range(2)]
    order = {0: list(range(NTt)), 1: [1, 0] + list(range(NTt - 1, 1, -1))}
    S32, Sbf = {}, {}
    for sq_ in seqs:
        S32[sq_] = P.sb([128, 128], F32)
        Sbf[sq_] = S32[sq_]
        P.op('pool', lambda e, sq_=sq_: e.memset(S32[sq_][:], 0.0), writes=[S32[sq_]])

    def ring(shape, dt):
        return {sq_: [P.sb(shape, dt) for _ in range(2)] for sq_ in seqs}
    gbc_r = ring([128, 128], F32)
    gcs_r = ring([128, 130], F32)
    F_r = ring([128, 128], F32)
    FM_r = ring([128, 256], F32)
    Nb_r = ring([128, 2, 128], F32)
    Nb2_r = ring([128, 2, 128], F32)
    QK_r = ring([128, 128], F32)
    R_r = ring([128, 2, 256], F32)
    sc_r = ring([128, 4], F32)
    eg_r = ring([128, 128], F32)
    qd_r = ring([128, 128], F32)
    kd_r = ring([128, 128], F32)
    wT_r = ring([128, 128], F32)
    vn_r = ring([128, 128], F32)

    class _Y:
        def __init__(self):
            self.q = []
        def op(self, *a, **k):
            self.q.append((a, k))

    def _interleave(lists):
        n = max(len(l) for l in lists)
        for i in range(n):
            for l in lists:
                if i < len(l):
                    a, k = l[i]
                    P.op(*a, **k)

    def precompute(sq_, s, P=None):
        hh, d = sq_
        t = order[d][s]
        r = s % 2
        cs = slice(t * 128, (t + 1) * 128)
        gcol = gg[sq_][:, t:t + 1]
        gbc, gcs, Ft, FM, Nb, Nb2, QK, R, sc, eg, qd, kd, wT = (gbc_r[sq_][r], gcs_r[sq_][r], F_r[sq_][r], FM_r[sq_][r], Nb_r[sq_][r], Nb2_r[sq_][r],
                                                                QK_r[sq_][r], R_r[sq_][r], sc_r[sq_][r], eg_r[sq_][r], qd_r[sq_][r], kd_r[sq_][r], wT_r[sq_][r])
        lastc = 127 if d == 0 else 0
        bp = bankP_[sq_]
        b_cs = bp[:, 0:129]
        b_kk = bp[:, 256:384]
        b_kq = bp[:, 384:512]
        b_trN = bp[:, 0:128]
        b_sq = bp[:, 0:256]
        b_ap = bp[:, 256:512]
        P.op('dve', lambda e: e.tensor_scalar(out=gbc[:], in0=ones_f[:], scalar1=gcol, scalar2=zero1[:, 0:1], op0=ALU.mult, op1=ALU.add),
             reads=[ones_f, gg[sq_], zero1], writes=[gbc])
        P.op('pe', lambda e: e.matmul(bp[:, 0:128], lhsT=gbc[:], rhs=L[d][:], start=True, stop=True), reads=[gbc, L[d]], writes=[bp])
        P.op('pe', lambda e: e.matmul(bp[:, 128:129], lhsT=L[d][:], rhs=gcol, start=True, stop=True), reads=[gg[sq_], L[d]], writes=[bp])
        P.op('dve', lambda e: e.tensor_copy(out=gcs[:, 0:129], in_=b_cs), reads=[bp], writes=[gcs])
        P.op('dve', lambda e: e.tensor_scalar(out=Ft[:], in0=gcs[:, 0:128], scalar1=gcs[:, 128:129], scalar2=zero1[:, 0:1], op0=ALU.subtract, op1=ALU.add),
             reads=[gcs, zero1], writes=[Ft])
        P.op('act', lambda e: e.activation(out=Ft[:], in_=Ft[:], func=AF.Abs), reads=[Ft], writes=[Ft])
        P.op('act', lambda e: e.activation(out=Ft[:], in_=Ft[:], func=AF.Exp, scale=negone1[:, 0:1], bias=zero1[:, 0:1]), reads=[Ft, negone1, zero1], writes=[Ft])
        P.op('pool', lambda e: e.tensor_tensor(out=FM[:, 0:128], in0=Ft[:], in1=SM[d][:], op=ALU.mult), reads=[Ft, SM[d]], writes=[(FM, 0)])
        P.op('pool', lambda e: e.tensor_tensor(out=FM[:, 128:256], in0=Ft[:], in1=L[d][:], op=ALU.mult), reads=[Ft, L[d]], writes=[(FM, 1)])
        P.op('act', lambda e: e.activation(out=eg[:], in_=gcs[:, 0:128], func=AF.Exp), reads=[gcs], writes=[eg])
        P.op('act', lambda e: e.activation(out=sc[:, 0:1], in_=gcs[:, 128:129], func=AF.Exp), reads=[gcs], writes=[(sc, 0)])
        P.op('dve', lambda e: e.tensor_tensor(out=sc[:, 0:1], in0=sc[:, 0:1], in1=beta[sq_][:, t:t + 1], op=ALU.mult), reads=[(sc, 0), beta[sq_]], writes=[(sc, 0)])
        P.op('act', lambda e: e.activation(out=sc[:, 1:2], in_=gcs[:, 128:129], func=AF.Exp, scale=negone1[:, 0:1], bias=gcs[:, lastc:lastc + 1]),
             reads=[gcs, negone1], writes=[(sc, 1)])
        P.op('act', lambda e: e.activation(out=sc[:, 2:3], in_=gcs[:, lastc:lastc + 1], func=AF.Exp), reads=[gcs], writes=[(sc, 2)])
        P.op('pe', lambda e: e.matmul(b_kk, lhsT=kT[hh][:, cs], rhs=kT[hh][:, cs], start=True, stop=True), reads=[kT[hh]], writes=[bp])
        P.op('pe', lambda e: e.matmul(b_kq, lhsT=kT[hh][:, cs], rhs=qT[hh][:, cs], start=True, stop=True), reads=[kT[hh], qT[hh]], writes=[bp])
        P.op('dve', lambda e: e.scalar_tensor_tensor(out=Nb[:, 0, :], in0=b_kk, scalar=nbeta[sq_][:, t:t + 1], in1=FM[:, 0:128], op0=ALU.mult, op1=ALU.mult),
             reads=[bp, nbeta[sq_], (FM, 0)], writes=[(Nb, 0)])
        P.op('dve', lambda e: e.tensor_tensor(out=QK[:], in0=b_kq, in1=FM[:, 128:256], op=ALU.mult), reads=[bp, (FM, 1)], writes=[QK])
        P.op('pe', lambda e: e.transpose(out=b_trN, in_=Nb[:, 0, :], identity=identf[:]), reads=[(Nb, 0), identf], writes=[bp])
        P.op('act', lambda e: e.copy(out=Nb[:, 1, :], in_=b_trN), reads=[bp], writes=[(Nb, 1)])
        P.op('pool', lambda e: e.tensor_scalar(out=R[:, 0, 0:128], in0=vt[hh][:, t, :], scalar1=beta[sq_][:, t:t + 1], scalar2=zero1[:, 0:1], op0=ALU.mult, op1=ALU.add),
             reads=[vt[hh], beta[sq_], zero1], writes=[(R, 0)])
        P.op('pool', lambda e: e.tensor_scalar(out=R[:, 0, 128:256], in0=kt[hh][:, t, :], scalar1=sc[:, 0:1], scalar2=zero1[:, 0:1], op0=ALU.mult, op1=ALU.add),
             reads=[kt[hh], (sc, 0), zero1], writes=[(R, 0)])
        cur, nxt = Nb, Nb2
        ri = 0
        for k in range(7):
            P.op('pe', lambda e, cur=cur, ri=ri: e.matmul(b_ap, lhsT=cur[:, 1, :], rhs=R[:, ri, :], start=True, stop=True),
                 reads=[(cur, 1), (R, ri)], writes=[bp])
            P.op('dve', lambda e, ri=ri: e.tensor_tensor(out=R[:, 1 - ri, :], in0=b_ap, in1=R[:, ri, :], op=ALU.add),
                 reads=[bp, (R, ri)], writes=[(R, 1 - ri)])
            ri = 1 - ri
            if k < 6:
                P.op('pe', lambda e, cur=cur: e.matmul(bp[:, 0:128], lhsT=cur[:, 1, :], rhs=cur[:, 0, :], start=True, stop=True),
                     reads=[(cur, 0), (cur, 1)], writes=[bp])
                P.op('pe', lambda e, cur=cur: e.matmul(bp[:, 128:256], lhsT=cur[:, 0, :], rhs=cur[:, 1, :], start=True, stop=True),
                     reads=[(cur, 0), (cur, 1)], writes=[bp])
                P.op('act', lambda e, nxt=nxt: e.copy(out=nxt[:, :, :], in_=b_sq.rearrange("p (a b) -> p a b", a=2)), reads=[bp], writes=[(nxt, 0), (nxt, 1)])
                cur, nxt = nxt, cur
        P.op('pe', lambda e, ri=ri: e.transpose(out=b_trN, in_=R[:, ri, 128:256], identity=identf[:]), reads=[(R, ri), identf], writes=[bp])
        P.op('act', lambda e: e.copy(out=wT[:], in_=b_trN), reads=[bp], writes=[wT])
        P.op('pool', lambda e: e.tensor_tensor(out=qd[:], in0=qT[hh][:, cs], in1=eg[:], op=ALU.mult), reads=[qT[hh], eg], writes=[qd])
        P.op('pool', lambda e: e.tensor_scalar(out=kd[:], in0=kt[hh][:, t, :], scalar1=sc[:, 1:2], scalar2=zero1[:, 0:1], op0=ALU.mult, op1=ALU.add),
             reads=[kt[hh], (sc, 1), zero1], writes=[kd])
        return ri

    def scan(sq_, s, ri, P=None):
        hh, d = sq_
        t = order[d][s]
        r = s % 2
        R, sc, qd, kd, wT, QK, vn = R_r[sq_][r], sc_r[sq_][r], qd_r[sq_][r], kd_r[sq_][r], wT_r[sq_][r], QK_r[sq_][r], vn_r[sq_][r]
        bs = bankS_[sq_]
        b_s1 = bs
        b_s2 = bs[:, 256:384]
        P.op('pe', lambda e: e.matmul(b_s1[:, 0:128], lhsT=wT[:], rhs=Sbf[sq_][:], start=True, stop=True), reads=[wT, Sbf[sq_]], writes=[bs])
        P.op('dve', lambda e: e.tensor_tensor(out=vn[:], in0=R[:, ri, 0:128], in1=b_s1[:, 0:128], op=ALU.subtract), reads=[(R, ri), bs], writes=[vn])
        P.op('pe', lambda e: e.matmul(b_s2, lhsT=qd[:], rhs=Sbf[sq_][:], start=True, stop=False), reads=[qd, Sbf[sq_]], writes=[bs])
        P.op('pe', lambda e: e.matmul(b_s2, lhsT=QK[:], rhs=vn[:], start=False, stop=True), reads=[QK, vn], writes=[bs])
        key = (hh, t)
        if key not in written:
            written.add(key)
            P.op('act', lambda e: e.copy(out=o_acc[hh][:, t, :], in_=b_s2), reads=[bs], writes=[(o_acc[hh], t)])
        else:
            P.op('dve', lambda e: e.tensor_tensor(out=o_acc[hh][:, t, :], in0=b_s2, in1=o_acc[hh][:, t, :], op=ALU.add),
                 reads=[bs, (o_acc[hh], t)], writes=[(o_acc[hh], t)])
        P.op('pe', lambda e: e.matmul(b_s1[:, 128:256], lhsT=kd[:], rhs=vn[:], start=True, stop=True), reads=[kd, vn], writes=[bs])
        P.op('dve', lambda e: e.scalar_tensor_tensor(out=S32[sq_][:], in0=S32[sq_][:], scalar=sc[:, 2:3], in1=b_s1[:, 128:256], op0=ALU.mult, op1=ALU.add),
             reads=[S32[sq_], (sc, 2), bs], writes=[S32[sq_]])

    ris = {}
    ys = []
    for sq_ in seqs:
        y = _Y()
        ris[(sq_, 0)] = precompute(sq_, 0, P=y)
        ys.append(y.q)
    _interleave(ys)
    for s in range(NTt):
        ys = []
        for sq_ in seqs:
            y = _Y()
            if s + 1 < NTt:
                ris[(sq_, s + 1)] = precompute(sq_, s + 1, P=y)
            ys.append(y.q)
        ys2 = []
        for sq_ in seqs:
            y = _Y()
            scan(sq_, s, ris[(sq_, s)], P=y)
            ys2.append(y.q)
        _interleave([a + b for a, b in zip(ys2, ys)] if False else ys2 + ys)
    gb = P.sb([128, 128], F32)
    load_bc(P, 'sp', gb, dng)
    zt = [P.sb([128, 256], F32) for _ in range(2)]
    ot = [P.sb([128, 256], F32) for _ in range(2)]
    junk = P.sb([128, 128], F32)
    ss = [P.sb([128, 1], F32) for _ in range(4)]
    rs = [P.sb([128, 1], F32) for _ in range(4)]
    for t in range(NTt):
        b = t % 2
        P.op('sp', lambda e, t=t, b=b: e.dma_start(out=zt[b][:], in_=tm[t * 128:(t + 1) * 128, 0:256]), writes=[zt[b]], dma=1)
        P.op('act', lambda e, b=b: e.activation(out=zt[b][:], in_=zt[b][:], func=AF.Silu), reads=[zt[b]], writes=[zt[b]])
        for hh in range(2):
            k = b * 2 + hh
            P.op('act', lambda e, hh=hh, t=t, k=k: e.activation(out=junk[:], in_=o_acc[hh][:, t, :], func=AF.Square, accum_out=ss[k][:]),
                 reads=[(o_acc[hh], t)], writes=[junk, ss[k]])
            rstd_from_ss(P, ss[k], rs[k], 128)
            P.op('dve', lambda e, hh=hh, t=t, k=k, b=b: e.scalar_tensor_tensor(out=ot[b][:, hh * 128:(hh + 1) * 128], in0=o_acc[hh][:, t, :], scalar=rs[k][:, 0:1],
                                                                             in1=gb[:], op0=ALU.mult, op1=ALU.mult), reads=[(o_acc[hh], t), rs[k], gb], writes=[(ot[b], hh)])
            P.op('pool', lambda e, hh=hh, b=b: e.tensor_tensor(out=ot[b][:, hh * 128:(hh + 1) * 128], in0=ot[b][:, hh * 128:(hh + 1) * 128],
                                                              in1=zt[b][:, hh * 128:(hh + 1) * 128], op=ALU.mult), reads=[(ot[b], hh), zt[b]], writes=[(ot[b], hh)])
        P.op('sp', lambda e, t=t, b=b: e.dma_start(out=dnout[t * 128:(t + 1) * 128, :], in_=ot[b][:]), reads=[(ot[b], 0), (ot[b], 1)], writes=['dnout'], dma=1)
    P.end()


def stage_mla_proj(P, fm, qg, kvg, wq, wkv, cosT, ssT, qkT, krT, vtok):
    P.begin()
    ident = make_ident(P)
    pcl = PCLoader(P, ident, nchunk=6)
    g6 = P.sb([128, 6], F32)
    g4 = P.sb([128, 6], F32)
    pcl.load('sp', g6, qg, n=6)
    pcl.load('sp', g4, kvg, n=4)
    ones_b = P.sb([128, 128], BF16)
    P.op('pool', lambda e: e.memset(ones_b[:], 1.0), writes=[ones_b])
    wqs = P.sb([128, 6, 512], BF16)
    wkvs = P.sb([128, 4, 512], BF16)
    P.op('pool', lambda e: e.dma_start(out=wqs[:], in_=wq.rearrange("(c p) n -> p c n", p=128)), writes=[wqs], dma=1)
    P.op('pool', lambda e: e.dma_start(out=wkvs[:], in_=wkv.rearrange("(c p) n -> p c n", p=128)), writes=[wkvs], dma=1)
    cqn = P.sb([128, 6, NTB], BF16)
    ckn = P.sb([128, 4, NTB], BF16)
    xin = [P.sb([128, 6, 512], F32) for _ in range(2)]
    sq = [P.sb([128, 6, 512], BF16) for _ in range(2)]
    rn = [P.sb([128, 512], F32) for _ in range(2)]
    pss = [P.ps([128, 512], F32) for _ in range(2)]
    nb = 0
    NBLK = (NTB + 511) // 512
    for (base, nch, dst, gcol, dim) in ((0, 6, cqn, g6, 768.0), (6, 4, ckn, g4, 512.0)):
        for bi in range(NBLK):
            lo = bi * 512
            w = min(512, NTB - lo)
            k = nb % 2
            nb += 1
            P.op('sp', lambda e, k=k, base=base, nch=nch, lo=lo, w=w: e.dma_start(out=xin[k][:, 0:nch, 0:w], in_=fm[base:base + nch, :, lo:lo + w].rearrange("c p t -> p c t")),
                 writes=[xin[k]], dma=1)
            P.op('act', lambda e, k=k, nch=nch, w=w: e.activation(out=sq[k][:, 0:nch, 0:w], in_=xin[k][:, 0:nch, 0:w], func=AF.Square), reads=[xin[k]], writes=[sq[k]])
            for c in range(nch):
                P.op('pe', lambda e, k=k, c=c, w=w, nch=nch: e.matmul(pss[k][:, 0:w], lhsT=ones_b[:], rhs=sq[k][:, c, 0:w], start=(c == 0), stop=(c == nch - 1)),
                     reads=[ones_b, sq[k]], writes=[pss[k]])
            P.op('dve', lambda e, k=k, w=w, dim=dim: e.tensor_scalar(out=rn[k][:, 0:w], in0=pss[k][:, 0:w], scalar1=1.0 / dim, scalar2=EPS, op0=ALU.mult, op1=ALU.add),
                 reads=[pss[k]], writes=[rn[k]])
            P.op('act', lambda e, k=k, w=w: e.activation(out=rn[k][:, 0:w], in_=rn[k][:, 0:w], func=AF.Sqrt), reads=[rn[k]], writes=[rn[k]])
            P.op('dve', lambda e, k=k, w=w: e.reciprocal(out=rn[k][:, 0:w], in_=rn[k][:, 0:w]), reads=[rn[k]], writes=[rn[k]])
            for c in range(nch):
                P.op('dve', lambda e, k=k, c=c, w=w, lo=lo, dst=dst, gcol=gcol: e.scalar_tensor_tensor(
                    out=dst[:, c, lo:lo + w], in0=xin[k][:, c, 0:w], scalar=gcol[:, c:c + 1], in1=rn[k][:, 0:w], op0=ALU.mult, op1=ALU.mult),
                    reads=[xin[k], gcol, rn[k]], writes=[(dst, bi)])
    cs = P.sb([64, NTB], F32)
    sn = P.sb([64, NTB], F32)
    P.op('sp', lambda e: e.dma_start(out=cs[:], in_=cosT), writes=[cs], dma=1)
    P.op('act', lambda e: e.dma_start(out=sn[:], in_=ssT), writes=[sn], dma=1)
    pj = [P.ps([128, 512], F32) for _ in range(4)]
    ob = [P.sb([128, 512], BF16) for _ in range(4)]
    t1 = [P.sb([64, 512], F32) for _ in range(2)]
    t2 = [P.sb([64, 512], F32) for _ in range(2)]
    krs = [P.sb([64, 2, 512], F32) for _ in range(2)]
    n = 0
    nr = 0
    for bi in range(NBLK):
        lo = bi * 512
        w = min(512, NTB - lo)
        kk = bi % 2
        P.op('sp', lambda e, kk=kk, lo=lo, w=w: e.dma_start(out=krs[kk][:, :, 0:w], in_=fm[10:12, 0:64, lo:lo + w].rearrange("c p t -> p c t")), writes=[krs[kk]], dma=1)
        r = nr % 2
        nr += 1
        P.op('dve', lambda e, kk=kk, r=r, lo=lo, w=w: e.tensor_tensor(out=t1[r][:, 0:w], in0=krs[kk][:, 0, 0:w], in1=cs[:, lo:lo + w], op=ALU.mult), reads=[krs[kk], cs], writes=[t1[r]])
        P.op('pool', lambda e, kk=kk, r=r, lo=lo, w=w: e.tensor_tensor(out=t2[r][:, 0:w], in0=krs[kk][:, 1, 0:w], in1=sn[:, lo:lo + w], op=ALU.mult), reads=[krs[kk], sn], writes=[t2[r]])
        k = n % 4
        n += 1
        P.op('dve', lambda e, k=k, r=r, w=w: e.tensor_tensor(out=ob[k][0:64, 0:w], in0=t1[r][:, 0:w], in1=t2[r][:, 0:w], op=ALU.add), reads=[t1[r], t2[r]], writes=[ob[k]])
        P.op('sp', lambda e, k=k, lo=lo, w=w: e.dma_start(out=krT[:, lo:lo + w], in_=ob[k][0:64, 0:w]), reads=[ob[k]], writes=['krT'], dma=1)
        for hh in range(2):
            k = n % 4
            n += 1
            for c in range(6):
                P.op('pe', lambda e, k=k, c=c, hh=hh, lo=lo, w=w: e.matmul(pj[k][:, 0:w], lhsT=wqs[:, c, hh * 128:(hh + 1) * 128], rhs=cqn[:, c, lo:lo + w], start=(c == 0), stop=(c == 5)),
                     reads=[wqs, (cqn, bi)], writes=[pj[k]])
            P.op('act', lambda e, k=k, w=w: e.copy(out=ob[k][:, 0:w], in_=pj[k][:, 0:w]), reads=[pj[k]], writes=[ob[k]])
            P.op('sp', lambda e, k=k, hh=hh, lo=lo, w=w: e.dma_start(out=qkT[hh, 0, :, lo:lo + w], in_=ob[k][:, 0:w]), reads=[ob[k]], writes=['qkT'], dma=1)
            k1 = n % 4
            n += 1
            k2 = n % 4
            n += 1
            for c in range(6):
                P.op('pe', lambda e, k1=k1, c=c, hh=hh, lo=lo, w=w: e.matmul(pj[k1][0:64, 0:w], lhsT=wqs[:, c, 256 + hh * 64:256 + (hh + 1) * 64], rhs=cqn[:, c, lo:lo + w], start=(c == 0), stop=(c == 5)),
                     reads=[wqs, (cqn, bi)], writes=[pj[k1]])
            for c in range(6):
                P.op('pe', lambda e, k2=k2, c=c, hh=hh, lo=lo, w=w: e.matmul(pj[k2][0:64, 0:w], lhsT=wqs[:, c, 384 + hh * 64:384 + (hh + 1) * 64], rhs=cqn[:, c, lo:lo + w], start=(c == 0), stop=(c == 5)),
                     reads=[wqs, (cqn, bi)], writes=[pj[k2]])
            r = nr % 2
            nr += 1
            P.op('dve', lambda e, k1=k1, r=r, lo=lo, w=w: e.tensor_tensor(out=t1[r][:, 0:w], in0=pj[k1][0:64, 0:w], in1=cs[:, lo:lo + w], op=ALU.mult), reads=[pj[k1], cs], writes=[t1[r]])
            P.op('dve', lambda e, k2=k2, r=r, lo=lo, w=w: e.tensor_tensor(out=t2[r][:, 0:w], in0=pj[k2][0:64, 0:w], in1=sn[:, lo:lo + w], op=ALU.mult), reads=[pj[k2], sn], writes=[t2[r]])
            P.op('pool', lambda e, k1=k1, r=r, w=w: e.tensor_tensor(out=ob[k1][0:64, 0:w], in0=t1[r][:, 0:w], in1=t2[r][:, 0:w], op=ALU.add), reads=[t1[r], t2[r]], writes=[ob[k1]])
            P.op('sp', lambda e, k1=k1, hh=hh, lo=lo, w=w: e.dma_start(out=qkT[hh, 1, 0:64, lo:lo + w], in_=ob[k1][0:64, 0:w]), reads=[ob[k1]], writes=['qkT'], dma=1)
            k = n % 4
            n += 1
            for c in range(4):
                P.op('pe', lambda e, k=k, c=c, hh=hh, lo=lo, w=w: e.matmul(pj[k][:, 0:w], lhsT=wkvs[:, c, hh * 128:(hh + 1) * 128], rhs=ckn[:, c, lo:lo + w], start=(c == 0), stop=(c == 3)),
                     reads=[wkvs, (ckn, bi)], writes=[pj[k]])
            P.op('act', lambda e, k=k, w=w: e.copy(out=ob[k][:, 0:w], in_=pj[k][:, 0:w]), reads=[pj[k]], writes=[ob[k]])
            P.op('sp', lambda e, k=k, hh=hh, lo=lo, w=w: e.dma_start(out=qkT[hh, 2, :, lo:lo + w], in_=ob[k][:, 0:w]), reads=[ob[k]], writes=['qkT'], dma=1)
            for ti in range(w // 128):
                k = n % 4
                n += 1
                for c in range(4):
                    P.op('pe', lambda e, k=k, c=c, hh=hh, lo=lo, ti=ti: e.matmul(pj[k][:, 0:128], lhsT=ckn[:, c, lo + ti * 128:lo + (ti + 1) * 128], rhs=wkvs[:, c, 256 + hh * 128:256 + (hh + 1) * 128],
                                                                                start=(c == 0), stop=(c == 3)), reads=[wkvs, (ckn, bi)], writes=[pj[k]])
                P.op('dve', lambda e, k=k: e.tensor_copy(out=ob[k][:, 0:128], in_=pj[k][:, 0:128]), reads=[pj[k]], writes=[ob[k]])
                P.op('act', lambda e, k=k, hh=hh, lo=lo, ti=ti: e.dma_start(out=vtok[hh, lo + ti * 128:lo + (ti + 1) * 128, :], in_=ob[k][:, 0:128]), reads=[ob[k]], writes=['vtok'], dma=1)
    P.end()


def stage_mla_attn(P, qkT, krT, vtok, oT, scale):
    P.begin()
    ones_b = P.sb([128, 128], BF16)
    P.op('pool', lambda e: e.memset(ones_b[:], 1.0), writes=[ones_b])
    kr = P.sb([64, NTB], BF16)
    P.op('sp', lambda e: e.dma_start(out=kr[:], in_=krT), writes=[kr], dma=1)
    ps_s = [P.ps([128, 512], F32) for _ in range(3)]
    ps_o = [P.ps([128, 512], F32) for _ in range(2)]
    ps_d = [P.ps([128, 512], F32) for _ in range(2)]
    pt = [P.sb([128, 512], BF16) for _ in range(3)]
    rinv = [P.sb([128, 512], F32) for _ in range(2)]
    osb = [P.sb([128, 512], BF16) for _ in range(2)]
    n = 0
    nq = 0
    for hh in range(2):
        qn = P.sb([128, NTB], BF16)
        qr = P.sb([64, NTB], BF16)
        kn = P.sb([128, NTB], BF16)
        vt = P.sb([128, NTB_T, 128], BF16)
        P.op('sp', lambda e, hh=hh, qn=qn: e.dma_start(out=qn[:], in_=qkT[hh, 0]), writes=[qn], dma=1)
        P.op('act', lambda e, hh=hh, qr=qr: e.dma_start(out=qr[:], in_=qkT[hh, 1, 0:64, :]), writes=[qr], dma=1)
        P.op('sp', lambda e, hh=hh, kn=kn: e.dma_start(out=kn[:], in_=qkT[hh, 2]), writes=[kn], dma=1)
        P.op('act', lambda e, hh=hh, vt=vt: e.dma_start(out=vt[:], in_=vtok[hh].rearrange("(a p) d -> p a d", p=128)), writes=[vt], dma=1)
        qblocks = [(0, 256, 2)] + [(256 + i * 512, 512, NTB_T) for i in range(8)]
        for (qlo, qw, nkt) in qblocks:
            a = nq % 2
            nq += 1
            for kt in range(nkt):
                k = n % 3
                n += 1
                ks = slice(kt * 128, (kt + 1) * 128)
                P.op('pe', lambda e, k=k, ks=ks, qlo=qlo, qw=qw, kn=kn, qn=qn: e.matmul(ps_s[k][:, 0:qw], lhsT=kn[:, ks], rhs=qn[:, qlo:qlo + qw], start=True, stop=False),
                     reads=[kn, qn], writes=[ps_s[k]])
                P.op('pe', lambda e, k=k, ks=ks, qlo=qlo, qw=qw, qr=qr: e.matmul(ps_s[k][:, 0:qw], lhsT=kr[:, ks], rhs=qr[:, qlo:qlo + qw], start=False, stop=True),
                     reads=[kr, qr], writes=[ps_s[k]])
                P.op('act', lambda e, k=k, qw=qw: e.activation(out=pt[k][:, 0:qw], in_=ps_s[k][:, 0:qw], func=AF.Exp, scale=float(scale)), reads=[ps_s[k]], writes=[pt[k]])
                P.op('pe', lambda e, k=k, a=a, kt=kt, qw=qw, nkt=nkt, vt=vt: e.matmul(ps_o[a][:, 0:qw], lhsT=vt[:, kt, :], rhs=pt[k][:, 0:qw], start=(kt == 0), stop=(kt == nkt - 1)),
                     reads=[vt, pt[k]], writes=[ps_o[a]])
                P.op('pe', lambda e, k=k, a=a, kt=kt, qw=qw, nkt=nkt: e.matmul(ps_d[a][:, 0:qw], lhsT=ones_b[:], rhs=pt[k][:, 0:qw], start=(kt == 0), stop=(kt == nkt - 1)),
                     reads=[ones_b, pt[k]], writes=[ps_d[a]])
            P.op('dve', lambda e, a=a, qw=qw: e.reciprocal(out=rinv[a][:, 0:qw], in_=ps_d[a][:, 0:qw]), reads=[ps_d[a]], writes=[rinv[a]])
            P.op('dve', lambda e, a=a, qw=qw: e.tensor_tensor(out=osb[a][:, 0:qw], in0=ps_o[a][:, 0:qw], in1=rinv[a][:, 0:qw], op=ALU.mult), reads=[ps_o[a], rinv[a]], writes=[osb[a]])
            P.op('sp', lambda e, a=a, hh=hh, qlo=qlo, qw=qw: e.dma_start(out=oT[hh, :, qlo:qlo + qw], in_=osb[a][:, 0:qw]), reads=[osb[a]], writes=['oT'], dma=1)
    P.end()


NT1 = 1152
NKEXT = 256 + 1280


def stage_rope(P, fm, cosT, ssT, out, npair, ncols):
    P.begin()
    cs = P.sb([128, ncols], F32)
    sn = P.sb([128, ncols], F32)
    P.op('sp', lambda e: e.dma_start(out=cs[:], in_=cosT), writes=[cs], dma=1)
    P.op('act', lambda e: e.dma_start(out=sn[:], in_=ssT), writes=[sn], dma=1)
    a = [P.sb([128, ncols], F32) for _ in range(2)]
    b = [P.sb([128, ncols], F32) for _ in range(2)]
    o = [P.sb([128, ncols], BF16) for _ in range(2)]
    for i in range(npair):
        k = i % 2
        P.op('sp', lambda e, i=i, k=k: e.dma_start(out=a[k][:], in_=fm[i]), writes=[a[k]], dma=1)
        P.op('act', lambda e, i=i, k=k: e.dma_start(out=b[k][:], in_=fm[npair + i]), writes=[b[k]], dma=1)
        P.op('dve', lambda e, k=k: e.tensor_tensor(out=a[k][:], in0=a[k][:], in1=cs[:], op=ALU.mult), reads=[a[k], cs], writes=[a[k]])
        P.op('pool', lambda e, k=k: e.tensor_tensor(out=b[k][:], in0=b[k][:], in1=sn[:], op=ALU.mult), reads=[b[k], sn], writes=[b[k]])
        P.op('dve', lambda e, k=k: e.tensor_tensor(out=o[k][:], in0=a[k][:], in1=b[k][:], op=ALU.add), reads=[a[k], b[k]], writes=[o[k]])
        P.op('sp', lambda e, i=i, k=k: e.dma_start(out=out[i], in_=o[k][:]), reads=[o[k]], writes=['ropeout'], dma=1)
    P.end()


def stage_gqa_attn(P, qT, kTd, vd, sink, lrv, attnT, scale):
    P.begin()
    ones_b = P.sb([128, 128], BF16)
    P.op('pool', lambda e: e.memset(ones_b[:], 1.0), writes=[ones_b])
    zero1 = P.sb([128, 1], F32)
    P.op('pool', lambda e: e.memset(zero1[:], 0.0), writes=[zero1])
    mge = tri_mask(P, 'ge')
    mle = tri_mask(P, 'le')
    lr = P.sb([128, 2], F32)
    load_bc(P, 'sp', lr, lrv)
    es = P.sb([128, 32], F32)
    load_bc(P, 'sp', es, sink)
    P.op('act', lambda e: e.activation(out=es[:], in_=es[:], func=AF.Exp), reads=[es], writes=[es])
    m8 = [P.sb([128, 8, 128], BF16) for _ in range(4)]
    for hl in range(8):
        P.op('dve', lambda e, hl=hl: e.tensor_copy(out=m8[0][:, hl, :], in_=mge[:]), reads=[mge], writes=[(m8[0], hl)])
        P.op('dve', lambda e, hl=hl: e.tensor_copy(out=m8[1][:, hl, :], in_=mle[:]), reads=[mle], writes=[(m8[1], hl)])
        P.op('dve', lambda e, hl=hl: e.tensor_scalar(out=m8[2][:, hl, :], in0=mge[:], scalar1=lr[:, 0:1], scalar2=zero1[:, 0:1], op0=ALU.mult, op1=ALU.add),
             reads=[mge, lr, zero1], writes=[(m8[2], hl)])
        P.op('dve', lambda e, hl=hl: e.tensor_scalar(out=m8[3][:, hl, :], in0=mle[:], scalar1=lr[:, 1:2], scalar2=zero1[:, 0:1], op0=ALU.mult, op1=ALU.add),
             reads=[mle, lr, zero1], writes=[(m8[3], hl)])
    m8k = [[(m8[i], hl) for hl in range(8)] for i in range(4)]
    aT = P.sb([128, 16, 1024], BF16)
    ps_s = [[P.ps([128, 512], F32) for _ in range(2)] for _ in range(2)]
    ps_o = [P.ps([128, 512], F32) for _ in range(2)]
    ps_d = [P.ps([128, 512], F32) for _ in range(2)]
    pt = [P.sb([128, 8, 128], BF16) for _ in range(2)]
    den = P.sb([128, 8, 128], F32)
    n = 0
    for g in range(4):
        kT = P.sb([128, 2, NKEXT], BF16)
        vv = P.sb([128, NKEXT // 128, 128], BF16)
        q4 = P.sb([128, 4, 1024], BF16)
        P.op('sp', lambda e, g=g, kT=kT: e.dma_start(out=kT[:], in_=kTd[g].rearrange("v p t -> p v t")), writes=[kT], dma=1)
        P.op('act', lambda e, g=g, vv=vv: e.dma_start(out=vv[:], in_=vd[g].rearrange("(a p) d -> p a d", p=128)), writes=[vv], dma=1)
        P.op('sp', lambda e, g=g, q4=q4: e.dma_start(out=q4[:], in_=qT[4 * g:4 * g + 4, :, 0:1024].rearrange("c p t -> p c t")), writes=[q4], dma=1)
        for qb in range(8):
            qs = slice(qb * 128, (qb + 1) * 128)
            ktiles = [(0, None), (1, None), (2 + qb, 2 if qb == 0 else 0), (3 + qb, None), (4 + qb, 3 if qb == 7 else 1)]
            for ki, (kt, mi) in enumerate(ktiles):
                k = n % 2
                n += 1
                ks = slice(kt * 128, (kt + 1) * 128)
                for hl in range(8):
                    P.op('pe', lambda e, k=k, ks=ks, hl=hl, qs=qs, kT=kT, q4=q4: e.matmul(ps_s[k][hl // 4][:, (hl % 4) * 128:(hl % 4 + 1) * 128], lhsT=kT[:, hl % 2, ks],
                                                                                rhs=q4[:, hl // 2, qs], start=True, stop=True),
                         reads=[kT, q4], writes=[ps_s[k][hl // 4]])
                for half in range(2):
                    P.op('act', lambda e, k=k, half=half: e.activation(out=pt[k][:, half * 4:(half + 1) * 4, :], in_=ps_s[k][half][:].rearrange("p (a b) -> p a b", a=4),
                                                                     func=AF.Exp, scale=float(scale)), reads=[ps_s[k][half]], writes=[(pt[k], half)])
                if mi is not None:
                    P.op('pool', lambda e, k=k, mi=mi: e.tensor_tensor(out=pt[k][:], in0=pt[k][:], in1=m8[mi][:], op=ALU.mult), reads=[(pt[k], 0), (pt[k], 1)] + m8k[mi], writes=[(pt[k], 0), (pt[k], 1)])
                for half in range(2):
                    P.op('pe', lambda e, k=k, kt=kt, ki=ki, half=half, vv=vv: e.matmul(ps_o[half][:], lhsT=vv[:, kt, :],
                                                                                 rhs=pt[k][:, half * 4:(half + 1) * 4, :], start=(ki == 0), stop=(ki == 4)),
                         reads=[vv, (pt[k], half)], writes=[ps_o[half]])
                    P.op('pe', lambda e, k=k, ki=ki, half=half: e.matmul(ps_d[half][:], lhsT=ones_b[:],
                                                                       rhs=pt[k][:, half * 4:(half + 1) * 4, :], start=(ki == 0), stop=(ki == 4)),
                         reads=[ones_b, (pt[k], half)], writes=[ps_d[half]])
            for hl in range(8):
                h = 8 * g + hl
                P.op('dve', lambda e, hl=hl, h=h: e.tensor_scalar(out=den[:, hl, :], in0=ps_d[hl // 4][:, (hl % 4) * 128:(hl % 4 + 1) * 128], scalar1=es[:, h:h + 1], scalar2=zero1[:, 0:1],
                                                                  op0=ALU.add, op1=ALU.add), reads=[ps_d[hl // 4], es, zero1], writes=[(den, hl)])
            P.op('dve', lambda e: e.reciprocal(out=den[:], in_=den[:]), reads=[(den, hl) for hl in range(8)], writes=[(den, hl) for hl in range(8)])
            for hl in range(8):
                r0 = (hl % 2) * 64
                m = 4 * g + hl // 2
                P.op('dve', lambda e, hl=hl, r0=r0, m=m, qs=qs: e.tensor_tensor(out=aT[r0:r0 + 64, m, qs], in0=ps_o[hl // 4][r0:r0 + 64, (hl % 4) * 128:(hl % 4 + 1) * 128],
                                                                              in1=den[r0:r0 + 64, hl, :], op=ALU.mult), reads=[ps_o[hl // 4], (den, hl)], writes=[(aT, m, qb)])
    allk = [(aT, m, qb) for m in range(16) for qb in range(8)]
    for c4 in range(4):
        P.op('sp', lambda e, c4=c4: e.dma_start(out=attnT[:, c4 * 4:(c4 + 1) * 4, 0:1024], in_=aT[:, c4 * 4:(c4 + 1) * 4, :]), reads=allk, writes=['attnT'], dma=1)
    P.end()


def stage_router(P, uT32, wr, sel, wgt, ntiles):
    P.begin()
    NT = ntiles * 128
    ws = P.sb([128, 16, 8], F32)
    P.op('sp', lambda e: e.dma_start(out=ws[:], in_=wr.rearrange("(c p) n -> p c n", p=128)), writes=[ws], dma=1)
    zero1 = P.sb([128, 1], F32)
    one1 = P.sb([128, 1], F32)
    negone1 = P.sb([128, 1], F32)
    negbig1 = P.sb([128, 1], F32)
    P.op('pool', lambda e: e.memset(zero1[:], 0.0), writes=[zero1])
    P.op('pool', lambda e: e.memset(one1[:], 1.0), writes=[one1])
    P.op('pool', lambda e: e.memset(negone1[:], -1.0), writes=[negone1])
    P.op('pool', lambda e: e.memset(negbig1[:], -1e30), writes=[negbig1])
    ut = [P.sb([128, 16, 128], F32) for _ in range(2)]
    pl = [P.ps([128, 512], F32) for _ in range(2)]
    for t in range(ntiles):
        b = t % 2
        lg = P.sb([128, 8], F32, name=f"lg{b}") if t < 2 else lg_[b]
        if t < 2:
            if t == 0:
                lg_ = {}
                tmp_ = {}
            lg_[b] = lg
            tmp_[b] = [P.sb([128, 8], F32) for _ in range(3)] + [P.sb([128, 1], F32) for _ in range(4)]
        e8, l2, so, m1, m2, nm1, dn = tmp_[b]
        P.op('sp', lambda e, t=t, b=b: e.dma_start(out=ut[b][:], in_=uT32[:, :, t * 128:(t + 1) * 128]), writes=[ut[b]], dma=1)
        for c in range(16):
            P.op('pe', lambda e, b=b, c=c: e.matmul(pl[b][:, 0:8], lhsT=ut[b][:, c, :], rhs=ws[:, c, :], start=(c == 0), stop=(c == 15)), reads=[ut[b], ws], writes=[pl[b]])
        P.op('dve', lambda e, b=b, lg=lg: e.tensor_copy(out=lg[:], in_=pl[b][:, 0:8]), reads=[pl[b]], writes=[lg])
        P.op('dve', lambda e, lg=lg, m1=m1: e.reduce_max(out=m1[:], in_=lg[:], axis=AX.X), reads=[lg], writes=[m1])
        P.op('dve', lambda e, lg=lg, m1=m1, l2=l2: e.tensor_scalar(out=l2[:], in0=lg[:], scalar1=m1[:, 0:1], scalar2=negbig1[:, 0:1], op0=ALU.is_ge, op1=ALU.mult),
             reads=[lg, m1, negbig1], writes=[l2])
        P.op('dve', lambda e, lg=lg, l2=l2: e.tensor_tensor(out=l2[:], in0=l2[:], in1=lg[:], op=ALU.add), reads=[l2, lg], writes=[l2])
        P.op('dve', lambda e, l2=l2, m2=m2: e.reduce_max(out=m2[:], in_=l2[:], axis=AX.X), reads=[l2], writes=[m2])
        P.op('dve', lambda e, lg=lg, m2=m2, so=so: e.tensor_scalar(out=so[:], in0=lg[:], scalar1=m2[:, 0:1], scalar2=zero1[:, 0:1], op0=ALU.is_ge, op1=ALU.add),
             reads=[lg, m2, zero1], writes=[so])
        P.op('dve', lambda e, m1=m1, nm1=nm1: e.tensor_scalar(out=nm1[:], in0=m1[:], scalar1=negone1[:, 0:1], scalar2=zero1[:, 0:1], op0=ALU.mult, op1=ALU.add),
             reads=[m1, negone1, zero1], writes=[nm1])
        P.op('act', lambda e, lg=lg, nm1=nm1, e8=e8: e.activation(out=e8[:], in_=lg[:], func=AF.Exp, bias=nm1[:, 0:1], scale=one1[:, 0:1]), reads=[lg, nm1, one1], writes=[e8])
        P.op('act', lambda e, m2=m2, nm1=nm1, dn=dn: e.activation(out=dn[:], in_=m2[:], func=AF.Exp, bias=nm1[:, 0:1], scale=one1[:, 0:1]), reads=[m2, nm1, one1], writes=[dn])
        P.op('dve', lambda e, dn=dn: e.tensor_scalar(out=dn[:], in0=dn[:], scalar1=one1[:, 0:1], scalar2=zero1[:, 0:1], op0=ALU.add, op1=ALU.add), reads=[dn, one1, zero1], writes=[dn])
        P.op('dve', lambda e, dn=dn: e.reciprocal(out=dn[:], in_=dn[:]), reads=[dn], writes=[dn])
        P.op('dve', lambda e, e8=e8, so=so: e.tensor_tensor(out=e8[:], in0=e8[:], in1=so[:], op=ALU.mult), reads=[e8, so], writes=[e8])
        P.op('dve', lambda e, e8=e8, dn=dn: e.tensor_scalar(out=e8[:], in0=e8[:], scalar1=dn[:, 0:1], scalar2=zero1[:, 0:1], op0=ALU.mult, op1=ALU.add), reads=[e8, dn, zero1], writes=[e8])
        P.op('sp', lambda e, t=t, so=so: e.dma_start(out=sel[t * 128:(t + 1) * 128, :], in_=so[:]), reads=[so], writes=['sel'], dma=1)
        P.op('sp', lambda e, t=t, e8=e8: e.dma_start(out=wgt[t * 128:(t + 1) * 128, :], in_=e8[:]), reads=[e8], writes=['wgt'], dma=1)
    P.end()


def stage_combine(P, y1, y2, w12, ysum, ntiles):
    P.begin()
    zero1 = P.sb([128, 1], F32)
    P.op('pool', lambda e: e.memset(zero1[:], 0.0), writes=[zero1])
    a = [P.sb([128, D], F32) for _ in range(2)]
    b = [P.sb([128, D], F32) for _ in range(2)]
    w = [P.sb([128, 2], F32) for _ in range(2)]
    for t in range(ntiles):
        k = t % 2
        rs = slice(t * 128, (t + 1) * 128)
        P.op('sp', lambda e, k=k, rs=rs: e.dma_start(out=a[k][:], in_=y1[rs, :]), writes=[a[k]], dma=1)
        P.op('act', lambda e, k=k, rs=rs: e.dma_start(out=b[k][:], in_=y2[rs, :]), writes=[b[k]], dma=1)
        P.op('sp', lambda e, k=k, rs=rs: e.dma_start(out=w[k][:], in_=w12[rs, :]), writes=[w[k]], dma=1)
        P.op('pool', lambda e, k=k: e.tensor_scalar(out=a[k][:], in0=a[k][:], scalar1=w[k][:, 0:1], scalar2=zero1[:, 0:1], op0=ALU.mult, op1=ALU.add),
             reads=[a[k], w[k], zero1], writes=[a[k]])
        P.op('dve', lambda e, k=k: e.scalar_tensor_tensor(out=b[k][:], in0=b[k][:], scalar=w[k][:, 1:2], in1=a[k][:], op0=ALU.mult, op1=ALU.add),
             reads=[a[k], b[k], w[k]], writes=[b[k]])
        P.op('sp', lambda e, k=k, rs=rs: e.dma_start(out=ysum[rs, :], in_=b[k][:]), reads=[b[k]], writes=['ysum'], dma=1)
    P.end()


def stage_transpose(P, src, dst, ntiles, nch):
    P.begin()
    NT = ntiles * 128
    ident = make_ident(P)
    xt = [P.sb([128, nch * 128], F32) for _ in range(2)]
    ot = P.sb([128, nch, NT], BF16)
    pst = [P.ps([128, 512], F32) for _ in range(4)]
    n = 0
    for t in range(ntiles):
        b = t % 2
        P.op('sp', lambda e, t=t, b=b: e.dma_start(out=xt[b][:], in_=src[t * 128:(t + 1) * 128, :]), writes=[xt[b]], dma=1)
        for g4 in range(nch // 4):
            ps = pst[n % 4]
            for j in range(4):
                c = g4 * 4 + j
                P.op('pe', lambda e, ps=ps, j=j, c=c, b=b: e.transpose(out=ps[:, j * 128:(j + 1) * 128], in_=xt[b][:, c * 128:(c + 1) * 128], identity=ident[:]),
                     reads=[xt[b], ident], writes=[ps])
            if n % 2 == 0:
                P.op('act', lambda e, ps=ps, g4=g4, t=t: e.copy(out=ot[:, g4 * 4:(g4 + 1) * 4, t * 128:(t + 1) * 128], in_=ps[:].rearrange("p (a b) -> p a b", a=4)),
                     reads=[ps], writes=[(ot, t, g4)])
            else:
                P.op('dve', lambda e, ps=ps, g4=g4, t=t: e.tensor_copy(out=ot[:, g4 * 4:(g4 + 1) * 4, t * 128:(t + 1) * 128], in_=ps[:].rearrange("p (a b) -> p a b", a=4)),
                     reads=[ps], writes=[(ot, t, g4)])
            n += 1
    allk = [(ot, t, g4) for t in range(ntiles) for g4 in range(nch // 4)]
    P.op('sp', lambda e: e.dma_start(out=dst, in_=ot[:]), reads=allk, writes=['tdst'], dma=1)
    P.end()


import ml_dtypes as _mld
_BF = _mld.bfloat16
_DBG = {}
GROUPS9 = [(0, 8, 0), (8, 9, 1)]
BLOCKS9 = [(0, 384), (384, 768), (768, 1152)]
CAP = 2304
PIECES_B = [(0, 9), (9, 9), (18, 8), (26, 8)]
FM_ROWS_B = [128] * 16 + [64, 64]


def _dt(nc, name, shape, dtype, kind):
    return nc.dram_tensor(name, list(shape), dtype, kind=kind).ap()


def _run(nc, in_maps):
    res = run_bass_kernel_spmd(nc, in_maps, core_ids=list(range(8)))
    return res.results


def _rope_angles():
    row = np.repeat(np.arange(64), 64).astype(np.float32)
    col = np.tile(np.arange(64), 64).astype(np.float32)
    inv = (np.float32(10000.0) ** (-np.arange(16, dtype=np.float32) / np.float32(16))).astype(np.float32)
    ang = np.concatenate([row[:, None] * inv, col[:, None] * inv], axis=-1).astype(np.float32)
    return np.cos(ang).astype(np.float32), np.sin(ang).astype(np.float32)


def _G_of(modq, b):
    return np.ascontiguousarray(np.stack([modq[b * 4 + q] for q in range(4)]))


def build_L1():
    nc = bass.Bass("TRN2", target_bir_lowering=False)
    cv = _dt(nc, "cv", [128, 16, 2], F32, "ExternalInput")
    adaw = _dt(nc, "adaw", [2048, 6144], F32, "ExternalInput")
    adab = _dt(nc, "adab", [6144], F32, "ExternalInput")
    modq = _dt(nc, "modq", [2, 6144], F32, "ExternalOutput")
    P = Prog(nc)
    stage_ada(P, cv, adaw, adab, modq)
    P.close()
    return nc


def build_L2():
    nc = bass.Bass("TRN2", target_bir_lowering=False)
    I, O, N = "ExternalInput", "ExternalOutput", "Internal"
    h = _dt(nc, "h", [NTB, D], F32, I)
    G = _dt(nc, "G", [4, 2, 6144], F32, I)
    ng = _dt(nc, "ng", [4, D], F32, I)
    wfm = _dt(nc, "wfm", [D, sum(FM_ROWS_B)], F32, I)
    wtm = _dt(nc, "wtm", [D, 264], F32, I)
    alog8 = _dt(nc, "alog8", [8], F32, I)
    dtb8 = _dt(nc, "dtb8", [8], F32, I)
    convw = _dt(nc, "convw", [6, 128, 5], F32, I)
    dng = _dt(nc, "dng", [128], F32, I)
    qg = _dt(nc, "qg", [768], F32, I)
    kvg = _dt(nc, "kvg", [512], F32, I)
    wq = _dt(nc, "wq", [768, 512], F32, I)
    wkv = _dt(nc, "wkv", [512, 512], F32, I)
    cosT = _dt(nc, "cosT", [64, NTB], F32, I)
    ssT = _dt(nc, "ssT", [64, NTB], F32, I)
    uT = _dt(nc, "uT", [128, 16, NTB], BF16, N)
    fm = _dt(nc, "fm", [18, 128, NTB], F32, N)
    tm = _dt(nc, "tm", [NTB, 264], F32, N)
    fmT = _dt(nc, "fmT", [4, 128, NTB], BF16, N)
    tokm = _dt(nc, "tokm", [4, NTB, 128], BF16, N)
    qkT = _dt(nc, "qkT", [2, 3, 128, NTB], BF16, N)
    krT = _dt(nc, "krT", [64, NTB], BF16, N)
    vtok = _dt(nc, "vtok", [2, NTB, 128], BF16, N)
    dnout = _dt(nc, "dnout", [NTB, 256], F32, O)
    oT = _dt(nc, "oT", [2, 128, NTB], BF16, O)
    P = Prog(nc)
    for (t0, n) in PIECES_B:
        groups = [(t, t + 1, 1 if (t0 + t) < 2 else 0) for t in range(n)]
        stage_modulate(P, h, uT, G, None, 0, 1, ng[0], groups, n, t0=t0)
    stage_proj(P, uT, wfm, fm, FM_ROWS_B, wtm, tm, 264)
    stage_dn_pre(P, fm[0:6], convw, fmT, tokm)
    stage_dn_scan(P, fmT, tokm, tm, alog8, dtb8, dng, dnout)
    stage_mla_proj(P, fm[6:18], qg, kvg, wq, wkv, cosT, ssT, qkT, krT, vtok)
    stage_mla_attn(P, qkT, krT, vtok, oT, 192 ** -0.5)
    P.close()
    return nc


def build_L3():
    nc = bass.Bass("TRN2", target_bir_lowering=False)
    I, O, N = "ExternalInput", "ExternalOutput", "Internal"
    dn_tok = _dt(nc, "dn_tok", [NT1, 1024], F32, I)
    mlaT = _dt(nc, "mlaT", [128, 8, NT1], BF16, I)
    hin = _dt(nc, "hin", [NT1, D], F32, I)
    G = _dt(nc, "G", [4, 2, 6144], F32, I)
    ng = _dt(nc, "ng", [2, 4, D], F32, I)
    wout = _dt(nc, "wout", [D, D], F32, I)
    wg = _dt(nc, "wg", [D, DFF], F32, I)
    wu = _dt(nc, "wu", [D, DFF], F32, I)
    wd = _dt(nc, "wd", [DFF, D], F32, I)
    wfm = _dt(nc, "wfm", [D, 36 * 128], F32, I)
    bfm = _dt(nc, "bfm", [36 * 128], F32, I)
    wtm = _dt(nc, "wtm", [D, 256], F32, I)
    btm = _dt(nc, "btm", [256], F32, I)
    cosT = _dt(nc, "cosT", [128, NT1], F32, I)
    ssT = _dt(nc, "ssT", [128, NT1], F32, I)
    dnT = _dt(nc, "dnT", [128, 8, NT1], BF16, N)
    y1 = _dt(nc, "y1", [NT1, D], F32, N)
    hm = _dt(nc, "hm", [NT1, D], F32, N)
    uT = _dt(nc, "uT", [128, 16, NT1], BF16, N)
    y2 = _dt(nc, "y2", [NT1, D], F32, N)
    h0 = _dt(nc, "h0", [NT1, D], F32, O)
    uT1 = _dt(nc, "uT1", [128, 16, NT1], BF16, N)
    fm = _dt(nc, "fm", [36, 128, NT1], F32, N)
    qk = _dt(nc, "qk", [18, 128, NT1], BF16, O)
    vt = _dt(nc, "vt", [NT1, 256], F32, O)
    P = Prog(nc)
    stage_transpose(P, dn_tok, dnT, 9, 8)
    stage_outproj(P, [(dnT, 8), (mlaT, 8)], wout, y1, 9)
    stage_resid(P, y1, hin, hm, G, 2, ng[0, 1], GROUPS9)
    stage_modulate(P, hm, uT, G, None, 3, 4, ng[0, 2], GROUPS9, 9)
    stage_ffn(P, uT, wg, wu, wd, y2, 9, BLOCKS9)
    stage_resid(P, y2, hm, h0, G, 5, ng[0, 3], GROUPS9)
    stage_modulate(P, h0, uT1, G, None, 6, 7, ng[1, 0], GROUPS9, 9)
    stage_proj(P, uT1, wfm, fm, [128] * 36, wtm, vt, 256, ntiles=9, bias_fm=bfm, bias_tm=btm)
    stage_rope(P, fm, cosT, ssT, qk, 18, NT1)
    P.close()
    return nc


def build_L4():
    nc = bass.Bass("TRN2", target_bir_lowering=False)
    I, O, N = "ExternalInput", "ExternalOutput", "Internal"
    qT = _dt(nc, "qT", [16, 128, NT1], BF16, I)
    kTd = _dt(nc, "kTd", [4, 2, 128, NKEXT], BF16, I)
    vd = _dt(nc, "vd", [4, NKEXT, 128], BF16, I)
    sink = _dt(nc, "sink", [32], F32, I)
    lrv = _dt(nc, "lrv", [2], F32, I)
    h0 = _dt(nc, "h0", [1024, D], F32, I)
    G = _dt(nc, "G", [4, 2, 6144], F32, I)
    ng = _dt(nc, "ng", [2, 4, D], F32, I)
    wout = _dt(nc, "wout", [D, D], F32, I)
    bout = _dt(nc, "bout", [D], F32, I)
    wr = _dt(nc, "wr", [D, 8], F32, I)
    attnT = _dt(nc, "attnT", [128, 16, NT1], BF16, N)
    y1 = _dt(nc, "y1", [1024, D], F32, N)
    h1m = _dt(nc, "h1m", [1024, D], F32, O)
    uT = _dt(nc, "uT", [128, 16, 1024], BF16, O)
    uT32 = _dt(nc, "uT32", [128, 16, 1024], F32, N)
    sel = _dt(nc, "sel", [1024, 8], F32, O)
    wgt = _dt(nc, "wgt", [1024, 8], F32, O)
    P = Prog(nc)
    L8 = [(0, 8, 0)]
    stage_gqa_attn(P, qT, kTd, vd, sink, lrv, attnT, 64 ** -0.5)
    stage_outproj(P, attnT[:, :, 0:1024], wout, y1, 8, bias=bout)
    stage_resid(P, y1, h0, h1m, G, 8, ng[1, 1], L8)
    stage_modulate(P, h1m, uT, G, None, 9, 10, ng[1, 2], L8, 8, uT32=uT32)
    stage_router(P, uT32, wr, sel, wgt, 8)
    P.close()
    return nc


NGRP = 3


def build_L5(NGRP=NGRP):
    nc = bass.Bass("TRN2", target_bir_lowering=False)
    I, O, N = "ExternalInput", "ExternalOutput", "Internal"
    xT = _dt(nc, "xT", [NGRP, 128, 16, NT1], BF16, I)
    wg = _dt(nc, "wg", [NGRP, D, DFF], F32, I)
    wu = _dt(nc, "wu", [NGRP, D, DFF], F32, I)
    wd = _dt(nc, "wd", [NGRP, DFF, D], F32, I)
    ye = _dt(nc, "ye", [NGRP, NT1, D], F32, O)
    P = Prog(nc)
    for gi in range(NGRP):
        stage_ffn(P, xT[gi], wg[gi], wu[gi], wd[gi], ye[gi], 9, BLOCKS9)
    P.close()
    return nc


def build_L6():
    nc = bass.Bass("TRN2", target_bir_lowering=False)
    I, O, N = "ExternalInput", "ExternalOutput", "Internal"
    ya = _dt(nc, "ya", [1024, D], F32, I)
    yb = _dt(nc, "yb", [1024, D], F32, I)
    w12 = _dt(nc, "w12", [1024, 2], F32, I)
    h1m = _dt(nc, "h1m", [1024, D], F32, I)
    G = _dt(nc, "G", [4, 2, 6144], F32, I)
    ng = _dt(nc, "ng", [2, 4, D], F32, I)
    ysum = _dt(nc, "ysum", [1024, D], F32, N)
    out = _dt(nc, "out", [1024, D], F32, O)
    P = Prog(nc)
    stage_combine(P, ya, yb, w12, ysum, 8)
    stage_resid(P, ysum, h1m, out, G, 11, ng[1, 3], [(0, 8, 0)])
    P.close()
    return nc


def kernel(x, c, ctx, c_ctx, ada_w, ada_b, norm_g, ab_w_in, dn_conv_w, dn_a_log, dn_dt_bias, dn_norm_g,
           mla_q_norm_g, mla_w_qb, mla_kv_norm_g, mla_w_kvb, ab_w_out, ffn_w_gate, ffn_w_up, ffn_w_down,
           gqa_w_qkv, gqa_b_qkv, gqa_sink, gqa_w_out, gqa_b_out, moe_w_router, moe_w_gate, moe_w_up, moe_w_down):
    f32 = np.float32
    A = lambda v: np.ascontiguousarray(np.asarray(v))
    x, c, ctx, c_ctx = A(x), A(c), A(ctx), A(c_ctx)
    norm_g = A(norm_g)
    ims = []
    for core in range(8):
        b, j = core // 4, core % 4
        cvv = np.stack([c[b], c_ctx], axis=-1).reshape(16, 128, 2).transpose(1, 0, 2)
        ims.append({"cv": A(cvv), "adaw": A(ada_w[j // 2][:, (j % 2) * 6144:(j % 2 + 1) * 6144]),
                    "adab": A(ada_b[j // 2][(j % 2) * 6144:(j % 2 + 1) * 6144])})
    r1 = _run(build_L1(), ims)
    modq = [np.asarray(r["modq"]) for r in r1]
    Gb = [_G_of(modq, 0), _G_of(modq, 1)]
    W = np.asarray(ab_w_in[0])
    cos, sin = _rope_angles()
    cosB = np.ones((64, NTB), f32); ssB = np.zeros((64, NTB), f32)
    cosB[:32, 256:] = cos.T; cosB[32:, 256:] = cos.T
    ssB[:32, 256:] = -sin.T; ssB[32:, 256:] = sin.T
    wqb, wkvb = np.asarray(mla_w_qb[0]), np.asarray(mla_w_kvb[0])
    ims = []
    for core in range(8):
        b, j = core // 4, core % 4
        hs = [2 * j, 2 * j + 1]
        cols = []
        for h in hs:
            for ty in range(3):
                cols += list(range(ty * 1024 + h * 128, ty * 1024 + (h + 1) * 128))
        cols += list(range(4128, 4128 + 768 + 512 + 64))
        cols += list(range(5408 + 32, 5408 + 64)) + list(range(5408, 5408 + 32))
        tcols = []
        for h in hs:
            tcols += list(range(3072 + h * 128, 3072 + (h + 1) * 128))
        for h in hs:
            tcols += [4096 + h, 4096 + 8 + h, 4096 + 16 + h, 4096 + 24 + h]
        al = np.zeros(8, f32); db = np.zeros(8, f32)
        cw = np.zeros((6, 128, 5), f32)
        for hh, h in enumerate(hs):
            for d in range(2):
                al[hh * 4 + 2 + d] = dn_a_log[0][d, h]
                db[hh * 4 + 2 + d] = dn_dt_bias[0][d, h]
            for ty in range(3):
                ch = ty * 1024 + h * 128
                cw[hh * 3 + ty] = np.asarray(dn_conv_w[0])[:, ch:ch + 128].T
        qc = []
        for h in hs: qc += list(range(h * 192, h * 192 + 128))
        for h in hs: qc += list(range(h * 192 + 128, h * 192 + 192))
        for h in hs: qc += list(range(h * 192 + 160, h * 192 + 192)) + list(range(h * 192 + 128, h * 192 + 160))
        kc = []
        for h in hs: kc += list(range(h * 256, h * 256 + 128))
        for h in hs: kc += list(range(h * 256 + 128, h * 256 + 256))
        ims.append({"h": A(np.concatenate([ctx[b], x[b]], axis=0)), "G": Gb[b], "ng": A(norm_g[0]), "wfm": A(W[:, cols]), "wtm": A(W[:, tcols]),
                    "alog8": al, "dtb8": db, "convw": cw, "dng": A(dn_norm_g[0]), "qg": A(mla_q_norm_g[0]), "kvg": A(mla_kv_norm_g[0]),
                    "wq": A(wqb[:, qc]), "wkv": A(wkvb[:, kc]), "cosT": cosB, "ssT": ssB})
    r2 = _run(build_L2(), ims)
    Wq = np.asarray(gqa_w_qkv[0]); Bq = np.asarray(gqa_b_qkv[0])
    qp = []
    for h in range(32):
        qp += list(range(h * 64 + 32, h * 64 + 64)) + list(range(h * 64, h * 64 + 32))
    kp = []
    for g in range(4):
        kp += list(range(2048 + g * 64 + 32, 2048 + g * 64 + 64)) + list(range(2048 + g * 64, 2048 + g * 64 + 32))
    pc = list(range(2048)) + list(range(2048, 2304)) + qp + kp

    def tok_rows(j):
        return np.concatenate([256 + j * 1024 + np.arange(1024), j * 64 + np.arange(64)])
    ims = []
    for core in range(8):
        b, j = core // 4, core % 4
        rows = tok_rows(j)
        dn_tok = np.zeros((NT1, 1024), f32)
        mlaT = np.zeros((128, 8, NT1), _BF)
        for q in range(4):
            rq = r2[b * 4 + q]
            dn_tok[:1088, q * 256:(q + 1) * 256] = np.asarray(rq["dnout"])[rows]
            oT = np.asarray(rq["oT"])
            for hh in range(2):
                mlaT[:, q * 2 + hh, :1088] = oT[hh][:, rows]
        hin = np.zeros((NT1, D), f32)
        hin[:1024] = x[b, j * 1024:(j + 1) * 1024]; hin[1024:1088] = ctx[b, j * 64:(j + 1) * 64]
        cosT = np.ones((128, NT1), f32); ssT = np.zeros((128, NT1), f32)
        cj, sj = cos[j * 1024:(j + 1) * 1024].T, sin[j * 1024:(j + 1) * 1024].T
        for r in range(2):
            cosT[r * 64:r * 64 + 32, :1024] = cj; cosT[r * 64 + 32:r * 64 + 64, :1024] = cj
            ssT[r * 64:r * 64 + 32, :1024] = -sj; ssT[r * 64 + 32:r * 64 + 64, :1024] = sj
        ims.append({"dn_tok": dn_tok, "mlaT": mlaT, "hin": hin, "G": Gb[b], "ng": norm_g, "wout": A(ab_w_out[0]), "wg": A(ffn_w_gate[0]),
                    "wu": A(ffn_w_up[0]), "wd": A(ffn_w_down[0]), "wfm": A(Wq[:, pc]), "bfm": A(Bq[pc]), "wtm": A(Wq[:, 2304:2560]),
                    "btm": A(Bq[2304:2560]), "cosT": cosT, "ssT": ssT})
    r3 = _run(build_L3(), ims)
    _DBG['r2'] = r2; _DBG['r3'] = r3
    ims = []
    for core in range(8):
        b, j = core // 4, core % 4
        kT = np.zeros((4, 2, 128, NKEXT), _BF); vd = np.zeros((4, NKEXT, 128), _BF)

        def kv_of(cc, lo, hi):
            return np.asarray(r3[cc]["qk"])[16:18][:, :, lo:hi], np.asarray(r3[cc]["vt"])[lo:hi]
        segs = [(jj * 64, kv_of(b * 4 + jj, 1024, 1088)) for jj in range(4)]
        if j > 0:
            segs.append((256, kv_of(core - 1, 896, 1024)))
        segs.append((384, kv_of(core, 0, 1024)))
        if j < 3:
            segs.append((1408, kv_of(core + 1, 0, 128)))
        for off, (kk, vv) in segs:
            n = kk.shape[2]
            for g in range(4):
                rws = kk[g // 2, (g % 2) * 64:(g % 2) * 64 + 64]
                kT[g, 0, 0:64, off:off + n] = rws; kT[g, 1, 64:128, off:off + n] = rws
                v_ = vv[:, g * 64:(g + 1) * 64]
                vd[g, off:off + n, 0:64] = v_; vd[g, off:off + n, 64:128] = v_
        ims.append({"qT": A(np.asarray(r3[core]["qk"])[0:16]), "kTd": kT, "vd": vd, "sink": A(gqa_sink[0]),
                    "lrv": np.array([1.0 if j > 0 else 0.0, 1.0 if j < 3 else 0.0], f32), "h0": A(np.asarray(r3[core]["h0"])[:1024]),
                    "G": Gb[b], "ng": norm_g, "wout": A(gqa_w_out[0]), "bout": A(gqa_b_out[0]), "wr": A(moe_w_router[0])})
    r4 = _run(build_L4(), ims)
    _DBG['r4'] = r4
    sel = np.concatenate([np.asarray(r["sel"]) for r in r4], axis=0)
    wgt = np.concatenate([np.asarray(r["wgt"]) for r in r4], axis=0)
    uT_all = np.concatenate([np.asarray(r["uT"]) for r in r4], axis=2)
    items = []
    for e in range(8):
        tok = np.nonzero(sel[:, e] > 0.5)[0]
        for k0 in range(0, len(tok), NT1):
            items.append((e, tok[k0:k0 + NT1]))
    NGRP = max(1, -(-len(items) // 8))
    assert NGRP <= 3
    wge, wue, wde = np.asarray(moe_w_gate[0]), np.asarray(moe_w_up[0]), np.asarray(moe_w_down[0])
    ims = []
    for core in range(8):
        xT = np.zeros((NGRP, 128, 16, NT1), _BF)
        es = []
        for gi in range(NGRP):
            idx = core * NGRP + gi
            e = 0
            if idx < len(items):
                e, tok = items[idx]
                xT[gi, :, :, :len(tok)] = uT_all[:, :, tok]
            es.append(e)
        ims.append({"xT": xT, "wg": A(wge[es]), "wu": A(wue[es]), "wd": A(wde[es])})
    r5 = _run(build_L5(NGRP), ims)
    _DBG['r5'] = r5
    ye_all = np.concatenate([np.asarray(r["ye"]).reshape(NGRP * NT1, D) for r in r5], axis=0)
    pos = np.full((8192, 8), -1, np.int64)
    for idx, (e, tok) in enumerate(items):
        pos[tok, e] = idx * NT1 + np.arange(len(tok))
    order = np.argsort(-sel, axis=1, kind="stable")[:, :2]
    ims = []
    for core in range(8):
        b, j = core // 4, core % 4
        t0 = core * 1024
        ya = np.zeros((1024, D), f32); yb = np.zeros((1024, D), f32); w12 = np.zeros((1024, 2), f32)
        tt = np.arange(t0, t0 + 1024)
        for i, dst in enumerate((ya, yb)):
            ee = order[tt, i]
            pp = pos[tt, ee]
            m = pp >= 0
            dst[m] = ye_all[pp[m]]
            w12[m, i] = wgt[tt[m], ee[m]]
        ims.append({"ya": ya, "yb": yb, "w12": w12, "h1m": A(np.asarray(r4[core]["h1m"])), "G": Gb[b], "ng": norm_g})
    r6 = _run(build_L6(), ims)
    out = np.stack([np.concatenate([np.asarray(r6[b * 4 + j]["out"]) for j in range(4)], axis=0) for b in range(2)])
    return out.astype(f32)
```
